# Optimizing a Trainium2 kernel written in Bass

```python
import math
import jax, jax.numpy as jnp
from jax import lax
import numpy as np

D_MODEL = 1024
BATCH = 8
SEQ = 4096
DEPTH = 4

N_MIXERS = 3
N_A = (DEPTH + 2) // 3
N_B = (DEPTH + 1) // 3
N_C = DEPTH // 3

ROPE_THETA = 10000.0
ROPE_DIM = 64
NORM_EPS = 1e-6
Q_BLOCK = 128
D_FF = -(-8 * D_MODEL // (3 * 256)) * 256

DIFF_HEADS = 8
DIFF_QK_DIM = 64
DIFF_V_DIM = 2 * DIFF_QK_DIM
DSA_HEADS = 16
DSA_HEAD_DIM = 64
IDX_HEADS = 8
IDX_DIM = 64
TOPK_MAX = 256
DSA_IN = DSA_HEADS * DSA_HEAD_DIM + 2 * DSA_HEAD_DIM + IDX_HEADS * IDX_DIM + IDX_DIM + IDX_HEADS
MLA_HEADS = 8
MLA_Q_RANK = 384
MLA_KV_RANK = 256
MLA_NOPE_DIM = 128
MLA_ROPE_DIM = 64
MLA_V_DIM = 128

kernel_name = "hybrid_diff_dsa_mla_trunk"


def _split(a, sizes):
    offs = np.cumsum(sizes)[:-1].tolist()
    return jnp.split(a, offs, axis=-1)


def _rmsnorm(x, g):
    xf = x.astype(jnp.float32)
    y = xf * lax.rsqrt(jnp.mean(xf * xf, axis=-1, keepdims=True) + NORM_EPS)
    return (y * g.astype(jnp.float32)).astype(x.dtype)


def _rope_tables(seq):
    pos = jnp.arange(seq, dtype=jnp.float32)
    inv = 1.0 / (ROPE_THETA ** (jnp.arange(0, ROPE_DIM, 2, dtype=jnp.float32) / ROPE_DIM))
    ang = pos[:, None] * inv[None, :]
    cos = jnp.concatenate([jnp.cos(ang), jnp.cos(ang)], axis=-1)
    sin = jnp.concatenate([jnp.sin(ang), jnp.sin(ang)], axis=-1)
    return cos, sin


def _rope(x, cos, sin):
    if x.ndim == 4:
        cos, sin = cos[:, None, :], sin[:, None, :]
    x1, x2 = jnp.split(x, 2, axis=-1)
    rot = jnp.concatenate([-x2, x1], axis=-1)
    return (x * cos + rot * sin).astype(x.dtype)


def _masked_softmax(s, mask):
    return jax.nn.softmax(jnp.where(mask, s.astype(jnp.float32), -jnp.inf), axis=-1)


def _to_blocks(a):
    b, s = a.shape[:2]
    return jnp.moveaxis(a.reshape((b, s // Q_BLOCK, Q_BLOCK) + a.shape[2:]), 1, 0)


def _from_blocks(a):
    nb, b, qb = a.shape[:3]
    return jnp.moveaxis(a, 0, 1).reshape((b, nb * qb) + a.shape[3:])


def _sweep(fn, *qs):
    nb = qs[0].shape[1] // Q_BLOCK
    out = lax.map(lambda xs: fn(xs[0], *xs[1:]),
                  (jnp.arange(nb, dtype=jnp.int32),) + tuple(_to_blocks(q) for q in qs))
    return _from_blocks(out)


def _causal_mask(blk, seq):
    q_pos = blk * Q_BLOCK + jnp.arange(Q_BLOCK, dtype=jnp.int32)
    k_pos = jnp.arange(seq, dtype=jnp.int32)
    return q_pos, k_pos[None, :] <= q_pos[:, None]


def _diff_attention(h, w_qkv, w_o, lam, subln, cos, sin, lambda_init):
    b, s, _ = h.shape
    q, k, v = _split(h @ w_qkv, [2 * DIFF_HEADS * DIFF_QK_DIM, 2 * DIFF_HEADS * DIFF_QK_DIM, DIFF_HEADS * DIFF_V_DIM])
    q = _rope(q.reshape(b, s, 2 * DIFF_HEADS, DIFF_QK_DIM), cos, sin).reshape(b, s, DIFF_HEADS, 2, DIFF_QK_DIM)
    k = _rope(k.reshape(b, s, 2 * DIFF_HEADS, DIFF_QK_DIM), cos, sin).reshape(b, s, DIFF_HEADS, 2, DIFF_QK_DIM)
    v = v.reshape(b, s, DIFF_HEADS, DIFF_V_DIM)
    lf = lam.astype(jnp.float32)
    lam_full = jnp.exp(jnp.sum(lf[0] * lf[1])) - jnp.exp(jnp.sum(lf[2] * lf[3])) + lambda_init
    scale = DIFF_QK_DIM ** -0.5

    def block(blk, qb):
        _, mask = _causal_mask(blk, s)
        sc = jnp.einsum('bqhmd,bkhmd->bmhqk', qb, k) * scale
        p = _masked_softmax(sc, mask)
        p = p[:, 0] - lam_full * p[:, 1]
        return jnp.einsum('bhqk,bkhe->bqhe', p.astype(v.dtype), v)

    o = _sweep(block, q)
    o = _rmsnorm(o, subln) * (1.0 - lambda_init)
    return o.reshape(b, s, DIFF_HEADS * DIFF_V_DIM) @ w_o


def _dsa_attention(h, w_in, w_o, cos, sin):
    b, s, _ = h.shape
    topk = min(TOPK_MAX, s // 4)
    q, k, v, iq, ik, iw = _split(h @ w_in, [DSA_HEADS * DSA_HEAD_DIM, DSA_HEAD_DIM, DSA_HEAD_DIM,
                                            IDX_HEADS * IDX_DIM, IDX_DIM, IDX_HEADS])
    q = _rope(q.reshape(b, s, DSA_HEADS, DSA_HEAD_DIM), cos, sin)
    k = _rope(k, cos, sin)
    iq = _rope(iq.reshape(b, s, IDX_HEADS, IDX_DIM), cos, sin)
    ik = _rope(ik, cos, sin)
    iw = iw * (IDX_HEADS ** -0.5)
    scale = DSA_HEAD_DIM ** -0.5
    gather = jax.vmap(lambda kb, ib: kb[ib])

    def block(blk, qb, iqb, iwb):
        q_pos, mask = _causal_mask(blk, s)
        logits = jax.nn.relu(jnp.einsum('bqhd,bkd->bqhk', iqb, ik).astype(jnp.float32) * (IDX_DIM ** -0.5))
        score = jnp.einsum('bqhk,bqh->bqk', logits, iwb.astype(jnp.float32))
        score = jnp.where(mask, score, -jnp.inf)
        _, idx = lax.top_k(score, topk)
        valid = idx <= q_pos[None, :, None]
        k_sel = gather(k, idx)
        v_sel = gather(v, idx)
        sc = jnp.einsum('bqhd,bqjd->bqhj', qb, k_sel) * scale
        p = _masked_softmax(sc, valid[:, :, None, :])
        return jnp.einsum('bqhj,bqjd->bqhd', p.astype(v.dtype), v_sel)

    o = _sweep(block, q, iq, iw)
    return o.reshape(b, s, DSA_HEADS * DSA_HEAD_DIM) @ w_o


def _mla(h, w_down, q_norm, kv_norm, w_uq, w_ukv, w_o, cos, sin):
    b, s, _ = h.shape
    cq, ckv, kr = _split(h @ w_down, [MLA_Q_RANK, MLA_KV_RANK, MLA_ROPE_DIM])
    cq = _rmsnorm(cq, q_norm)
    ckv = _rmsnorm(ckv, kv_norm)
    q = (cq @ w_uq).reshape(b, s, MLA_HEADS, MLA_NOPE_DIM + MLA_ROPE_DIM)
    q_nope, q_rope = _split(q, [MLA_NOPE_DIM, MLA_ROPE_DIM])
    q_rope = _rope(q_rope, cos, sin)
    kr = _rope(kr, cos, sin)
    kv = (ckv @ w_ukv).reshape(b, s, MLA_HEADS, MLA_NOPE_DIM + MLA_V_DIM)
    k_nope, v = _split(kv, [MLA_NOPE_DIM, MLA_V_DIM])
    scale = (MLA_NOPE_DIM + MLA_ROPE_DIM) ** -0.5

    def block(blk, qnb, qrb):
        _, mask = _causal_mask(blk, s)
        sc = (jnp.einsum('bqhd,bkhd->bhqk', qnb, k_nope) + jnp.einsum('bqhr,bkr->bhqk', qrb, kr)) * scale
        p = _masked_softmax(sc, mask)
        return jnp.einsum('bhqk,bkhd->bqhd', p.astype(v.dtype), v)

    o = _sweep(block, q_nope, q_rope)
    return o.reshape(b, s, MLA_HEADS * MLA_V_DIM) @ w_o


def _swiglu(h, w1, w3, w2):
    return (jax.nn.silu(h @ w1) * (h @ w3)) @ w2


def setup_inputs(seed: int = 0) -> dict:
    key = jax.random.key(seed)
    ks = jax.random.split(key, 20)
    f32 = jnp.float32

    def w(k, shape, fan_in):
        return jax.random.normal(k, shape, f32) * (fan_in ** -0.5)

    def gain(k, shape):
        return 1.0 + 0.02 * jax.random.normal(k, shape, f32)

    return {
        "x": jax.random.normal(ks[0], (BATCH, SEQ, D_MODEL), f32),
        "attn_norm": gain(ks[1], (DEPTH, D_MODEL)),
        "ffn_norm": gain(ks[2], (DEPTH, D_MODEL)),
        "final_norm": gain(ks[3], (D_MODEL,)),
        "ffn_w1": w(ks[4], (DEPTH, D_MODEL, D_FF), D_MODEL),
        "ffn_w3": w(ks[5], (DEPTH, D_MODEL, D_FF), D_MODEL),
        "ffn_w2": w(ks[6], (DEPTH, D_FF, D_MODEL), D_FF),
        "diff_wqkv": w(ks[7], (N_A, D_MODEL, 4 * DIFF_HEADS * DIFF_QK_DIM + DIFF_HEADS * DIFF_V_DIM), D_MODEL),
        "diff_wo": w(ks[8], (N_A, DIFF_HEADS * DIFF_V_DIM, D_MODEL), DIFF_HEADS * DIFF_V_DIM),
        "diff_lambda": 0.1 * jax.random.normal(ks[9], (N_A, 4, DIFF_QK_DIM), f32),
        "diff_subln": gain(ks[10], (N_A, DIFF_V_DIM)),
        "dsa_win": w(ks[11], (N_B, D_MODEL, DSA_IN), D_MODEL),
        "dsa_wo": w(ks[12], (N_B, DSA_HEADS * DSA_HEAD_DIM, D_MODEL), DSA_HEADS * DSA_HEAD_DIM),
        "mla_wdown": w(ks[13], (N_C, D_MODEL, MLA_Q_RANK + MLA_KV_RANK + MLA_ROPE_DIM), D_MODEL),
        "mla_q_norm": gain(ks[14], (N_C, MLA_Q_RANK)),
        "mla_kv_norm": gain(ks[15], (N_C, MLA_KV_RANK)),
        "mla_wuq": w(ks[16], (N_C, MLA_Q_RANK, MLA_HEADS * (MLA_NOPE_DIM + MLA_ROPE_DIM)), MLA_Q_RANK),
        "mla_wukv": w(ks[17], (N_C, MLA_KV_RANK, MLA_HEADS * (MLA_NOPE_DIM + MLA_V_DIM)), MLA_KV_RANK),
        "mla_wo": w(ks[18], (N_C, MLA_HEADS * MLA_V_DIM, D_MODEL), MLA_HEADS * MLA_V_DIM),
    }


def reference(x, attn_norm, ffn_norm, final_norm, ffn_w1, ffn_w3, ffn_w2,
              diff_wqkv, diff_wo, diff_lambda, diff_subln,
              dsa_win, dsa_wo,
              mla_wdown, mla_q_norm, mla_kv_norm, mla_wuq, mla_wukv, mla_wo):
    cos, sin = _rope_tables(x.shape[1])
    h = x
    for i in range(DEPTH):
        j, m = i // N_MIXERS, i % N_MIXERS
        u = _rmsnorm(h, attn_norm[i])
        if m == 0:
            lambda_init = 0.8 - 0.6 * math.exp(-0.3 * i)
            y = _diff_attention(u, diff_wqkv[j], diff_wo[j], diff_lambda[j], diff_subln[j], cos, sin, lambda_init)
        elif m == 1:
            y = _dsa_attention(u, dsa_win[j], dsa_wo[j], cos, sin)
        else:
            y = _mla(u, mla_wdown[j], mla_q_norm[j], mla_kv_norm[j], mla_wuq[j], mla_wukv[j], mla_wo[j], cos, sin)
        h = h + y
        h = h + _swiglu(_rmsnorm(h, ffn_norm[i]), ffn_w1[i], ffn_w3[i], ffn_w2[i])
    return _rmsnorm(h, final_norm)
```

```python
import math
from contextlib import ExitStack
import numpy as np
import concourse.bass as bass
import concourse.mybir as mybir
from concourse.bass_utils import run_bass_kernel_spmd

F32 = mybir.dt.float32
BF16 = mybir.dt.bfloat16
I32 = mybir.dt.int32
ALU = mybir.AluOpType
AF = mybir.ActivationFunctionType
AX = mybir.AxisListType

S = 4096
D = 1024
DFF = 2816
NCH = 8
TC = 512
EPS = 1e-6
DEPTH = 4
NEG = -1.0e30


class Sem:
    def __init__(self, nc, name):
        self.h = nc.alloc_semaphore(name)
        self.count = 0


class Tok:
    __slots__ = ("sem", "val")

    def __init__(self, sem=None, val=0):
        self.sem = sem
        self.val = val


class Buf:
    __slots__ = ("ap", "w", "r", "excl")

    def __init__(self, ap, excl=False):
        self.ap = ap
        self.w = None
        self.r = {}
        self.excl = excl


class Eng:
    def __init__(self, k, eng, name, is_pe=False):
        self.eng = eng
        self.sem = Sem(k.nc, "e_" + name)
        self.seen = {}
        self.is_pe = is_pe
        self.name = name

    def wait(self, sem, val):
        if val <= self.seen.get(sem, 0):
            return
        self.eng.wait_ge(sem.h, val)
        self.seen[sem] = val


class K:
    def __init__(self, nc):
        self.nc = nc
        self.pe = Eng(self, nc.tensor, "pe", True)
        self.act = Eng(self, nc.scalar, "act")
        self.dve = Eng(self, nc.vector, "dve")
        self.pool = Eng(self, nc.gpsimd, "pool")
        self.sp = Eng(self, nc.sync, "sp")
        self.engs = [self.pe, self.act, self.dve, self.pool, self.sp]
        self.dsems = []
        self.fixed = []
        self.nsem = 0

    def dsem(self, name=None):
        if self.nsem < len(self.dsems):
            s = self.dsems[self.nsem]
        else:
            s = Sem(self.nc, "d%d" % self.nsem)
            self.dsems.append(s)
        self.nsem += 1
        return s

    def _w(self, e, tok):
        if e.is_pe and tok.sem is e.sem:
            return
        e.wait(tok.sem, tok.val)

    def _deps(self, e, reads, writes):
        for b in reads:
            if b.w is not None:
                self._w(e, b.w)
            if b.excl:
                for t in b.r.values():
                    if t.sem is not e.sem:
                        self._w(e, t)
        for b in writes:
            if b.w is not None:
                self._w(e, b.w)
            for t in b.r.values():
                self._w(e, t)

    def _upd(self, tok, reads, writes):
        for b in writes:
            b.w = tok
            b.r = {}
        for b in reads:
            if b not in writes:
                o = b.r.get(tok.sem)
                if o is None or o.val < tok.val or o is tok:
                    b.r[tok.sem] = tok

    def op(self, e, fn, reads=(), writes=(), signal=True):
        self._deps(e, reads, writes)
        inst = fn()
        if signal:
            e.sem.count += 1
            inst.then_inc(e.sem.h, 1)
            tok = Tok(e.sem, e.sem.count)
        else:
            tok = Tok(e.sem, e.sem.count + 1)
        self._upd(tok, reads, writes)
        return inst

    def dma(self, q, dsem, out_ap, in_ap, reads=(), writes=(), group=None, **kw):
        self._deps(q, reads, writes)
        if group is None or group.val == 0:
            q.wait(dsem, dsem.count)
        inst = q.eng.dma_start(out=out_ap, in_=in_ap, **kw)
        dsem.count += 16
        inst.then_inc(dsem.h, 16)
        if group is not None:
            group.sem = dsem
            group.val = dsem.count
            tok = group
        else:
            tok = Tok(dsem, dsem.count)
        self._upd(tok, reads, writes)
        return inst

    def barrier(self):
        self.nsem = 0
        for e in self.engs:
            for o in self.engs[:4]:
                if o is not e and o.sem.count > 0:
                    e.wait(o.sem, o.sem.count)
            for d in self.dsems + self.fixed:
                if d.count > 0:
                    e.wait(d, d.count)


class Ring:
    def __init__(self, bufs):
        self.bufs = bufs
        self.i = 0

    def next(self):
        b = self.bufs[self.i % len(self.bufs)]
        self.i += 1
        return b


class Prog:
    def __init__(self, n_layers=DEPTH):
        self.n_layers = n_layers
        nc = bass.Bass("TRN2", target_bir_lowering=False)
        self.nc = nc
        self.k = K(nc)
        self.build()

    def sb(self, st, name, shape, dt):
        self.uid = getattr(self, "uid", 0) + 1
        t = st.enter_context(self.nc.sbuf_tensor("s%d_%s" % (self.uid, name), list(shape), dt))
        return Buf(t[:])

    def din(self, name, shape, dt=F32):
        return self.nc.dram_tensor(name, list(shape), dt, kind="ExternalInput").ap()

    def dscr(self, name, shape, dt):
        import os as _os
        if _os.environ.get("KDEBUG_OUT") and name in ("uT", "cs_d", "oT", "hT"):
            return Buf(self.nc.dram_tensor(name, list(shape), dt, kind="ExternalOutput").ap())
        return Buf(self.nc.dram_tensor(name, list(shape), dt).ap())

    def build(self):
        nc, k = self.nc, self.k
        self.x = self.din("x", [S, D])
        self.out = nc.dram_tensor("out", [S, D], F32, kind="ExternalOutput").ap()
        self.p_small = self.din("small", [128, 32 + 32 + 8 + 3 + 2])
        self.p_lam = self.din("lam", [128, 512])
        self.p_subln = self.din("subln", [128, 256])
        self.wspec = {}
        wl = []
        for l in range(DEPTH):
            m = l % 3
            if m == 0:
                wl.append([("wqkv%d" % l, D, 3072), ("wo%d" % l, D, D)])
            elif m == 1:
                wl.append([("win%d" % l, D, 1736), ("wo%d" % l, D, D)])
            else:
                wl.append([("wdown%d" % l, D, 704), ("wuq%d" % l, 384, 1536), ("wukv%d" % l, 256, 2048), ("wo%d" % l, D, D)])
            wl[-1] += [("w1_%d" % l, D, DFF), ("w3_%d" % l, D, DFF), ("w2_%d" % l, DFF, D)]
        self.wf = {}
        self.wb = {}
        for l in range(DEPTH):
            for (n, kk, nn) in wl[l]:
                self.wf[n] = self.din(n, [kk, nn])
                self.wb[n] = self.dscr(n + "_b", [kk, nn], BF16)
        self.wl = wl
        self.hT = [self.dscr("hT", [D, S], F32)]
        self.uT = self.dscr("uT", [D, S], BF16)
        self.oT = self.dscr("oT", [D, S], BF16)
        self.cs_d = self.dscr("cs_d", [128, 2, S], F32)

        with ExitStack() as st0:
            self.setup_persistent(st0)
            self.cast_weights()
            with ExitStack() as st:
                self.rope_tables(st)
            k.barrier()
            with ExitStack() as st:
                self.post_phase(st, -1)
            for l in range(self.n_layers):
                k.barrier()
                m = l % 3
                import os as _os
                with ExitStack() as st:
                    if _os.environ.get("KSKIP_ATTN"):
                        z = self.sb(st, "zz", [128, S], BF16)
                        k.op(k.dve, lambda: nc.vector.memset(z.ap, 0.0), writes=[z])
                        for i in range(8):
                            k.dma(k.sp, k.dsem(), self.oT.ap[i * 128:(i + 1) * 128, :], z.ap, reads=[z], writes=[self.oT])
                    elif m == 0:
                        self.diff_phase(st, l)
                    elif m == 1:
                        self.dsa_phase(st, l)
                    else:
                        self.mla_phase(st, l)
                k.barrier()
                with ExitStack() as st:
                    if not _os.environ.get("KSKIP_POST"):
                        self.post_phase(st, l)
            k.barrier()

    def setup_persistent(self, st):
        nc, k = self.nc, self.k
        self.ps = [Buf(nc.alloc_psum_tensor("ps%d" % i, [128, 512], F32)[:], excl=True) for i in range(8)]
        self.ident_f = self.sb(st, "ident_f", [128, 128], F32)
        self.ident_b = self.sb(st, "ident_b", [128, 128], BF16)
        self.ones_f = self.sb(st, "ones_f", [128, 128], F32)
        self.tri = self.sb(st, "tri", [128, 128], BF16)
        self.rperm = self.sb(st, "rperm", [128, 128], BF16)
        self.small = self.sb(st, "small", [128, 77], F32)
        self.lam = self.sb(st, "lamsb", [128, 512], F32)
        self.subln = self.sb(st, "sublnsb", [128, 256], F32)
        self.neglam = self.sb(st, "neglam", [128, 2], F32)
        self.epsb = self.sb(st, "epsb", [128, 1], F32)
        k.op(k.dve, lambda: nc.vector.memset(self.epsb.ap, EPS), writes=[self.epsb])
        k.dma(k.sp, k.dsem(), self.small.ap, self.p_small, writes=[self.small])
        k.dma(k.sp, k.dsem(), self.lam.ap, self.p_lam, writes=[self.lam])
        k.dma(k.sp, k.dsem(), self.subln.ap, self.p_subln, writes=[self.subln])
        with ExitStack() as t:
            io = self.sb(t, "io", [128, 128], I32)
            dif = self.sb(t, "dif", [128, 128], F32)
            ta = self.sb(t, "ta", [128, 128], F32)
            tb = self.sb(t, "tb", [128, 128], F32)
            co = self.sb(t, "co", [128, 128], F32)
            ioj = self.sb(t, "ioj", [128, 128], I32)
            k.op(k.pool, lambda: nc.gpsimd.iota(io.ap, pattern=[[1, 128]], base=0, channel_multiplier=-1), writes=[io])
            k.op(k.dve, lambda: nc.vector.tensor_copy(out=dif.ap, in_=io.ap), reads=[io], writes=[dif])
            k.op(k.dve, lambda: nc.vector.tensor_single_scalar(out=self.ident_f.ap, in_=dif.ap, scalar=0.0, op=ALU.is_equal), reads=[dif], writes=[self.ident_f])
            k.op(k.dve, lambda: nc.vector.tensor_copy(out=self.ident_b.ap, in_=self.ident_f.ap), reads=[self.ident_f], writes=[self.ident_b])
            k.op(k.dve, lambda: nc.vector.memset(self.ones_f.ap, 1.0), writes=[self.ones_f])
            k.op(k.dve, lambda: nc.vector.tensor_single_scalar(out=self.tri.ap, in_=dif.ap, scalar=0.0, op=ALU.is_ge), reads=[dif], writes=[self.tri])
            k.op(k.pool, lambda: nc.gpsimd.iota(ioj.ap, pattern=[[1, 128]], base=0, channel_multiplier=0), writes=[ioj])
            k.op(k.dve, lambda: nc.vector.tensor_single_scalar(out=ioj.ap, in_=ioj.ap, scalar=32, op=ALU.bitwise_and), reads=[ioj], writes=[ioj])
            k.op(k.dve, lambda: nc.vector.tensor_copy(out=co.ap, in_=ioj.ap), reads=[ioj], writes=[co])
            k.op(k.dve, lambda: nc.vector.tensor_single_scalar(out=co.ap, in_=co.ap, scalar=16.0, op=ALU.is_ge), reads=[co], writes=[co])
            k.op(k.dve, lambda: nc.vector.tensor_single_scalar(out=ta.ap, in_=dif.ap, scalar=32.0, op=ALU.is_equal), reads=[dif], writes=[ta])
            k.op(k.dve, lambda: nc.vector.tensor_single_scalar(out=tb.ap, in_=dif.ap, scalar=-32.0, op=ALU.is_equal), reads=[dif], writes=[tb])
            k.op(k.dve, lambda: nc.vector.tensor_tensor(out=ta.ap, in0=ta.ap, in1=co.ap, op=ALU.mult), reads=[ta, co], writes=[ta])
            k.op(k.dve, lambda: nc.vector.tensor_scalar(out=co.ap, in0=co.ap, scalar1=-1.0, scalar2=1.0, op0=ALU.mult, op1=ALU.add), reads=[co], writes=[co])
            k.op(k.dve, lambda: nc.vector.tensor_tensor(out=tb.ap, in0=tb.ap, in1=co.ap, op=ALU.mult), reads=[tb, co], writes=[tb])
            k.op(k.dve, lambda: nc.vector.tensor_tensor(out=self.rperm.ap, in0=ta.ap, in1=tb.ap, op=ALU.subtract), reads=[ta, tb], writes=[self.rperm])
            pr = self.sb(t, "lampr", [128, 256], F32)
            sm = self.sb(t, "lamsm", [128, 4], F32)
            for j in range(2):
                layer = 3 * j
                li = 0.8 - 0.6 * math.exp(-0.3 * layer)
                lm = self.lam.ap[:, j * 256:(j + 1) * 256]
                k.op(k.dve, lambda: nc.vector.tensor_tensor(out=pr.ap[:, 0:64], in0=lm[:, 0:64], in1=lm[:, 64:128], op=ALU.mult), reads=[self.lam], writes=[pr])
                k.op(k.dve, lambda: nc.vector.tensor_tensor(out=pr.ap[:, 64:128], in0=lm[:, 128:192], in1=lm[:, 192:256], op=ALU.mult), reads=[self.lam], writes=[pr])
                k.op(k.dve, lambda: nc.vector.tensor_reduce(out=sm.ap[:, 0:2], in_=pr.ap[:, 0:128].rearrange("p (a b) -> p a b", a=2), axis=AX.X, op=ALU.add), reads=[pr], writes=[sm])
                k.op(k.act, lambda: nc.scalar.activation(out=sm.ap[:, 2:4], in_=sm.ap[:, 0:2], func=AF.Exp), reads=[sm], writes=[sm])
                k.op(k.dve, lambda: nc.vector.tensor_tensor(out=sm.ap[:, 0:1], in0=sm.ap[:, 3:4], in1=sm.ap[:, 2:3], op=ALU.subtract), reads=[sm], writes=[sm])
                k.op(k.dve, lambda: nc.vector.tensor_scalar(out=self.neglam.ap[:, j:j + 1], in0=sm.ap[:, 0:1], scalar1=-li, scalar2=None, op0=ALU.add), reads=[sm], writes=[self.neglam])
            for j in range(2):
                li = 0.8 - 0.6 * math.exp(-0.3 * 3 * j)
                sl = self.subln.ap[:, j * 128:(j + 1) * 128]
                k.op(k.dve, lambda: nc.vector.tensor_scalar(out=sl, in0=sl, scalar1=1.0 - li, scalar2=None, op0=ALU.mult), reads=[self.subln], writes=[self.subln])
            k.barrier()

    def g_attn(self, l, dc):
        return self.small.ap[:, l * 8 + dc:l * 8 + dc + 1]

    def g_ffn(self, l, dc):
        return self.small.ap[:, 32 + l * 8 + dc:32 + l * 8 + dc + 1]

    def g_final(self, dc):
        return self.small.ap[:, 64 + dc:64 + dc + 1]

    def cast_weights(self):
        k = self.k
        for l in range(DEPTH):
            ds = Sem(self.nc, "cast%d" % l)
            k.fixed.append(ds)
            g = Tok()
            for (n, kk, nn) in self.wl[l]:
                src, dst = self.wf[n], self.wb[n]
                nsplit = 1 if nn <= 2048 else 2
                w = nn // nsplit
                for i in range(nsplit):
                    k.dma(k.pool, ds, dst.ap[:, i * w:(i + 1) * w], src[:, i * w:(i + 1) * w], writes=[dst], group=g)

    def rope_tables(self, st):
        nc, k = self.nc, self.k
        pi = self.sb(st, "r_pi", [128, 1], I32)
        pf = self.sb(st, "r_pf", [128, 1], F32)
        inv = self.sb(st, "r_inv", [128, 1], F32)
        k.op(k.pool, lambda: nc.gpsimd.iota(pi.ap, pattern=[[0, 1]], base=0, channel_multiplier=1), writes=[pi])
        k.op(k.dve, lambda: nc.vector.tensor_single_scalar(out=pi.ap, in_=pi.ap, scalar=31, op=ALU.bitwise_and), reads=[pi], writes=[pi])
        k.op(k.dve, lambda: nc.vector.tensor_copy(out=pf.ap, in_=pi.ap), reads=[pi], writes=[pf])
        k.op(k.act, lambda: nc.scalar.activation(out=inv.ap, in_=pf.ap, func=AF.Exp, scale=-math.log(10000.0) / 32.0), reads=[pf], writes=[inv])
        k.op(k.dve, lambda: nc.vector.tensor_scalar(out=inv.ap, in0=inv.ap, scalar1=1.0 / (2.0 * math.pi), scalar2=None, op0=ALU.mult), reads=[inv], writes=[inv])
        W = 1024
        ti = self.sb(st, "r_ti", [128, W], I32)
        tt = self.sb(st, "r_tt", [128, W], F32)
        ff = self.sb(st, "r_ff", [128, W], F32)
        ni = self.sb(st, "r_ni", [128, W], I32)
        nf = self.sb(st, "r_nf", [128, W], F32)
        g = self.sb(st, "r_g", [128, W], F32)
        res = [self.sb(st, "r_res%d" % i, [128, 2, W], F32) for i in range(2)]
        dss = [k.dsem(), k.dsem()]
        for c in range(S // W):
            rb = res[c % 2]
            k.op(k.pool, lambda: nc.gpsimd.iota(ti.ap, pattern=[[1, W]], base=c * W, channel_multiplier=0), writes=[ti])
            k.op(k.dve, lambda: nc.vector.tensor_copy(out=tt.ap, in_=ti.ap), reads=[ti], writes=[tt])
            k.op(k.dve, lambda: nc.vector.tensor_scalar(out=tt.ap, in0=tt.ap, scalar1=inv.ap, scalar2=None, op0=ALU.mult), reads=[tt, inv], writes=[tt])
            for which in range(2):
                off = 0.25 if which == 0 else 0.0
                k.op(k.dve, lambda: nc.vector.tensor_scalar(out=ff.ap, in0=tt.ap, scalar1=off, scalar2=None, op0=ALU.add), reads=[tt], writes=[ff])
                k.op(k.dve, lambda: nc.vector.tensor_copy(out=ni.ap, in_=ff.ap), reads=[ff], writes=[ni])
                k.op(k.dve, lambda: nc.vector.tensor_copy(out=nf.ap, in_=ni.ap), reads=[ni], writes=[nf])
                k.op(k.dve, lambda: nc.vector.tensor_tensor(out=ff.ap, in0=ff.ap, in1=nf.ap, op=ALU.subtract), reads=[ff, nf], writes=[ff])
                k.op(k.dve, lambda: nc.vector.tensor_single_scalar(out=g.ap, in_=ff.ap, scalar=0.5, op=ALU.is_gt), reads=[ff], writes=[g])
                k.op(k.dve, lambda: nc.vector.tensor_tensor(out=ff.ap, in0=ff.ap, in1=g.ap, op=ALU.subtract), reads=[ff, g], writes=[ff])
                k.op(k.dve, lambda: nc.vector.tensor_single_scalar(out=g.ap, in_=ff.ap, scalar=-0.5, op=ALU.is_lt), reads=[ff], writes=[g])
                k.op(k.dve, lambda: nc.vector.tensor_tensor(out=ff.ap, in0=ff.ap, in1=g.ap, op=ALU.add), reads=[ff, g], writes=[ff])
                k.op(k.act, lambda: nc.scalar.activation(out=rb.ap[:, which, :], in_=ff.ap, func=AF.Sin, scale=2.0 * math.pi * (1.0 - 1e-6)), reads=[ff], writes=[rb])
            k.dma(k.sp, dss[c % 2], self.cs_d.ap[:, :, c * W:(c + 1) * W], rb.ap, reads=[rb], writes=[self.cs_d])

    def rmsnorm_fm(self, src, nchunk, width, gain_fn, dst, dst_ap_fn, sqr, ssps, lnb, rstd, dim, src_ap_fn=None, src_bufs=None):
        nc, k = self.nc, self.k
        sbufs = src_bufs if src_bufs is not None else [src]
        for dc in range(nchunk):
            sq = sqr.next()
            sap = src_ap_fn(dc)
            k.op(k.act, lambda: nc.scalar.activation(out=sq.ap[:, :width], in_=sap, func=AF.Square), reads=sbufs, writes=[sq])
            k.op(k.pe, lambda: nc.tensor.matmul(ssps.ap[:, :width], lhsT=self.ones_f.ap, rhs=sq.ap[:, :width], start=(dc == 0), stop=(dc == nchunk - 1)),
                 reads=[sq, self.ones_f], writes=[ssps])
        k.op(k.act, lambda: nc.scalar.activation(out=lnb.ap[:, :width], in_=ssps.ap[:, :width], func=AF.Ln, scale=1.0 / dim, bias=self.epsb.ap), reads=[ssps, self.epsb], writes=[lnb])
        k.op(k.act, lambda: nc.scalar.activation(out=rstd.ap[:, :width], in_=lnb.ap[:, :width], func=AF.Exp, scale=-0.5), reads=[lnb], writes=[rstd])
        for dc in range(nchunk):
            sap = src_ap_fn(dc)
            dap = dst_ap_fn(dc)
            k.op(k.dve, lambda: nc.vector.scalar_tensor_tensor(out=dap, in0=sap, scalar=gain_fn(dc), in1=rstd.ap[:, :width], op0=ALU.mult, op1=ALU.mult),
                 reads=sbufs + [rstd, self.small], writes=[dst])

    def eps_ap(self):
        return self.epsb.ap

    def post_phase(self, st, l):
        nc, k = self.nc, self.k
        first = (l < 0)
        last = (l == self.n_layers - 1)
        hcs = Ring([self.sb(st, "hc%d" % i, [128, 8, TC], F32) for i in range(2)])
        h_ds = [k.dsem(), k.dsem()]
        hst_ds = [k.dsem(), k.dsem()]
        sqr = Ring([self.sb(st, "sq%d" % i, [128, TC], F32) for i in range(2)])
        lnb = self.sb(st, "lnb", [128, TC], F32)
        rstd = self.sb(st, "rstd", [128, TC], F32)
        if not last:
            uns = Ring([self.sb(st, "un%d" % i, [128, 8, TC], BF16) for i in range(1)])
            un_ds = [k.dsem(), k.dsem()]
        ssps = self.ps[7]
        hT_v = self.hT[0].ap.rearrange("(c p) t -> p c t", p=128)
        uT_v = self.uT.ap.rearrange("(c p) t -> p c t", p=128)
        oT_v = self.oT.ap.rearrange("(c p) t -> p c t", p=128)
        if first:
            xts = Ring([self.sb(st, "xt%d" % i, [128, D], F32) for i in range(8)])
            x_ds = [k.dsem() for _ in range(8)]
            psr = Ring(self.ps[0:6])
        else:
            ocs = Ring([self.sb(st, "oc%d" % i, [128, 8, TC], BF16) for i in range(2)])
            o_ds = [k.dsem(), k.dsem()]
            ucs = Ring([self.sb(st, "uc%d" % i, [128, 8, TC], BF16) for i in range(2)])
            gT = self.sb(st, "gT", [128, 22, TC], BF16)
            sil = Ring([self.sb(st, "sil%d" % i, [128, TC], F32) for i in range(2)])
            wo = self.sb(st, "wo", [128, 8, D], BF16)
            w2 = self.sb(st, "w2", [128, 22, D], BF16)
            w13 = Ring([self.sb(st, "w13_%d" % i, [128, 2, 8, 512], BF16) for i in range(2)])
            w_ds = [k.dsem(), k.dsem()]
            wres_ds = k.dsem()
            psr = Ring(self.ps[0:6])
            wob, w1b, w3b, w2b = self.wb["wo%d" % l], self.wb["w1_%d" % l], self.wb["w3_%d" % l], self.wb["w2_%d" % l]
            k.dma(k.sp, wres_ds, wo.ap, wob.ap.rearrange("(c p) f -> p c f", p=128), reads=[wob], writes=[wo])
            k.dma(k.sp, k.dsem(), w2.ap, w2b.ap.rearrange("(c p) f -> p c f", p=128), reads=[w2b], writes=[w2])
            w1v = w1b.ap.rearrange("(c p) f -> p c f", p=128)
            w3v = w3b.ap.rearrange("(c p) f -> p c f", p=128)
            fgs = [(i * 512, 512) for i in range(5)] + [(2560, 256)]
        if last:
            orow = Ring([self.sb(st, "orow%d" % i, [128, D], F32) for i in range(2)])
            or_ds = [k.dsem(), k.dsem()]

        def load_chunk(c):
            hc = hcs.bufs[c % 2]
            cols = slice(c * TC, (c + 1) * TC)
            if not first:
                k.dma(k.sp, h_ds[c % 2], hc.ap, hT_v[:, :, cols], reads=[self.hT[0]], writes=[hc])
                oc = ocs.bufs[c % 2]
                k.dma(k.sp, o_ds[c % 2], oc.ap, oT_v[:, :, cols], reads=[self.oT], writes=[oc])

        if not first:
            load_chunk(0)
        for c in range(NCH):
            cols = slice(c * TC, (c + 1) * TC)
            hc = hcs.bufs[c % 2]
            if first:
                xtl = []
                for i in range(4):
                    xt = xts.next()
                    k.dma(k.sp, x_ds[(xts.i - 1) % 8], xt.ap, self.x[c * TC + i * 128:c * TC + (i + 1) * 128, :], writes=[xt])
                    xtl.append(xt)
                for dc in range(8):
                    pb = psr.next()
                    for i in range(4):
                        k.op(k.pe, lambda: nc.tensor.transpose(pb.ap[:, i * 128:(i + 1) * 128], xtl[i].ap[:, dc * 128:(dc + 1) * 128], self.ident_f.ap),
                             reads=[xtl[i], self.ident_f], writes=[pb], signal=(i == 3))
                    e = k.act if dc % 2 == 0 else k.dve
                    if e is k.act:
                        k.op(e, lambda: nc.scalar.copy(out=hc.ap[:, dc, :], in_=pb.ap), reads=[pb], writes=[hc])
                    else:
                        k.op(e, lambda: nc.vector.tensor_copy(out=hc.ap[:, dc, :], in_=pb.ap), reads=[pb], writes=[hc])
            else:
                if c + 1 < NCH:
                    load_chunk(c + 1)
                oc = ocs.bufs[c % 2]
                for dc in range(8):
                    pb = psr.next()
                    for cc in range(8):
                        k.op(k.pe, lambda: nc.tensor.matmul(pb.ap, lhsT=wo.ap[:, cc, dc * 128:(dc + 1) * 128], rhs=oc.ap[:, cc, :], start=(cc == 0), stop=(cc == 7)),
                             reads=[wo, oc], writes=[pb], signal=(cc == 7))
                    k.op(k.dve, lambda: nc.vector.tensor_tensor(out=hc.ap[:, dc, :], in0=pb.ap, in1=hc.ap[:, dc, :], op=ALU.add), reads=[pb, hc], writes=[hc])
                uc = ucs.next()
                self.rmsnorm_fm(hc, 8, TC, lambda dc: self.g_ffn(l, dc), uc, lambda dc: uc.ap[:, dc, :], sqr, ssps, lnb, rstd, float(D),
                                src_ap_fn=lambda dc: hc.ap[:, dc, :])
                def load_w13(gi):
                    f0, fw = fgs[gi]
                    wt = w13.bufs[gi % 2]
                    g = Tok()
                    k.dma(k.sp, w_ds[gi % 2], wt.ap[:, 0, :, :fw], w1v[:, :, f0:f0 + fw], reads=[w1b], writes=[wt], group=g)
                    k.dma(k.sp, w_ds[gi % 2], wt.ap[:, 1, :, :fw], w3v[:, :, f0:f0 + fw], reads=[w3b], writes=[wt], group=g)
                load_w13(0)
                for gi, (f0, fw) in enumerate(fgs):
                    if gi + 1 < len(fgs):
                        load_w13(gi + 1)
                    wt = w13.bufs[gi % 2]
                    for fi in range(fw // 128):
                        fc = f0 // 128 + fi
                        p1 = psr.next()
                        p3 = psr.next()
                        for dc in range(8):
                            k.op(k.pe, lambda: nc.tensor.matmul(p1.ap, lhsT=wt.ap[:, 0, dc, fi * 128:(fi + 1) * 128], rhs=uc.ap[:, dc, :], start=(dc == 0), stop=(dc == 7)),
                                 reads=[wt, uc], writes=[p1], signal=(dc == 7))
                        for dc in range(8):
                            k.op(k.pe, lambda: nc.tensor.matmul(p3.ap, lhsT=wt.ap[:, 1, dc, fi * 128:(fi + 1) * 128], rhs=uc.ap[:, dc, :], start=(dc == 0), stop=(dc == 7)),
                                 reads=[wt, uc], writes=[p3], signal=(dc == 7))
                        sb_ = sil.next()
                        k.op(k.act, lambda: nc.scalar.activation(out=sb_.ap, in_=p1.ap, func=AF.Silu), reads=[p1], writes=[sb_])
                        k.op(k.dve, lambda: nc.vector.tensor_tensor(out=gT.ap[:, fc, :], in0=sb_.ap, in1=p3.ap, op=ALU.mult), reads=[sb_, p3], writes=[gT])
                for dc in range(8):
                    pb = psr.next()
                    for fc in range(22):
                        k.op(k.pe, lambda: nc.tensor.matmul(pb.ap, lhsT=w2.ap[:, fc, dc * 128:(dc + 1) * 128], rhs=gT.ap[:, fc, :], start=(fc == 0), stop=(fc == 21)),
                             reads=[w2, gT], writes=[pb], signal=(fc == 21))
                    k.op(k.dve, lambda: nc.vector.tensor_tensor(out=hc.ap[:, dc, :], in0=pb.ap, in1=hc.ap[:, dc, :], op=ALU.add), reads=[pb, hc], writes=[hc])
            if not last:
                k.dma(k.sp, hst_ds[c % 2], hT_v[:, :, cols], hc.ap, reads=[hc], writes=[self.hT[0]])
                un = uns.next()
                self.rmsnorm_fm(hc, 8, TC, lambda dc: self.g_attn(l + 1, dc), un, lambda dc: un.ap[:, dc, :], sqr, ssps, lnb, rstd, float(D),
                                src_ap_fn=lambda dc: hc.ap[:, dc, :])
                k.dma(k.sp, un_ds[c % 2], uT_v[:, :, cols], un.ap, reads=[un], writes=[self.uT])
            else:
                for dc in range(8):
                    sq = sqr.next()
                    k.op(k.act, lambda: nc.scalar.activation(out=sq.ap, in_=hc.ap[:, dc, :], func=AF.Square), reads=[hc], writes=[sq])
                    k.op(k.pe, lambda: nc.tensor.matmul(ssps.ap, lhsT=self.ones_f.ap, rhs=sq.ap, start=(dc == 0), stop=(dc == 7)),
                         reads=[sq, self.ones_f], writes=[ssps])
                k.op(k.act, lambda: nc.scalar.activation(out=lnb.ap, in_=ssps.ap, func=AF.Ln, scale=1.0 / D, bias=self.epsb.ap), reads=[ssps, self.epsb], writes=[lnb])
                k.op(k.act, lambda: nc.scalar.activation(out=rstd.ap, in_=lnb.ap, func=AF.Exp, scale=-0.5), reads=[lnb], writes=[rstd])
                for dc in range(8):
                    k.op(k.dve, lambda: nc.vector.scalar_tensor_tensor(out=hc.ap[:, dc, :], in0=hc.ap[:, dc, :], scalar=self.g_final(dc), in1=rstd.ap, op0=ALU.mult, op1=ALU.mult),
                         reads=[hc, rstd, self.small], writes=[hc])
                for i in range(4):
                    ob = orow.next()
                    for half in range(2):
                        pb = psr.next()
                        for j in range(4):
                            dc = half * 4 + j
                            k.op(k.pe, lambda: nc.tensor.transpose(pb.ap[:, j * 128:(j + 1) * 128], hc.ap[:, dc, i * 128:(i + 1) * 128], self.ident_f.ap),
                                 reads=[hc, self.ident_f], writes=[pb], signal=(j == 3))
                        if half == 0:
                            k.op(k.act, lambda: nc.scalar.copy(out=ob.ap[:, 0:512], in_=pb.ap), reads=[pb], writes=[ob])
                        else:
                            k.op(k.dve, lambda: nc.vector.tensor_copy(out=ob.ap[:, 512:1024], in_=pb.ap), reads=[pb], writes=[ob])
                    k.dma(k.sp, or_ds[(orow.i - 1) % 2], self.out[c * TC + i * 128:c * TC + (i + 1) * 128, :], ob.ap, reads=[ob])

    def attn_core(self, c, lhs_fn, rhs_fn, rbufs, v_fn, vbuf, E, scale, st_ring, pt_ring, acc, tri=True, mask_fn=None, maskbuf=None, mask_eng=None):
        nc, k = self.nc, self.k
        nk = 4 * c + 4

        def emit_st(j):
            r = j - 4 * c
            off = 128 * max(r, 0)
            n = 512 - off
            stb = st_ring.next()
            ls = lhs_fn(j)
            rs = rhs_fn(off, n)
            nl_ = len(ls)
            for i in range(nl_):
                k.op(k.pe, lambda: nc.tensor.matmul(stb.ap[:, off:512], lhsT=ls[i], rhs=rs[i], start=(i == 0), stop=(i == nl_ - 1)),
                     reads=rbufs, writes=[stb], signal=(i == nl_ - 1))
            return stb, r, off, n

        nxt = emit_st(0)
        for j in range(nk):
            stb, r, off, n = nxt
            if j + 1 < nk:
                nxt = emit_st(j + 1)
            pt = pt_ring.next()
            k.op(k.act, lambda: nc.scalar.activation(out=pt.ap[:, off:512], in_=stb.ap[:, off:512], func=AF.Exp, scale=scale), reads=[stb], writes=[pt])
            if r >= 0 and tri:
                k.op(k.pool, lambda: nc.gpsimd.tensor_tensor(out=pt.ap[:, off:off + 128], in0=pt.ap[:, off:off + 128], in1=self.tri.ap, op=ALU.mult),
                     reads=[pt, self.tri], writes=[pt])
            if mask_fn is not None:
                me = mask_eng(j)
                mm = mask_fn(j, off, n)
                k.op(me, lambda: me.eng.tensor_tensor(out=pt.ap[:, off:512], in0=pt.ap[:, off:512], in1=mm, op=ALU.mult), reads=[pt, maskbuf], writes=[pt])
            for qt in range(max(r, 0), 4):
                k.op(k.pe, lambda: nc.tensor.matmul(acc[qt].ap[:, 0:E + 1], lhsT=pt.ap[:, qt * 128:(qt + 1) * 128], rhs=v_fn(j), start=(j == 0), stop=(j == 4 * c + qt)),
                     reads=[pt, vbuf], writes=[acc[qt]], signal=(qt == 3))

    def rope_fm(self, pa, np_, cs, cols, dst, dst_ap, xbr, rar, rbr, pb):
        nc, k = self.nc, self.k
        import os as _os
        lvl = _os.environ.get("KROPE", "z")
        xb = xbr.next()
        ra = rar.next()
        rb = rbr.next()
        k.op(k.act, lambda: nc.scalar.copy(out=xb.ap[0:np_, :], in_=pa.ap[0:np_, :]), reads=[pa], writes=[xb])
        if lvl >= "b":
            k.op(k.pe, lambda: nc.tensor.matmul(pb.ap[0:np_, :], lhsT=self.rperm.ap[0:np_, 0:np_], rhs=xb.ap[0:np_, :], start=True, stop=True), reads=[xb, self.rperm], writes=[pb])
        if lvl >= "c":
            k.op(k.dve, lambda: nc.vector.tensor_tensor(out=ra.ap[0:np_, :], in0=pa.ap[0:np_, :], in1=cs.ap[0:np_, 0, cols], op=ALU.mult), reads=[pa, cs], writes=[ra])
        if lvl >= "d":
            k.op(k.dve, lambda: nc.vector.tensor_tensor(out=rb.ap[0:np_, :], in0=pb.ap[0:np_, :], in1=cs.ap[0:np_, 1, cols], op=ALU.mult), reads=[pb, cs], writes=[rb])
        if lvl >= "e":
            k.op(k.pool, lambda: nc.gpsimd.tensor_tensor(out=dst_ap, in0=ra.ap[0:np_, :], in1=rb.ap[0:np_, :], op=ALU.add), reads=[ra, rb], writes=[dst])
        else:
            k.op(k.act, lambda: nc.scalar.copy(out=dst_ap, in_=pa.ap[0:np_, :]), reads=[pa], writes=[dst])

    def load_full(self, st, name, dbuf, shape_inner, dt, nsplit=8):
        k = self.k
        t = self.sb(st, name, [128] + shape_inner, dt)
        v = dbuf.ap.rearrange("(c p) t -> p c t", p=128)
        g = Tok()
        ds = k.dsem()
        w = S // nsplit
        for i in range(nsplit):
            k.dma(k.sp, ds, t.ap[:, :, i * w:(i + 1) * w], v[:, :, i * w:(i + 1) * w], reads=[dbuf], writes=[t], group=g)
        return t

    def evac_norm(self, acc, E, dst, dst_ap_fn, recr):
        nc, k = self.nc, self.k
        for qt in range(4):
            rec = recr.next()
            k.op(k.dve, lambda: nc.vector.reciprocal(out=rec.ap, in_=acc[qt].ap[:, E:E + 1]), reads=[acc[qt]], writes=[rec])
            k.op(k.dve, lambda: nc.vector.tensor_scalar(out=dst_ap_fn(qt), in0=acc[qt].ap[:, 0:E], scalar1=rec.ap, scalar2=None, op0=ALU.mult), reads=[acc[qt], rec], writes=[dst])

    def transpose_out(self, src, src_ap_fn, pbuf, dst, dst_ap, eng):
        nc, k = self.nc, self.k
        for qt in range(4):
            k.op(k.pe, lambda: nc.tensor.transpose(pbuf.ap[:, qt * 128:(qt + 1) * 128], src_ap_fn(qt), self.ident_f.ap), reads=[src, self.ident_f], writes=[pbuf], signal=(qt == 3))
        if eng is k.act:
            k.op(eng, lambda: nc.scalar.copy(out=dst_ap, in_=pbuf.ap), reads=[pbuf], writes=[dst])
        else:
            k.op(eng, lambda: nc.vector.tensor_copy(out=dst_ap, in_=pbuf.ap), reads=[pbuf], writes=[dst])

    def diff_phase(self, st, l):
        nc, k = self.nc, self.k
        jj = l // 3
        uT = self.load_full(st, "uTf", self.uT, [8, S], BF16)
        cs = self.sb(st, "cs", [128, 2, S], F32)
        k.dma(k.sp, k.dsem(), cs.ap, self.cs_d.ap, reads=[self.cs_d], writes=[cs])
        wqkv = self.wb["wqkv%d" % l]
        wv = wqkv.ap.rearrange("(c p) f -> p c f", p=128)
        whs = Ring([self.sb(st, "wh%d" % i, [128, 8, 384], BF16) for i in range(2)])
        wh_ds = [k.dsem(), k.dsem()]
        qTs = Ring([self.sb(st, "qT%d" % i, [128, S], BF16) for i in range(2)])
        kTs = Ring([self.sb(st, "kT%d" % i, [128, S], BF16) for i in range(2)])
        Vs = Ring([self.sb(st, "V%d" % i, [128, 32, 130], BF16) for i in range(2)])
        for vb in Vs.bufs:
            k.op(k.dve, lambda: nc.vector.memset(vb.ap[:, :, 128:130], 1.0), writes=[vb])
        xbr = Ring([self.sb(st, "xb%d" % i, [128, TC], BF16) for i in range(2)])
        rar = Ring([self.sb(st, "ra%d" % i, [128, TC], F32) for i in range(2)])
        rbr = Ring([self.sb(st, "rb%d" % i, [128, TC], F32) for i in range(2)])
        pts = Ring([self.sb(st, "pt%d" % i, [128, TC], BF16) for i in range(4)])
        recr = Ring([self.sb(st, "rec%d" % i, [128, 1], F32) for i in range(8)])
        om = [self.sb(st, "om%d" % i, [128, 4, 128], F32) for i in range(2)]
        d4 = self.sb(st, "d4", [128, 4, 128], F32)
        sq4 = self.sb(st, "sq4", [128, 4, 128], F32)
        ss4 = self.sb(st, "ss4", [128, 4], F32)
        ln4 = self.sb(st, "ln4", [128, 4], F32)
        rs4 = self.sb(st, "rs4", [128, 4], F32)
        dn = Ring([self.sb(st, "dn%d" % i, [128, 4, 128], F32) for i in range(2)])
        oTh = Ring([self.sb(st, "oTh%d" % i, [128, S], BF16) for i in range(2)])
        oth_ds = [k.dsem(), k.dsem()]
        st_ring = Ring(self.ps[0:2])
        acc = self.ps[2:6]
        misc = Ring(self.ps[6:8])
        sub_ap = self.subln.ap[:, jj * 128:(jj + 1) * 128].unsqueeze(1).broadcast_to([128, 4, 128])
        scale = 64 ** -0.5
        import os as _os
        for h in range(int(_os.environ.get("KHEADS", "8"))):
            wh = whs.next()
            g = Tok()
            for i in range(3):
                k.dma(k.sp, wh_ds[h % 2], wh.ap[:, :, i * 128:(i + 1) * 128], wv[:, :, i * 1024 + h * 128:i * 1024 + (h + 1) * 128], reads=[wqkv], writes=[wh], group=g)
            qT, kT, V = qTs.next(), kTs.next(), Vs.next()
            import os as _os
            stage = float(_os.environ.get("KDIFF_STAGE", "3"))
            for tc in range(NCH):
                if stage < 0.5:
                    break
                if _os.environ.get("KTC1") and tc >= int(_os.environ.get("KTC1")):
                    break
                cols = slice(tc * TC, (tc + 1) * TC)
                for which, dstb in ((0, qT), (1, kT)):
                    if _os.environ.get("KSKIPQK"):
                        break
                    pa = misc.next()
                    for dc in range(8):
                        k.op(k.pe, lambda: nc.tensor.matmul(pa.ap, lhsT=wh.ap[:, dc, which * 128:(which + 1) * 128], rhs=uT.ap[:, dc, cols], start=(dc == 0), stop=(dc == 7)),
                             reads=[wh, uT], writes=[pa], signal=(dc == 7))
                    pb = misc.next()
                    if stage < 0.7:
                        k.op(k.act, lambda: nc.scalar.copy(out=dstb.ap[:, cols], in_=pa.ap), reads=[pa], writes=[dstb])
                        continue
                    self.rope_fm(pa, 128, cs, cols, dstb, dstb.ap[:, cols], xbr, rar, rbr, pb)
                if _os.environ.get("KSKIPV"):
                    continue
                pa = misc.next()
                for i in range(4):
                    for dc in range(8):
                        k.op(k.pe, lambda: nc.tensor.matmul(pa.ap[:, i * 128:(i + 1) * 128], lhsT=uT.ap[:, dc, tc * TC + i * 128:tc * TC + (i + 1) * 128], rhs=wh.ap[:, dc, 256:384], start=(dc == 0), stop=(dc == 7)),
                             reads=[wh, uT], writes=[pa], signal=(dc == 7 and i == 3))
                k.op(k.act, lambda: nc.scalar.copy(out=V.ap[:, tc * 4:tc * 4 + 4, 0:128], in_=pa.ap.rearrange("p (a b) -> p a b", a=4)), reads=[pa], writes=[V])
            oT_h = oTh.next()
            if stage < 3:
                k.op(k.dve, lambda: nc.vector.memset(oT_h.ap, 0.0), writes=[oT_h])
            for c in range(NCH):
                if stage < 2:
                    break
                for m in range(2):
                    self.attn_core(c,
                                   lambda j: [kT.ap[m * 64:(m + 1) * 64, j * 128:(j + 1) * 128]],
                                   lambda off, n: [qT.ap[m * 64:(m + 1) * 64, c * TC + off:c * TC + off + n]],
                                   [kT, qT], lambda j: V.ap[:, j, 0:129], V, 128, scale, st_ring, pts, acc)
                    self.evac_norm(acc, 128, om[m], lambda qt: om[m].ap[:, qt, :], recr)
                if stage < 3:
                    continue
                k.op(k.dve, lambda: nc.vector.scalar_tensor_tensor(out=d4.ap, in0=om[1].ap, scalar=self.neglam.ap[:, jj:jj + 1], in1=om[0].ap, op0=ALU.mult, op1=ALU.add),
                     reads=[om[0], om[1], self.neglam], writes=[d4])
                k.op(k.dve, lambda: nc.vector.tensor_tensor(out=sq4.ap, in0=d4.ap, in1=d4.ap, op=ALU.mult), reads=[d4], writes=[sq4])
                k.op(k.dve, lambda: nc.vector.tensor_reduce(out=ss4.ap, in_=sq4.ap, axis=AX.X, op=ALU.add), reads=[sq4], writes=[ss4])
                k.op(k.act, lambda: nc.scalar.activation(out=ln4.ap, in_=ss4.ap, func=AF.Ln, scale=1.0 / 128.0, bias=self.epsb.ap), reads=[ss4, self.epsb], writes=[ln4])
                k.op(k.act, lambda: nc.scalar.activation(out=rs4.ap, in_=ln4.ap, func=AF.Exp, scale=-0.5), reads=[ln4], writes=[rs4])
                dnb = dn.next()
                sub2 = self.subln.ap[:, jj * 128:(jj + 1) * 128]
                for qt in range(4):
                    k.op(k.dve, lambda: nc.vector.scalar_tensor_tensor(out=dnb.ap[:, qt, :], in0=d4.ap[:, qt, :], scalar=rs4.ap[:, qt:qt + 1], in1=sub2, op0=ALU.mult, op1=ALU.mult),
                         reads=[d4, rs4, self.subln], writes=[dnb])
                self.transpose_out(dnb, lambda qt: dnb.ap[:, qt, :], misc.next(), oT_h, oT_h.ap[:, c * TC:(c + 1) * TC], k.dve)
            k.dma(k.sp, oth_ds[h % 2], self.oT.ap[h * 128:(h + 1) * 128, :], oT_h.ap, reads=[oT_h], writes=[self.oT])

    def mla_phase(self, st, l):
        nc, k = self.nc, self.k
        wd, wuq, wukv = self.wb["wdown%d" % l], self.wb["wuq%d" % l], self.wb["wukv%d" % l]
        cs = self.sb(st, "cs", [128, 2, S], F32)
        k.dma(k.sp, k.dsem(), cs.ap, self.cs_d.ap, reads=[self.cs_d], writes=[cs])
        wdsb = self.sb(st, "wdsb", [128, 8, 704], BF16)
        k.dma(k.sp, k.dsem(), wdsb.ap, wd.ap.rearrange("(c p) f -> p c f", p=128), reads=[wd], writes=[wdsb])
        cqn = self.sb(st, "cqn", [128, 3, S], BF16)
        ckvn = self.sb(st, "ckvn", [128, 2, S], BF16)
        krT = self.sb(st, "krT", [128, S], BF16)
        ucs = Ring([self.sb(st, "uc%d" % i, [128, 8, TC], BF16) for i in range(2)])
        u_ds = [k.dsem(), k.dsem()]
        sqr = Ring([self.sb(st, "sq%d" % i, [128, TC], F32) for i in range(2)])
        lnb = self.sb(st, "lnb", [128, TC], F32)
        rstd = self.sb(st, "rstd", [128, TC], F32)
        xbr = Ring([self.sb(st, "xb%d" % i, [128, TC], BF16) for i in range(2)])
        rar = Ring([self.sb(st, "ra%d" % i, [128, TC], F32) for i in range(2)])
        rbr = Ring([self.sb(st, "rb%d" % i, [128, TC], F32) for i in range(2)])
        uT_v = self.uT.ap.rearrange("(c p) t -> p c t", p=128)
        ps = self.ps
        for tc in range(NCH):
            cols = slice(tc * TC, (tc + 1) * TC)
            uc = ucs.next()
            k.dma(k.sp, u_ds[tc % 2], uc.ap, uT_v[:, :, cols], reads=[self.uT], writes=[uc])
            for i in range(3):
                for dc in range(8):
                    k.op(k.pe, lambda: nc.tensor.matmul(ps[i].ap, lhsT=wdsb.ap[:, dc, i * 128:(i + 1) * 128], rhs=uc.ap[:, dc, :], start=(dc == 0), stop=(dc == 7)),
                         reads=[wdsb, uc], writes=[ps[i]], signal=(dc == 7))
            self.rmsnorm_fm(None, 3, TC, lambda i: self.small.ap[:, 72 + i:73 + i], cqn, lambda i: cqn.ap[:, i, cols], sqr, ps[7], lnb, rstd, 384.0,
                            src_ap_fn=lambda i: ps[i].ap, src_bufs=[ps[0], ps[1], ps[2]])
            for i in range(2):
                for dc in range(8):
                    k.op(k.pe, lambda: nc.tensor.matmul(ps[3 + i].ap, lhsT=wdsb.ap[:, dc, 384 + i * 128:384 + (i + 1) * 128], rhs=uc.ap[:, dc, :], start=(dc == 0), stop=(dc == 7)),
                         reads=[wdsb, uc], writes=[ps[3 + i]], signal=(dc == 7))
            self.rmsnorm_fm(None, 2, TC, lambda i: self.small.ap[:, 75 + i:76 + i], ckvn, lambda i: ckvn.ap[:, i, cols], sqr, ps[7], lnb, rstd, 256.0,
                            src_ap_fn=lambda i: ps[3 + i].ap, src_bufs=[ps[3], ps[4]])
            for dc in range(8):
                k.op(k.pe, lambda: nc.tensor.matmul(ps[5].ap[0:64, :], lhsT=wdsb.ap[:, dc, 640:704], rhs=uc.ap[:, dc, :], start=(dc == 0), stop=(dc == 7)),
                     reads=[wdsb, uc], writes=[ps[5]], signal=(dc == 7))
            self.rope_fm(ps[5], 64, cs, cols, krT, krT.ap[0:64, cols], xbr, rar, rbr, ps[6])
        wqh = Ring([self.sb(st, "wqh%d" % i, [128, 3, 192], BF16) for i in range(2)])
        wkvh = Ring([self.sb(st, "wkvh%d" % i, [128, 2, 256], BF16) for i in range(2)])
        wq_ds = [k.dsem(), k.dsem()]
        wuq_v = wuq.ap.rearrange("(c p) f -> p c f", p=128)
        wukv_v = wukv.ap.rearrange("(c p) f -> p c f", p=128)
        qnT = self.sb(st, "qnT", [128, S], BF16)
        qrT = self.sb(st, "qrT", [128, S], BF16)
        knT = self.sb(st, "knT", [128, S], BF16)
        V = self.sb(st, "V", [128, 32, 130], BF16)
        k.op(k.dve, lambda: nc.vector.memset(V.ap[:, :, 128:130], 1.0), writes=[V])
        pts = Ring([self.sb(st, "pt%d" % i, [128, TC], BF16) for i in range(4)])
        recr = Ring([self.sb(st, "rec%d" % i, [128, 1], F32) for i in range(8)])
        on = Ring([self.sb(st, "on%d" % i, [128, 4, 128], F32) for i in range(2)])
        oTh = Ring([self.sb(st, "oTh%d" % i, [128, S], BF16) for i in range(2)])
        oth_ds = [k.dsem(), k.dsem()]
        st_ring = Ring(ps[0:2])
        acc = ps[2:6]
        misc = Ring(ps[6:8])
        scale = 192 ** -0.5
        for h in range(8):
            wq, wkv = wqh.next(), wkvh.next()
            g = Tok()
            k.dma(k.sp, wq_ds[h % 2], wq.ap, wuq_v[:, :, h * 192:(h + 1) * 192], reads=[wuq], writes=[wq], group=g)
            k.dma(k.sp, wq_ds[h % 2], wkv.ap, wukv_v[:, :, h * 256:(h + 1) * 256], reads=[wukv], writes=[wkv], group=g)
            for tc in range(NCH):
                cols = slice(tc * TC, (tc + 1) * TC)
                pa = misc.next()
                for i in range(3):
                    k.op(k.pe, lambda: nc.tensor.matmul(pa.ap, lhsT=wq.ap[:, i, 0:128], rhs=cqn.ap[:, i, cols], start=(i == 0), stop=(i == 2)), reads=[wq, cqn], writes=[pa], signal=(i == 2))
                k.op(k.act, lambda: nc.scalar.copy(out=qnT.ap[:, cols], in_=pa.ap), reads=[pa], writes=[qnT])
                pa = misc.next()
                for i in range(3):
                    k.op(k.pe, lambda: nc.tensor.matmul(pa.ap[0:64, :], lhsT=wq.ap[:, i, 128:192], rhs=cqn.ap[:, i, cols], start=(i == 0), stop=(i == 2)), reads=[wq, cqn], writes=[pa], signal=(i == 2))
                pb = misc.next()
                self.rope_fm(pa, 64, cs, cols, qrT, qrT.ap[0:64, cols], xbr, rar, rbr, pb)
                pa = misc.next()
                for i in range(2):
                    k.op(k.pe, lambda: nc.tensor.matmul(pa.ap, lhsT=wkv.ap[:, i, 0:128], rhs=ckvn.ap[:, i, cols], start=(i == 0), stop=(i == 1)), reads=[wkv, ckvn], writes=[pa], signal=(i == 1))
                k.op(k.dve, lambda: nc.vector.tensor_copy(out=knT.ap[:, cols], in_=pa.ap), reads=[pa], writes=[knT])
                pa = misc.next()
                for t4 in range(4):
                    for i in range(2):
                        k.op(k.pe, lambda: nc.tensor.matmul(pa.ap[:, t4 * 128:(t4 + 1) * 128], lhsT=ckvn.ap[:, i, tc * TC + t4 * 128:tc * TC + (t4 + 1) * 128], rhs=wkv.ap[:, i, 128:256], start=(i == 0), stop=(i == 1)),
                             reads=[wkv, ckvn], writes=[pa], signal=(i == 1 and t4 == 3))
                k.op(k.act, lambda: nc.scalar.copy(out=V.ap[:, tc * 4:tc * 4 + 4, 0:128], in_=pa.ap.rearrange("p (a b) -> p a b", a=4)), reads=[pa], writes=[V])
            oT_h = oTh.next()
            for c in range(NCH):
                self.attn_core(c,
                               lambda j: [knT.ap[:, j * 128:(j + 1) * 128], krT.ap[0:64, j * 128:(j + 1) * 128]],
                               lambda off, n: [qnT.ap[:, c * TC + off:c * TC + off + n], qrT.ap[0:64, c * TC + off:c * TC + off + n]],
                               [knT, krT, qnT, qrT], lambda j: V.ap[:, j, 0:129], V, 128, scale, st_ring, pts, acc)
                onb = on.next()
                self.evac_norm(acc, 128, onb, lambda qt: onb.ap[:, qt, :], recr)
                self.transpose_out(onb, lambda qt: onb.ap[:, qt, :], misc.next(), oT_h, oT_h.ap[:, c * TC:(c + 1) * TC], k.dve)
            k.dma(k.sp, oth_ds[h % 2], self.oT.ap[h * 128:(h + 1) * 128, :], oT_h.ap, reads=[oT_h], writes=[self.oT])

    def dsa_phase(self, st, l):
        nc, k = self.nc, self.k
        win = self.wb["win%d" % l]
        wv_ = win.ap.rearrange("(c p) f -> p c f", p=128)
        Wq = self.sb(st, "Wq", [128, 8, 1024], BF16)
        Wkk = self.sb(st, "Wkk", [128, 8, 128], BF16)
        Wikk = self.sb(st, "Wikk", [128, 8, 128], BF16)
        Wv = self.sb(st, "Wv", [128, 8, 64], BF16)
        Wiq = self.sb(st, "Wiq", [128, 8, 512], BF16)
        Wiw = self.sb(st, "Wiw", [128, 8, 8], BF16)
        k.dma(k.sp, k.dsem(), Wq.ap, wv_[:, :, 0:1024], reads=[win], writes=[Wq])
        g = Tok(); dsx = k.dsem()
        k.dma(k.sp, dsx, Wkk.ap[:, :, 0:64], wv_[:, :, 1024:1088], reads=[win], writes=[Wkk], group=g)
        k.dma(k.sp, dsx, Wkk.ap[:, :, 64:128], wv_[:, :, 1024:1088], reads=[win], writes=[Wkk], group=g)
        k.dma(k.sp, dsx, Wikk.ap[:, :, 0:64], wv_[:, :, 1664:1728], reads=[win], writes=[Wikk], group=g)
        k.dma(k.sp, dsx, Wikk.ap[:, :, 64:128], wv_[:, :, 1664:1728], reads=[win], writes=[Wikk], group=g)
        k.dma(k.sp, dsx, Wv.ap, wv_[:, :, 1088:1152], reads=[win], writes=[Wv], group=g)
        k.dma(k.sp, dsx, Wiq.ap, wv_[:, :, 1152:1664], reads=[win], writes=[Wiq], group=g)
        k.dma(k.sp, dsx, Wiw.ap, wv_[:, :, 1728:1736], reads=[win], writes=[Wiw], group=g)
        KK = self.sb(st, "KK", [128, S], BF16)
        IKK = self.sb(st, "IKK", [128, S], BF16)
        Vaug = self.sb(st, "Vaug", [128, 32, 66], BF16)
        k.op(k.dve, lambda: nc.vector.memset(Vaug.ap[:, :, 64:66], 1.0), writes=[Vaug])
        ucs = Ring([self.sb(st, "uc%d" % i, [128, 8, TC], BF16) for i in range(1)])
        u_ds = [k.dsem()]
        csc = Ring([self.sb(st, "csc%d" % i, [128, 2, TC], F32) for i in range(2)])
        cs_ds = [k.dsem(), k.dsem()]
        xbr = Ring([self.sb(st, "xb%d" % i, [128, TC], BF16) for i in range(2)])
        rar = Ring([self.sb(st, "ra%d" % i, [128, TC], F32) for i in range(2)])
        rbr = Ring([self.sb(st, "rb%d" % i, [128, TC], F32) for i in range(2)])
        uT_v = self.uT.ap.rearrange("(c p) t -> p c t", p=128)
        ps = self.ps
        st_ring = Ring(ps[0:2])
        acc = ps[2:6]
        misc = Ring(ps[6:8])

        class CsView:
            pass

        def load_chunk(tc):
            cols = slice(tc * TC, (tc + 1) * TC)
            uc = ucs.next()
            k.dma(k.sp, u_ds[0], uc.ap, uT_v[:, :, cols], reads=[self.uT], writes=[uc])
            cc = csc.next()
            k.dma(k.sp, cs_ds[(csc.i - 1) % 2], cc.ap, self.cs_d.ap[:, :, cols], reads=[self.cs_d], writes=[cc])
            return uc, cc

        lcols = slice(0, TC)
        for tc in range(NCH):
            cols = slice(tc * TC, (tc + 1) * TC)
            uc, cc = load_chunk(tc)
            for W_, dstb in ((Wkk, KK), (Wikk, IKK)):
                pa = misc.next()
                for dc in range(8):
                    k.op(k.pe, lambda: nc.tensor.matmul(pa.ap, lhsT=W_.ap[:, dc, :], rhs=uc.ap[:, dc, :], start=(dc == 0), stop=(dc == 7)), reads=[W_, uc], writes=[pa], signal=(dc == 7))
                pb = misc.next()
                self.rope_fm(pa, 128, cc, lcols, dstb, dstb.ap[:, cols], xbr, rar, rbr, pb)
            pa = misc.next()
            for t4 in range(4):
                for dc in range(8):
                    k.op(k.pe, lambda: nc.tensor.matmul(pa.ap[:, t4 * 64:(t4 + 1) * 64], lhsT=uc.ap[:, dc, t4 * 128:(t4 + 1) * 128], rhs=Wv.ap[:, dc, :], start=(dc == 0), stop=(dc == 7)),
                         reads=[Wv, uc], writes=[pa], signal=(dc == 7 and t4 == 3))
            k.op(k.act, lambda: nc.scalar.copy(out=Vaug.ap[:, tc * 4:tc * 4 + 4, 0:64], in_=pa.ap[:, 0:256].rearrange("p (a b) -> p a b", a=4)), reads=[pa], writes=[Vaug])
        qTc = self.sb(st, "qTc", [128, 8, TC], BF16)
        iqTc = self.sb(st, "iqTc", [128, 4, TC], BF16)
        iwc = self.sb(st, "iwc", [128, 4, 8], F32)
        accr = Ring([self.sb(st, "sacc%d" % i, [128, S], F32) for i in range(2)])
        mb = self.sb(st, "mb", [128, S], F32)
        maskT = self.sb(st, "maskT", [128, 32, TC], BF16)
        rlr = Ring([self.sb(st, "rl%d" % i, [128, TC], F32) for i in range(3)])
        lo = self.sb(st, "lo", [128, 1], F32)
        mid = self.sb(st, "mid", [128, 1], F32)
        cnt = self.sb(st, "cnt", [128, 1], F32)
        tt = self.sb(st, "tt", [128, 1], F32)
        pts = Ring([self.sb(st, "pt%d" % i, [128, TC], BF16) for i in range(4)])
        recr = Ring([self.sb(st, "rec%d" % i, [128, 1], F32) for i in range(8)])
        onp = Ring([self.sb(st, "onp%d" % i, [128, 4, 128], F32) for i in range(2)])
        oTc = self.sb(st, "oTc", [128, 8, TC], BF16)
        oc_ds = k.dsem()
        oT_v = self.oT.ap.rearrange("(c p) t -> p c t", p=128)
        scale = 64 ** -0.5
        iw_scale = (64 ** -0.5) * (8 ** -0.5)
        NIT = 18
        for c in range(NCH):
            cols = slice(c * TC, (c + 1) * TC)
            uc, cc = load_chunk(c)
            for pair in range(8):
                pa = misc.next()
                for dc in range(8):
                    k.op(k.pe, lambda: nc.tensor.matmul(pa.ap, lhsT=Wq.ap[:, dc, pair * 128:(pair + 1) * 128], rhs=uc.ap[:, dc, :], start=(dc == 0), stop=(dc == 7)), reads=[Wq, uc], writes=[pa], signal=(dc == 7))
                pb = misc.next()
                self.rope_fm(pa, 128, cc, lcols, qTc, qTc.ap[:, pair, :], xbr, rar, rbr, pb)
            for pair in range(4):
                pa = misc.next()
                for dc in range(8):
                    k.op(k.pe, lambda: nc.tensor.matmul(pa.ap, lhsT=Wiq.ap[:, dc, pair * 128:(pair + 1) * 128], rhs=uc.ap[:, dc, :], start=(dc == 0), stop=(dc == 7)), reads=[Wiq, uc], writes=[pa], signal=(dc == 7))
                pb = misc.next()
                self.rope_fm(pa, 128, cc, lcols, iqTc, iqTc.ap[:, pair, :], xbr, rar, rbr, pb)
            pa = misc.next()
            for qt in range(4):
                for dc in range(8):
                    k.op(k.pe, lambda: nc.tensor.matmul(pa.ap[:, qt * 8:(qt + 1) * 8], lhsT=uc.ap[:, dc, qt * 128:(qt + 1) * 128], rhs=Wiw.ap[:, dc, :], start=(dc == 0), stop=(dc == 7)),
                         reads=[Wiw, uc], writes=[pa], signal=(dc == 7 and qt == 3))
            k.op(k.dve, lambda: nc.vector.tensor_scalar(out=iwc.ap, in0=pa.ap[:, 0:32].rearrange("p (a b) -> p a b", a=4), scalar1=iw_scale, scalar2=None, op0=ALU.mult), reads=[pa], writes=[iwc])
            for qt in range(4):
                L = c * TC + 128 * (qt + 1)
                sa = accr.next()
                for kc in range(c + 1):
                    n = min(TC, L - kc * TC)
                    for h in range(8):
                        b0 = 64 * (h % 2)
                        stb = st_ring.next()
                        k.op(k.pe, lambda: nc.tensor.matmul(stb.ap[:, 0:n], lhsT=iqTc.ap[b0:b0 + 64, h // 2, qt * 128:(qt + 1) * 128], rhs=IKK.ap[b0:b0 + 64, kc * TC:kc * TC + n], start=True, stop=True),
                             reads=[iqTc, IKK], writes=[stb])
                        rl = rlr.next()
                        k.op(k.act, lambda: nc.scalar.activation(out=rl.ap[:, 0:n], in_=stb.ap[:, 0:n], func=AF.Relu), reads=[stb], writes=[rl])
                        if h == 0:
                            k.op(k.dve, lambda: nc.vector.tensor_scalar(out=sa.ap[:, kc * TC:kc * TC + n], in0=rl.ap[:, 0:n], scalar1=iwc.ap[:, qt, 0:1], scalar2=None, op0=ALU.mult), reads=[rl, iwc], writes=[sa])
                        else:
                            k.op(k.dve, lambda: nc.vector.scalar_tensor_tensor(out=sa.ap[:, kc * TC:kc * TC + n], in0=rl.ap[:, 0:n], scalar=iwc.ap[:, qt, h:h + 1], in1=sa.ap[:, kc * TC:kc * TC + n], op0=ALU.mult, op1=ALU.add),
                                 reads=[rl, iwc, sa], writes=[sa])
                k.op(k.pool, lambda: nc.gpsimd.affine_select(out=sa.ap[:, L - 128:L], in_=sa.ap[:, L - 128:L], pattern=[[-1, 128]], compare_op=ALU.is_ge, fill=NEG, base=0, channel_multiplier=1),
                     reads=[sa], writes=[sa])
                k.op(k.dve, lambda: nc.vector.memset(lo.ap, -32.0), writes=[lo])
                k.op(k.dve, lambda: nc.vector.memset(mid.ap, 0.0), writes=[mid])
                w = 64.0
                for it in range(NIT):
                    k.op(k.dve, lambda: nc.vector.tensor_scalar(out=mb.ap[:, 0:L], in0=sa.ap[:, 0:L], scalar1=mid.ap, scalar2=None, op0=ALU.is_ge, op1=ALU.add, accum_out=cnt.ap), reads=[sa, mid], writes=[mb, cnt])
                    k.op(k.dve, lambda: nc.vector.tensor_scalar(out=tt.ap, in0=cnt.ap, scalar1=256.0, scalar2=w / 2, op0=ALU.is_ge, op1=ALU.mult), reads=[cnt], writes=[tt])
                    k.op(k.dve, lambda: nc.vector.tensor_tensor(out=lo.ap, in0=lo.ap, in1=tt.ap, op=ALU.add), reads=[lo, tt], writes=[lo])
                    k.op(k.dve, lambda: nc.vector.tensor_scalar(out=mid.ap, in0=lo.ap, scalar1=w / 4, scalar2=None, op0=ALU.add), reads=[lo], writes=[mid])
                    w = w / 2
                k.op(k.dve, lambda: nc.vector.tensor_scalar(out=mb.ap[:, 0:L], in0=sa.ap[:, 0:L], scalar1=lo.ap, scalar2=None, op0=ALU.is_ge), reads=[sa, lo], writes=[mb])
                nkt = L // 128
                for k0 in range(0, nkt, 4):
                    nb = min(4, nkt - k0)
                    pbk = misc.next()
                    for i in range(nb):
                        k.op(k.pe, lambda: nc.tensor.transpose(pbk.ap[:, i * 128:(i + 1) * 128], mb.ap[:, (k0 + i) * 128:(k0 + i + 1) * 128], self.ident_f.ap), reads=[mb, self.ident_f], writes=[pbk], signal=(i == nb - 1))
                    k.op(k.act, lambda: nc.scalar.copy(out=maskT.ap[:, k0:k0 + nb, qt * 128:(qt + 1) * 128], in_=pbk.ap[:, 0:nb * 128].rearrange("p (a b) -> p a b", a=nb)), reads=[pbk], writes=[maskT])
            for h in range(16):
                b0 = 64 * (h % 2)
                self.attn_core(c,
                               lambda j: [KK.ap[b0:b0 + 64, j * 128:(j + 1) * 128]],
                               lambda off, n: [qTc.ap[b0:b0 + 64, h // 2, off:off + n]],
                               [KK, qTc], lambda j: Vaug.ap[:, j, 0:65], Vaug, 64, scale, st_ring, pts, acc, tri=False,
                               mask_fn=lambda j, off, n: maskT.ap[:, j, off:off + n], maskbuf=maskT, mask_eng=lambda j: (k.pool if j % 3 != 2 else k.dve))
                if h % 2 == 0:
                    onb = onp.next()
                self.evac_norm(acc, 64, onb, lambda qt: onb.ap[:, qt, b0:b0 + 64], recr)
                if h % 2 == 1:
                    self.transpose_out(onb, lambda qt: onb.ap[:, qt, :], misc.next(), oTc, oTc.ap[:, h // 2, :], k.act)
            k.dma(k.sp, oc_ds, oT_v[:, :, cols], oTc.ap, reads=[oTc], writes=[self.oT])


_PROG_CACHE = {}


def _get_prog(n_layers=DEPTH):
    if n_layers not in _PROG_CACHE:
        _PROG_CACHE[n_layers] = Prog(n_layers)
    return _PROG_CACHE[n_layers]


def _host_inputs(inputs):
    f = lambda a: np.ascontiguousarray(np.asarray(a, dtype=np.float32))
    an = f(inputs["attn_norm"]).reshape(4, 8, 128).transpose(2, 0, 1).reshape(128, 32)
    fn = f(inputs["ffn_norm"]).reshape(4, 8, 128).transpose(2, 0, 1).reshape(128, 32)
    fin = f(inputs["final_norm"]).reshape(8, 128).T
    qn = f(inputs["mla_q_norm"]).reshape(3, 128).T
    kn = f(inputs["mla_kv_norm"]).reshape(2, 128).T
    small = f(np.concatenate([an, fn, fin, qn, kn], axis=1))
    lam = f(np.broadcast_to(f(inputs["diff_lambda"]).reshape(1, 512), (128, 512)))
    subln = f(np.broadcast_to(f(inputs["diff_subln"]).reshape(1, 256), (128, 256)))
    shared = {"small": small, "lam": lam, "subln": subln}
    for l in range(DEPTH):
        m, j = l % 3, l // 3
        if m == 0:
            shared["wqkv%d" % l] = f(inputs["diff_wqkv"][j])
            shared["wo%d" % l] = f(inputs["diff_wo"][j])
        elif m == 1:
            shared["win%d" % l] = f(inputs["dsa_win"][j])
            shared["wo%d" % l] = f(inputs["dsa_wo"][j])
        else:
            shared["wdown%d" % l] = f(inputs["mla_wdown"][j])
            shared["wuq%d" % l] = f(inputs["mla_wuq"][j])
            shared["wukv%d" % l] = f(inputs["mla_wukv"][j])
            shared["wo%d" % l] = f(inputs["mla_wo"][j])
        shared["w1_%d" % l] = f(inputs["ffn_w1"][l])
        shared["w3_%d" % l] = f(inputs["ffn_w3"][l])
        shared["w2_%d" % l] = f(inputs["ffn_w2"][l])
    return shared


def kernel(**inputs):
    x = np.asarray(inputs["x"], dtype=np.float32)
    B = x.shape[0]
    shared = _host_inputs(inputs)
    prog = _get_prog(DEPTH)
    in_maps = []
    for b in range(B):
        m = dict(shared)
        m["x"] = np.ascontiguousarray(x[b])
        in_maps.append(m)
    res = run_bass_kernel_spmd(prog.nc, in_maps, core_ids=list(range(B)))
    return np.stack([np.asarray(r["out"], dtype=np.float32) for r in res.results], axis=0)
```

```python
import math
from contextlib import ExitStack
import numpy as np
import concourse.bass as bass
import concourse.mybir as mybir
from concourse.bass_utils import run_bass_kernel_spmd

F32 = mybir.dt.float32
BF16 = mybir.dt.bfloat16
I32 = mybir.dt.int32
ALU = mybir.AluOpType
AF = mybir.ActivationFunctionType
AX = mybir.AxisListType

S = 4096
D = 1024
DFF = 2816
NCH = 8
TC = 512
EPS = 1e-6
DEPTH = 4
NEG = -1.0e30


class Sem:
    def __init__(self, nc, name):
        self.h = nc.alloc_semaphore(name)
        self.count = 0


class Tok:
    __slots__ = ("sem", "val")

    def __init__(self, sem=None, val=0):
        self.sem = sem
        self.val = val


class Buf:
    __slots__ = ("ap", "w", "r", "excl")

    def __init__(self, ap, excl=False):
        self.ap = ap
        self.w = None
        self.r = {}
        self.excl = excl


class Eng:
    def __init__(self, k, eng, name, is_pe=False):
        self.eng = eng
        self.sem = Sem(k.nc, "e_" + name)
        self.seen = {}
        self.is_pe = is_pe
        self.name = name

    def wait(self, sem, val):
        if val <= self.seen.get(sem, 0):
            return
        self.eng.wait_ge(sem.h, val)
        self.seen[sem] = val


class K:
    def __init__(self, nc):
        self.nc = nc
        self.pe = Eng(self, nc.tensor, "pe", True)
        self.act = Eng(self, nc.scalar, "act")
        self.dve = Eng(self, nc.vector, "dve")
        self.pool = Eng(self, nc.gpsimd, "pool")
        self.sp = Eng(self, nc.sync, "sp")
        self.engs = [self.pe, self.act, self.dve, self.pool, self.sp]
        self.dsems = []
        self.fixed = []
        self.nsem = 0

    def dsem(self, name=None):
        if self.nsem < len(self.dsems):
            s = self.dsems[self.nsem]
        else:
            s = Sem(self.nc, "d%d" % self.nsem)
            self.dsems.append(s)
        self.nsem += 1
        return s

    def _w(self, e, tok):
        if e.is_pe and tok.sem is e.sem:
            return
        e.wait(tok.sem, tok.val)

    def _deps(self, e, reads, writes):
        for b in reads:
            if b.w is not None:
                self._w(e, b.w)
            if b.excl:
                for t in b.r.values():
                    if t.sem is not e.sem:
                        self._w(e, t)
        for b in writes:
            if b.w is not None:
                self._w(e, b.w)
            for t in b.r.values():
                self._w(e, t)

    def _upd(self, tok, reads, writes):
        for b in writes:
            b.w = tok
            b.r = {}
        for b in reads:
            if b not in writes:
                o = b.r.get(tok.sem)
                if o is None or o.val < tok.val or o is tok:
                    b.r[tok.sem] = tok

    def op(self, e, fn, reads=(), writes=(), signal=True):
        self._deps(e, reads, writes)
        inst = fn()
        if signal:
            e.sem.count += 1
            inst.then_inc(e.sem.h, 1)
            tok = Tok(e.sem, e.sem.count)
        else:
            tok = Tok(e.sem, e.sem.count + 1)
        self._upd(tok, reads, writes)
        return inst

    def dma(self, q, dsem, out_ap, in_ap, reads=(), writes=(), group=None, **kw):
        self._deps(q, reads, writes)
        if group is None or group.val == 0:
            q.wait(dsem, dsem.count)
        inst = q.eng.dma_start(out=out_ap, in_=in_ap, **kw)
        dsem.count += 16
        inst.then_inc(dsem.h, 16)
        if group is not None:
            group.sem = dsem
            group.val = dsem.count
            tok = group
        else:
            tok = Tok(dsem, dsem.count)
        self._upd(tok, reads, writes)
        return inst

    def barrier(self):
        self.nsem = 0
        for e in self.engs:
            for o in self.engs[:4]:
                if o is not e and o.sem.count > 0:
                    e.wait(o.sem, o.sem.count)
            for d in self.dsems + self.fixed:
                if d.count > 0:
                    e.wait(d, d.count)


class Ring:
    def __init__(self, bufs):
        self.bufs = bufs
        self.i = 0

    def next(self):
        b = self.bufs[self.i % len(self.bufs)]
        self.i += 1
        return b


class Prog:
    def __init__(self, n_layers=DEPTH):
        self.n_layers = n_layers
        nc = bass.Bass("TRN2", target_bir_lowering=False)
        self.nc = nc
        self.k = K(nc)
        self.build()

    def sb(self, st, name, shape, dt):
        self.uid = getattr(self, "uid", 0) + 1
        t = st.enter_context(self.nc.sbuf_tensor("s%d_%s" % (self.uid, name), list(shape), dt))
        return Buf(t[:])

    def din(self, name, shape, dt=F32):
        return self.nc.dram_tensor(name, list(shape), dt, kind="ExternalInput").ap()

    def dscr(self, name, shape, dt):
        import os as _os
        if _os.environ.get("KDEBUG_OUT") and name in ("uT", "cs_d", "oT", "hT"):
            return Buf(self.nc.dram_tensor(name, list(shape), dt, kind="ExternalOutput").ap())
        return Buf(self.nc.dram_tensor(name, list(shape), dt).ap())

    def build(self):
        nc, k = self.nc, self.k
        self.x = self.din("x", [S, D])
        self.out = nc.dram_tensor("out", [S, D], F32, kind="ExternalOutput").ap()
        self.p_small = self.din("small", [128, 32 + 32 + 8 + 3 + 2])
        self.p_lam = self.din("lam", [128, 512])
        self.p_subln = self.din("subln", [128, 256])
        self.wspec = {}
        wl = []
        for l in range(DEPTH):
            m = l % 3
            if m == 0:
                wl.append([("wqkv%d" % l, D, 3072), ("wo%d" % l, D, D)])
            elif m == 1:
                wl.append([("win%d" % l, D, 1736), ("wo%d" % l, D, D)])
            else:
                wl.append([("wdown%d" % l, D, 704), ("wuq%d" % l, 384, 1536), ("wukv%d" % l, 256, 2048), ("wo%d" % l, D, D)])
            wl[-1] += [("w1_%d" % l, D, DFF), ("w3_%d" % l, D, DFF), ("w2_%d" % l, DFF, D)]
        self.wf = {}
        self.wb = {}
        for l in range(DEPTH):
            for (n, kk, nn) in wl[l]:
                self.wf[n] = self.din(n, [kk, nn])
                self.wb[n] = self.dscr(n + "_b", [kk, nn], BF16)
        self.wl = wl
        self.hT = [self.dscr("hT", [D, S], F32)]
        self.uT = self.dscr("uT", [D, S], BF16)
        self.oT = self.dscr("oT", [D, S], BF16)
        self.cs_d = self.dscr("cs_d", [128, 2, S], F32)

        with ExitStack() as st0:
            self.setup_persistent(st0)
            self.cast_weights()
            with ExitStack() as st:
                self.rope_tables(st)
            k.barrier()
            with ExitStack() as st:
                self.post_phase(st, -1)
            for l in range(self.n_layers):
                k.barrier()
                m = l % 3
                import os as _os
                with ExitStack() as st:
                    if _os.environ.get("KSKIP_ATTN"):
                        z = self.sb(st, "zz", [128, S], BF16)
                        k.op(k.dve, lambda: nc.vector.memset(z.ap, 0.0), writes=[z])
                        for i in range(8):
                            k.dma(k.sp, k.dsem(), self.oT.ap[i * 128:(i + 1) * 128, :], z.ap, reads=[z], writes=[self.oT])
                    elif m == 0:
                        self.diff_phase(st, l)
                    elif m == 1:
                        self.dsa_phase(st, l)
                    else:
                        self.mla_phase(st, l)
                k.barrier()
                with ExitStack() as st:
                    if not _os.environ.get("KSKIP_POST"):
                        self.post_phase(st, l)
            k.barrier()

    def setup_persistent(self, st):
        nc, k = self.nc, self.k
        self.ps = [Buf(nc.alloc_psum_tensor("ps%d" % i, [128, 512], F32)[:], excl=True) for i in range(8)]
        self.ident_f = self.sb(st, "ident_f", [128, 128], F32)
        self.ident_b = self.sb(st, "ident_b", [128, 128], BF16)
        self.ones_f = self.sb(st, "ones_f", [128, 128], F32)
        self.tri = self.sb(st, "tri", [128, 128], BF16)
        self.rperm = self.sb(st, "rperm", [128, 128], BF16)
        self.small = self.sb(st, "small", [128, 77], F32)
        self.lam = self.sb(st, "lamsb", [128, 512], F32)
        self.subln = self.sb(st, "sublnsb", [128, 256], F32)
        self.neglam = self.sb(st, "neglam", [128, 2], F32)
        self.epsb = self.sb(st, "epsb", [128, 1], F32)
        k.op(k.dve, lambda: nc.vector.memset(self.epsb.ap, EPS), writes=[self.epsb])
        k.dma(k.sp, k.dsem(), self.small.ap, self.p_small, writes=[self.small])
        k.dma(k.sp, k.dsem(), self.lam.ap, self.p_lam, writes=[self.lam])
        k.dma(k.sp, k.dsem(), self.subln.ap, self.p_subln, writes=[self.subln])
        with ExitStack() as t:
            io = self.sb(t, "io", [128, 128], I32)
            dif = self.sb(t, "dif", [128, 128], F32)
            ta = self.sb(t, "ta", [128, 128], F32)
            tb = self.sb(t, "tb", [128, 128], F32)
            co = self.sb(t, "co", [128, 128], F32)
            ioj = self.sb(t, "ioj", [128, 128], I32)
            k.op(k.pool, lambda: nc.gpsimd.iota(io.ap, pattern=[[1, 128]], base=0, channel_multiplier=-1), writes=[io])
            k.op(k.dve, lambda: nc.vector.tensor_copy(out=dif.ap, in_=io.ap), reads=[io], writes=[dif])
            k.op(k.dve, lambda: nc.vector.tensor_single_scalar(out=self.ident_f.ap, in_=dif.ap, scalar=0.0, op=ALU.is_equal), reads=[dif], writes=[self.ident_f])
            k.op(k.dve, lambda: nc.vector.tensor_copy(out=self.ident_b.ap, in_=self.ident_f.ap), reads=[self.ident_f], writes=[self.ident_b])
            k.op(k.dve, lambda: nc.vector.memset(self.ones_f.ap, 1.0), writes=[self.ones_f])
            k.op(k.dve, lambda: nc.vector.tensor_single_scalar(out=self.tri.ap, in_=dif.ap, scalar=0.0, op=ALU.is_ge), reads=[dif], writes=[self.tri])
            k.op(k.pool, lambda: nc.gpsimd.iota(ioj.ap, pattern=[[1, 128]], base=0, channel_multiplier=0), writes=[ioj])
            k.op(k.dve, lambda: nc.vector.tensor_single_scalar(out=ioj.ap, in_=ioj.ap, scalar=32, op=ALU.bitwise_and), reads=[ioj], writes=[ioj])
            k.op(k.dve, lambda: nc.vector.tensor_copy(out=co.ap, in_=ioj.ap), reads=[ioj], writes=[co])
            k.op(k.dve, lambda: nc.vector.tensor_single_scalar(out=co.ap, in_=co.ap, scalar=16.0, op=ALU.is_ge), reads=[co], writes=[co])
            k.op(k.dve, lambda: nc.vector.tensor_single_scalar(out=ta.ap, in_=dif.ap, scalar=32.0, op=ALU.is_equal), reads=[dif], writes=[ta])
            k.op(k.dve, lambda: nc.vector.tensor_single_scalar(out=tb.ap, in_=dif.ap, scalar=-32.0, op=ALU.is_equal), reads=[dif], writes=[tb])
            k.op(k.dve, lambda: nc.vector.tensor_tensor(out=ta.ap, in0=ta.ap, in1=co.ap, op=ALU.mult), reads=[ta, co], writes=[ta])
            k.op(k.dve, lambda: nc.vector.tensor_scalar(out=co.ap, in0=co.ap, scalar1=-1.0, scalar2=1.0, op0=ALU.mult, op1=ALU.add), reads=[co], writes=[co])
            k.op(k.dve, lambda: nc.vector.tensor_tensor(out=tb.ap, in0=tb.ap, in1=co.ap, op=ALU.mult), reads=[tb, co], writes=[tb])
            k.op(k.dve, lambda: nc.vector.tensor_tensor(out=self.rperm.ap, in0=ta.ap, in1=tb.ap, op=ALU.subtract), reads=[ta, tb], writes=[self.rperm])
            pr = self.sb(t, "lampr", [128, 256], F32)
            sm = self.sb(t, "lamsm", [128, 4], F32)
            for j in range(2):
                layer = 3 * j
                li = 0.8 - 0.6 * math.exp(-0.3 * layer)
                lm = self.lam.ap[:, j * 256:(j + 1) * 256]
                k.op(k.dve, lambda: nc.vector.tensor_tensor(out=pr.ap[:, 0:64], in0=lm[:, 0:64], in1=lm[:, 64:128], op=ALU.mult), reads=[self.lam], writes=[pr])
                k.op(k.dve, lambda: nc.vector.tensor_tensor(out=pr.ap[:, 64:128], in0=lm[:, 128:192], in1=lm[:, 192:256], op=ALU.mult), reads=[self.lam], writes=[pr])
                k.op(k.dve, lambda: nc.vector.tensor_reduce(out=sm.ap[:, 0:2], in_=pr.ap[:, 0:128].rearrange("p (a b) -> p a b", a=2), axis=AX.X, op=ALU.add), reads=[pr], writes=[sm])
                k.op(k.act, lambda: nc.scalar.activation(out=sm.ap[:, 2:4], in_=sm.ap[:, 0:2], func=AF.Exp), reads=[sm], writes=[sm])
                k.op(k.dve, lambda: nc.vector.tensor_tensor(out=sm.ap[:, 0:1], in0=sm.ap[:, 3:4], in1=sm.ap[:, 2:3], op=ALU.subtract), reads=[sm], writes=[sm])
                k.op(k.dve, lambda: nc.vector.tensor_scalar(out=self.neglam.ap[:, j:j + 1], in0=sm.ap[:, 0:1], scalar1=-li, scalar2=None, op0=ALU.add), reads=[sm], writes=[self.neglam])
            for j in range(2):
                li = 0.8 - 0.6 * math.exp(-0.3 * 3 * j)
                sl = self.subln.ap[:, j * 128:(j + 1) * 128]
                k.op(k.dve, lambda: nc.vector.tensor_scalar(out=sl, in0=sl, scalar1=1.0 - li, scalar2=None, op0=ALU.mult), reads=[self.subln], writes=[self.subln])
            k.barrier()

    def g_attn(self, l, dc):
        return self.small.ap[:, l * 8 + dc:l * 8 + dc + 1]

    def g_ffn(self, l, dc):
        return self.small.ap[:, 32 + l * 8 + dc:32 + l * 8 + dc + 1]

    def g_final(self, dc):
        return self.small.ap[:, 64 + dc:64 + dc + 1]

    def cast_weights(self):
        k = self.k
        for l in range(DEPTH):
            ds = Sem(self.nc, "cast%d" % l)
            k.fixed.append(ds)
            g = Tok()
            for (n, kk, nn) in self.wl[l]:
                src, dst = self.wf[n], self.wb[n]
                nsplit = 1 if nn <= 2048 else 2
                w = nn // nsplit
                for i in range(nsplit):
                    k.dma(k.pool, ds, dst.ap[:, i * w:(i + 1) * w], src[:, i * w:(i + 1) * w], writes=[dst], group=g)

    def rope_tables(self, st):
        nc, k = self.nc, self.k
        pi = self.sb(st, "r_pi", [128, 1], I32)
        pf = self.sb(st, "r_pf", [128, 1], F32)
        inv = self.sb(st, "r_inv", [128, 1], F32)
        k.op(k.pool, lambda: nc.gpsimd.iota(pi.ap, pattern=[[0, 1]], base=0, channel_multiplier=1), writes=[pi])
        k.op(k.dve, lambda: nc.vector.tensor_single_scalar(out=pi.ap, in_=pi.ap, scalar=31, op=ALU.bitwise_and), reads=[pi], writes=[pi])
        k.op(k.dve, lambda: nc.vector.tensor_copy(out=pf.ap, in_=pi.ap), reads=[pi], writes=[pf])
        k.op(k.act, lambda: nc.scalar.activation(out=inv.ap, in_=pf.ap, func=AF.Exp, scale=-math.log(10000.0) / 32.0), reads=[pf], writes=[inv])
        k.op(k.dve, lambda: nc.vector.tensor_scalar(out=inv.ap, in0=inv.ap, scalar1=1.0 / (2.0 * math.pi), scalar2=None, op0=ALU.mult), reads=[inv], writes=[inv])
        W = 1024
        ti = self.sb(st, "r_ti", [128, W], I32)
        tt = self.sb(st, "r_tt", [128, W], F32)
        ff = self.sb(st, "r_ff", [128, W], F32)
        ni = self.sb(st, "r_ni", [128, W], I32)
        nf = self.sb(st, "r_nf", [128, W], F32)
        g = self.sb(st, "r_g", [128, W], F32)
        res = [self.sb(st, "r_res%d" % i, [128, 2, W], F32) for i in range(2)]
        dss = [k.dsem(), k.dsem()]
        for c in range(S // W):
            rb = res[c % 2]
            k.op(k.pool, lambda: nc.gpsimd.iota(ti.ap, pattern=[[1, W]], base=c * W, channel_multiplier=0), writes=[ti])
            k.op(k.dve, lambda: nc.vector.tensor_copy(out=tt.ap, in_=ti.ap), reads=[ti], writes=[tt])
            k.op(k.dve, lambda: nc.vector.tensor_scalar(out=tt.ap, in0=tt.ap, scalar1=inv.ap, scalar2=None, op0=ALU.mult), reads=[tt, inv], writes=[tt])
            for which in range(2):
                off = 0.25 if which == 0 else 0.0
                k.op(k.dve, lambda: nc.vector.tensor_scalar(out=ff.ap, in0=tt.ap, scalar1=off, scalar2=None, op0=ALU.add), reads=[tt], writes=[ff])
                k.op(k.dve, lambda: nc.vector.tensor_copy(out=ni.ap, in_=ff.ap), reads=[ff], writes=[ni])
                k.op(k.dve, lambda: nc.vector.tensor_copy(out=nf.ap, in_=ni.ap), reads=[ni], writes=[nf])
                k.op(k.dve, lambda: nc.vector.tensor_tensor(out=ff.ap, in0=ff.ap, in1=nf.ap, op=ALU.subtract), reads=[ff, nf], writes=[ff])
                k.op(k.dve, lambda: nc.vector.tensor_single_scalar(out=g.ap, in_=ff.ap, scalar=0.5, op=ALU.is_gt), reads=[ff], writes=[g])
                k.op(k.dve, lambda: nc.vector.tensor_tensor(out=ff.ap, in0=ff.ap, in1=g.ap, op=ALU.subtract), reads=[ff, g], writes=[ff])
                k.op(k.dve, lambda: nc.vector.tensor_single_scalar(out=g.ap, in_=ff.ap, scalar=-0.5, op=ALU.is_lt), reads=[ff], writes=[g])
                k.op(k.dve, lambda: nc.vector.tensor_tensor(out=ff.ap, in0=ff.ap, in1=g.ap, op=ALU.add), reads=[ff, g], writes=[ff])
                k.op(k.act, lambda: nc.scalar.activation(out=rb.ap[:, which, :], in_=ff.ap, func=AF.Sin, scale=2.0 * math.pi * (1.0 - 1e-6)), reads=[ff], writes=[rb])
            k.dma(k.sp, dss[c % 2], self.cs_d.ap[:, :, c * W:(c + 1) * W], rb.ap, reads=[rb], writes=[self.cs_d])

    def rmsnorm_fm(self, src, nchunk, width, gain_fn, dst, dst_ap_fn, sqr, ssps, lnb, rstd, dim, src_ap_fn=None, src_bufs=None):
        nc, k = self.nc, self.k
        sbufs = src_bufs if src_bufs is not None else [src]
        for dc in range(nchunk):
            sq = sqr.next()
            sap = src_ap_fn(dc)
            k.op(k.act, lambda: nc.scalar.activation(out=sq.ap[:, :width], in_=sap, func=AF.Square), reads=sbufs, writes=[sq])
            k.op(k.pe, lambda: nc.tensor.matmul(ssps.ap[:, :width], lhsT=self.ones_f.ap, rhs=sq.ap[:, :width], start=(dc == 0), stop=(dc == nchunk - 1)),
                 reads=[sq, self.ones_f], writes=[ssps])
        k.op(k.act, lambda: nc.scalar.activation(out=lnb.ap[:, :width], in_=ssps.ap[:, :width], func=AF.Ln, scale=1.0 / dim, bias=self.epsb.ap), reads=[ssps, self.epsb], writes=[lnb])
        k.op(k.act, lambda: nc.scalar.activation(out=rstd.ap[:, :width], in_=lnb.ap[:, :width], func=AF.Exp, scale=-0.5), reads=[lnb], writes=[rstd])
        for dc in range(nchunk):
            sap = src_ap_fn(dc)
            dap = dst_ap_fn(dc)
            k.op(k.dve, lambda: nc.vector.scalar_tensor_tensor(out=dap, in0=sap, scalar=gain_fn(dc), in1=rstd.ap[:, :width], op0=ALU.mult, op1=ALU.mult),
                 reads=sbufs + [rstd, self.small], writes=[dst])

    def eps_ap(self):
        return self.epsb.ap

    def post_phase(self, st, l):
        nc, k = self.nc, self.k
        first = (l < 0)
        last = (l == self.n_layers - 1)
        hcs = Ring([self.sb(st, "hc%d" % i, [128, 8, TC], F32) for i in range(2)])
        h_ds = [k.dsem(), k.dsem()]
        hst_ds = [k.dsem(), k.dsem()]
        sqr = Ring([self.sb(st, "sq%d" % i, [128, TC], F32) for i in range(2)])
        lnb = self.sb(st, "lnb", [128, TC], F32)
        rstd = self.sb(st, "rstd", [128, TC], F32)
        if not last:
            uns = Ring([self.sb(st, "un%d" % i, [128, 8, TC], BF16) for i in range(1)])
            un_ds = [k.dsem(), k.dsem()]
        ssps = self.ps[7]
        hT_v = self.hT[0].ap.rearrange("(c p) t -> p c t", p=128)
        uT_v = self.uT.ap.rearrange("(c p) t -> p c t", p=128)
        oT_v = self.oT.ap.rearrange("(c p) t -> p c t", p=128)
        if first:
            xts = Ring([self.sb(st, "xt%d" % i, [128, D], F32) for i in range(8)])
            x_ds = [k.dsem() for _ in range(8)]
            psr = Ring(self.ps[0:6])
        else:
            ocs = Ring([self.sb(st, "oc%d" % i, [128, 8, TC], BF16) for i in range(2)])
            o_ds = [k.dsem(), k.dsem()]
            ucs = Ring([self.sb(st, "uc%d" % i, [128, 8, TC], BF16) for i in range(2)])
            gT = self.sb(st, "gT", [128, 22, TC], BF16)
            sil = Ring([self.sb(st, "sil%d" % i, [128, TC], F32) for i in range(2)])
            wo = self.sb(st, "wo", [128, 8, D], BF16)
            w2 = self.sb(st, "w2", [128, 22, D], BF16)
            w13 = Ring([self.sb(st, "w13_%d" % i, [128, 2, 8, 512], BF16) for i in range(2)])
            w_ds = [k.dsem(), k.dsem()]
            wres_ds = k.dsem()
            psr = Ring(self.ps[0:6])
            wob, w1b, w3b, w2b = self.wb["wo%d" % l], self.wb["w1_%d" % l], self.wb["w3_%d" % l], self.wb["w2_%d" % l]
            k.dma(k.sp, wres_ds, wo.ap, wob.ap.rearrange("(c p) f -> p c f", p=128), reads=[wob], writes=[wo])
            k.dma(k.sp, k.dsem(), w2.ap, w2b.ap.rearrange("(c p) f -> p c f", p=128), reads=[w2b], writes=[w2])
            w1v = w1b.ap.rearrange("(c p) f -> p c f", p=128)
            w3v = w3b.ap.rearrange("(c p) f -> p c f", p=128)
            fgs = [(i * 512, 512) for i in range(5)] + [(2560, 256)]
        if last:
            orow = Ring([self.sb(st, "orow%d" % i, [128, D], F32) for i in range(2)])
            or_ds = [k.dsem(), k.dsem()]

        def load_chunk(c):
            hc = hcs.bufs[c % 2]
            cols = slice(c * TC, (c + 1) * TC)
            if not first:
                k.dma(k.sp, h_ds[c % 2], hc.ap, hT_v[:, :, cols], reads=[self.hT[0]], writes=[hc])
                oc = ocs.bufs[c % 2]
                k.dma(k.sp, o_ds[c % 2], oc.ap, oT_v[:, :, cols], reads=[self.oT], writes=[oc])

        if not first:
            load_chunk(0)
        for c in range(NCH):
            cols = slice(c * TC, (c + 1) * TC)
            hc = hcs.bufs[c % 2]
            if first:
                xtl = []
                for i in range(4):
                    xt = xts.next()
                    k.dma(k.sp, x_ds[(xts.i - 1) % 8], xt.ap, self.x[c * TC + i * 128:c * TC + (i + 1) * 128, :], writes=[xt])
                    xtl.append(xt)
                for dc in range(8):
                    pb = psr.next()
                    for i in range(4):
                        k.op(k.pe, lambda: nc.tensor.transpose(pb.ap[:, i * 128:(i + 1) * 128], xtl[i].ap[:, dc * 128:(dc + 1) * 128], self.ident_f.ap),
                             reads=[xtl[i], self.ident_f], writes=[pb], signal=(i == 3))
                    e = k.act if dc % 2 == 0 else k.dve
                    if e is k.act:
                        k.op(e, lambda: nc.scalar.copy(out=hc.ap[:, dc, :], in_=pb.ap), reads=[pb], writes=[hc])
                    else:
                        k.op(e, lambda: nc.vector.tensor_copy(out=hc.ap[:, dc, :], in_=pb.ap), reads=[pb], writes=[hc])
            else:
                if c + 1 < NCH:
                    load_chunk(c + 1)
                oc = ocs.bufs[c % 2]
                for dc in range(8):
                    pb = psr.next()
                    for cc in range(8):
                        k.op(k.pe, lambda: nc.tensor.matmul(pb.ap, lhsT=wo.ap[:, cc, dc * 128:(dc + 1) * 128], rhs=oc.ap[:, cc, :], start=(cc == 0), stop=(cc == 7)),
                             reads=[wo, oc], writes=[pb], signal=(cc == 7))
                    k.op(k.dve, lambda: nc.vector.tensor_tensor(out=hc.ap[:, dc, :], in0=pb.ap, in1=hc.ap[:, dc, :], op=ALU.add), reads=[pb, hc], writes=[hc])
                uc = ucs.next()
                self.rmsnorm_fm(hc, 8, TC, lambda dc: self.g_ffn(l, dc), uc, lambda dc: uc.ap[:, dc, :], sqr, ssps, lnb, rstd, float(D),
                                src_ap_fn=lambda dc: hc.ap[:, dc, :])
                def load_w13(gi):
                    f0, fw = fgs[gi]
                    wt = w13.bufs[gi % 2]
                    g = Tok()
                    k.dma(k.sp, w_ds[gi % 2], wt.ap[:, 0, :, :fw], w1v[:, :, f0:f0 + fw], reads=[w1b], writes=[wt], group=g)
                    k.dma(k.sp, w_ds[gi % 2], wt.ap[:, 1, :, :fw], w3v[:, :, f0:f0 + fw], reads=[w3b], writes=[wt], group=g)
                load_w13(0)
                for gi, (f0, fw) in enumerate(fgs):
                    if gi + 1 < len(fgs):
                        load_w13(gi + 1)
                    wt = w13.bufs[gi % 2]
                    for fi in range(fw // 128):
                        fc = f0 // 128 + fi
                        p1 = psr.next()
                        p3 = psr.next()
                        for dc in range(8):
                            k.op(k.pe, lambda: nc.tensor.matmul(p1.ap, lhsT=wt.ap[:, 0, dc, fi * 128:(fi + 1) * 128], rhs=uc.ap[:, dc, :], start=(dc == 0), stop=(dc == 7)),
                                 reads=[wt, uc], writes=[p1], signal=(dc == 7))
                        for dc in range(8):
                            k.op(k.pe, lambda: nc.tensor.matmul(p3.ap, lhsT=wt.ap[:, 1, dc, fi * 128:(fi + 1) * 128], rhs=uc.ap[:, dc, :], start=(dc == 0), stop=(dc == 7)),
                                 reads=[wt, uc], writes=[p3], signal=(dc == 7))
                        sb_ = sil.next()
                        k.op(k.act, lambda: nc.scalar.activation(out=sb_.ap, in_=p1.ap, func=AF.Silu), reads=[p1], writes=[sb_])
                        k.op(k.dve, lambda: nc.vector.tensor_tensor(out=gT.ap[:, fc, :], in0=sb_.ap, in1=p3.ap, op=ALU.mult), reads=[sb_, p3], writes=[gT])
                for dc in range(8):
                    pb = psr.next()
                    for fc in range(22):
                        k.op(k.pe, lambda: nc.tensor.matmul(pb.ap, lhsT=w2.ap[:, fc, dc * 128:(dc + 1) * 128], rhs=gT.ap[:, fc, :], start=(fc == 0), stop=(fc == 21)),
                             reads=[w2, gT], writes=[pb], signal=(fc == 21))
                    k.op(k.dve, lambda: nc.vector.tensor_tensor(out=hc.ap[:, dc, :], in0=pb.ap, in1=hc.ap[:, dc, :], op=ALU.add), reads=[pb, hc], writes=[hc])
            if not last:
                k.dma(k.sp, hst_ds[c % 2], hT_v[:, :, cols], hc.ap, reads=[hc], writes=[self.hT[0]])
                un = uns.next()
                self.rmsnorm_fm(hc, 8, TC, lambda dc: self.g_attn(l + 1, dc), un, lambda dc: un.ap[:, dc, :], sqr, ssps, lnb, rstd, float(D),
                                src_ap_fn=lambda dc: hc.ap[:, dc, :])
                k.dma(k.sp, un_ds[c % 2], uT_v[:, :, cols], un.ap, reads=[un], writes=[self.uT])
            else:
                for dc in range(8):
                    sq = sqr.next()
                    k.op(k.act, lambda: nc.scalar.activation(out=sq.ap, in_=hc.ap[:, dc, :], func=AF.Square), reads=[hc], writes=[sq])
                    k.op(k.pe, lambda: nc.tensor.matmul(ssps.ap, lhsT=self.ones_f.ap, rhs=sq.ap, start=(dc == 0), stop=(dc == 7)),
                         reads=[sq, self.ones_f], writes=[ssps])
                k.op(k.act, lambda: nc.scalar.activation(out=lnb.ap, in_=ssps.ap, func=AF.Ln, scale=1.0 / D, bias=self.epsb.ap), reads=[ssps, self.epsb], writes=[lnb])
                k.op(k.act, lambda: nc.scalar.activation(out=rstd.ap, in_=lnb.ap, func=AF.Exp, scale=-0.5), reads=[lnb], writes=[rstd])
                for dc in range(8):
                    k.op(k.dve, lambda: nc.vector.scalar_tensor_tensor(out=hc.ap[:, dc, :], in0=hc.ap[:, dc, :], scalar=self.g_final(dc), in1=rstd.ap, op0=ALU.mult, op1=ALU.mult),
                         reads=[hc, rstd, self.small], writes=[hc])
                for i in range(4):
                    ob = orow.next()
                    for half in range(2):
                        pb = psr.next()
                        for j in range(4):
                            dc = half * 4 + j
                            k.op(k.pe, lambda: nc.tensor.transpose(pb.ap[:, j * 128:(j + 1) * 128], hc.ap[:, dc, i * 128:(i + 1) * 128], self.ident_f.ap),
                                 reads=[hc, self.ident_f], writes=[pb], signal=(j == 3))
                        if half == 0:
                            k.op(k.act, lambda: nc.scalar.copy(out=ob.ap[:, 0:512], in_=pb.ap), reads=[pb], writes=[ob])
                        else:
                            k.op(k.dve, lambda: nc.vector.tensor_copy(out=ob.ap[:, 512:1024], in_=pb.ap), reads=[pb], writes=[ob])
                    k.dma(k.sp, or_ds[(orow.i - 1) % 2], self.out[c * TC + i * 128:c * TC + (i + 1) * 128, :], ob.ap, reads=[ob])

    def attn_core(self, c, lhs_fn, rhs_fn, rbufs, v_fn, vbuf, E, scale, st_ring, pt_ring, acc, tri=True, mask_fn=None, maskbuf=None, mask_eng=None):
        nc, k = self.nc, self.k
        nk = 4 * c + 4

        def emit_st(j):
            r = j - 4 * c
            off = 128 * max(r, 0)
            n = 512 - off
            stb = st_ring.next()
            ls = lhs_fn(j)
            rs = rhs_fn(off, n)
            nl_ = len(ls)
            for i in range(nl_):
                k.op(k.pe, lambda: nc.tensor.matmul(stb.ap[:, off:512], lhsT=ls[i], rhs=rs[i], start=(i == 0), stop=(i == nl_ - 1)),
                     reads=rbufs, writes=[stb], signal=(i == nl_ - 1))
            return stb, r, off, n

        nxt = emit_st(0)
        for j in range(nk):
            stb, r, off, n = nxt
            if j + 1 < nk:
                nxt = emit_st(j + 1)
            pt = pt_ring.next()
            k.op(k.act, lambda: nc.scalar.activation(out=pt.ap[:, off:512], in_=stb.ap[:, off:512], func=AF.Exp, scale=scale), reads=[stb], writes=[pt])
            if r >= 0 and tri:
                k.op(k.pool, lambda: nc.gpsimd.tensor_tensor(out=pt.ap[:, off:off + 128], in0=pt.ap[:, off:off + 128], in1=self.tri.ap, op=ALU.mult),
                     reads=[pt, self.tri], writes=[pt])
            if mask_fn is not None:
                me = mask_eng(j)
                mm = mask_fn(j, off, n)
                k.op(me, lambda: me.eng.tensor_tensor(out=pt.ap[:, off:512], in0=pt.ap[:, off:512], in1=mm, op=ALU.mult), reads=[pt, maskbuf], writes=[pt])
            for qt in range(max(r, 0), 4):
                ab = acc[qt // 2]
                o0 = (qt % 2) * 256
                k.op(k.pe, lambda: nc.tensor.matmul(ab.ap[:, o0:o0 + E + 1], lhsT=pt.ap[:, qt * 128:(qt + 1) * 128], rhs=v_fn(j), start=(j == 0 and qt % 2 == 0), stop=(j == 4 * c + qt), skip_group_check=True),
                     reads=[pt, vbuf], writes=[ab], signal=(qt == 3))

    def rope_fm(self, pa, np_, cs, cols, dst, dst_ap, xbr, rar, rbr, pb):
        nc, k = self.nc, self.k
        import os as _os
        lvl = _os.environ.get("KROPE", "z")
        xb = xbr.next()
        ra = rar.next()
        rb = rbr.next()
        k.op(k.act, lambda: nc.scalar.copy(out=xb.ap[0:np_, :], in_=pa.ap[0:np_, :]), reads=[pa], writes=[xb])
        if lvl >= "b":
            k.op(k.pe, lambda: nc.tensor.matmul(pb.ap[0:np_, :], lhsT=self.rperm.ap[0:np_, 0:np_], rhs=xb.ap[0:np_, :], start=True, stop=True), reads=[xb, self.rperm], writes=[pb])
        if lvl >= "c":
            k.op(k.dve, lambda: nc.vector.tensor_tensor(out=ra.ap[0:np_, :], in0=pa.ap[0:np_, :], in1=cs.ap[0:np_, 0, cols], op=ALU.mult), reads=[pa, cs], writes=[ra])
        if lvl >= "d":
            k.op(k.dve, lambda: nc.vector.tensor_tensor(out=rb.ap[0:np_, :], in0=pb.ap[0:np_, :], in1=cs.ap[0:np_, 1, cols], op=ALU.mult), reads=[pb, cs], writes=[rb])
        if lvl >= "e":
            k.op(k.pool, lambda: nc.gpsimd.tensor_tensor(out=dst_ap, in0=ra.ap[0:np_, :], in1=rb.ap[0:np_, :], op=ALU.add), reads=[ra, rb], writes=[dst])
        else:
            k.op(k.act, lambda: nc.scalar.copy(out=dst_ap, in_=pa.ap[0:np_, :]), reads=[pa], writes=[dst])

    def load_full(self, st, name, dbuf, shape_inner, dt, nsplit=8):
        k = self.k
        t = self.sb(st, name, [128] + shape_inner, dt)
        v = dbuf.ap.rearrange("(c p) t -> p c t", p=128)
        g = Tok()
        ds = k.dsem()
        w = S // nsplit
        for i in range(nsplit):
            k.dma(k.sp, ds, t.ap[:, :, i * w:(i + 1) * w], v[:, :, i * w:(i + 1) * w], reads=[dbuf], writes=[t], group=g)
        return t

    def evac_norm(self, acc, E, dst, dst_ap_fn, recr):
        nc, k = self.nc, self.k
        for qt in range(4):
            rec = recr.next()
            ab = acc[qt // 2]
            o0 = (qt % 2) * 256
            k.op(k.dve, lambda: nc.vector.reciprocal(out=rec.ap, in_=ab.ap[:, o0 + E:o0 + E + 1]), reads=[ab], writes=[rec])
            k.op(k.dve, lambda: nc.vector.tensor_scalar(out=dst_ap_fn(qt), in0=ab.ap[:, o0:o0 + E], scalar1=rec.ap, scalar2=None, op0=ALU.mult), reads=[ab, rec], writes=[dst])

    def transpose_out(self, src, src_ap_fn, pbuf, dst, dst_ap, eng):
        nc, k = self.nc, self.k
        for qt in range(4):
            k.op(k.pe, lambda: nc.tensor.transpose(pbuf.ap[:, qt * 128:(qt + 1) * 128], src_ap_fn(qt), self.ident_f.ap), reads=[src, self.ident_f], writes=[pbuf], signal=(qt == 3))
        if eng is k.act:
            k.op(eng, lambda: nc.scalar.copy(out=dst_ap, in_=pbuf.ap), reads=[pbuf], writes=[dst])
        else:
            k.op(eng, lambda: nc.vector.tensor_copy(out=dst_ap, in_=pbuf.ap), reads=[pbuf], writes=[dst])

    def diff_phase(self, st, l):
        nc, k = self.nc, self.k
        jj = l // 3
        uT = self.load_full(st, "uTf", self.uT, [8, S], BF16)
        cs = self.sb(st, "cs", [128, 2, S], F32)
        k.dma(k.sp, k.dsem(), cs.ap, self.cs_d.ap, reads=[self.cs_d], writes=[cs])
        wqkv = self.wb["wqkv%d" % l]
        wv = wqkv.ap.rearrange("(c p) f -> p c f", p=128)
        whs = Ring([self.sb(st, "wh%d" % i, [128, 8, 384], BF16) for i in range(2)])
        wh_ds = [k.dsem(), k.dsem()]
        qTs = Ring([self.sb(st, "qT%d" % i, [128, S], BF16) for i in range(2)])
        kTs = Ring([self.sb(st, "kT%d" % i, [128, S], BF16) for i in range(2)])
        Vs = Ring([self.sb(st, "V%d" % i, [128, 32, 130], BF16) for i in range(2)])
        for vb in Vs.bufs:
            k.op(k.dve, lambda: nc.vector.memset(vb.ap[:, :, 128:130], 1.0), writes=[vb])
        xbr = Ring([self.sb(st, "xb%d" % i, [128, TC], BF16) for i in range(2)])
        rar = Ring([self.sb(st, "ra%d" % i, [128, TC], F32) for i in range(2)])
        rbr = Ring([self.sb(st, "rb%d" % i, [128, TC], F32) for i in range(2)])
        pts = Ring([self.sb(st, "pt%d" % i, [128, TC], BF16) for i in range(4)])
        recr = Ring([self.sb(st, "rec%d" % i, [128, 1], F32) for i in range(8)])
        om = [self.sb(st, "om%d" % i, [128, 4, 128], F32) for i in range(2)]
        d4 = self.sb(st, "d4", [128, 4, 128], F32)
        sq4 = self.sb(st, "sq4", [128, 4, 128], F32)
        ss4 = self.sb(st, "ss4", [128, 4], F32)
        ln4 = self.sb(st, "ln4", [128, 4], F32)
        rs4 = self.sb(st, "rs4", [128, 4], F32)
        dn = Ring([self.sb(st, "dn%d" % i, [128, 4, 128], F32) for i in range(2)])
        oTh = Ring([self.sb(st, "oTh%d" % i, [128, S], BF16) for i in range(2)])
        oth_ds = [k.dsem(), k.dsem()]
        st_ring = Ring(self.ps[0:2])
        accs = Ring([self.ps[2:4], self.ps[4:6]])
        misc = Ring(self.ps[6:8])
        sub_ap = self.subln.ap[:, jj * 128:(jj + 1) * 128].unsqueeze(1).broadcast_to([128, 4, 128])
        scale = 64 ** -0.5
        import os as _os
        for h in range(int(_os.environ.get("KHEADS", "8"))):
            wh = whs.next()
            g = Tok()
            for i in range(3):
                k.dma(k.sp, wh_ds[h % 2], wh.ap[:, :, i * 128:(i + 1) * 128], wv[:, :, i * 1024 + h * 128:i * 1024 + (h + 1) * 128], reads=[wqkv], writes=[wh], group=g)
            qT, kT, V = qTs.next(), kTs.next(), Vs.next()
            import os as _os
            stage = float(_os.environ.get("KDIFF_STAGE", "3"))
            for tc in range(NCH):
                if stage < 0.5:
                    break
                if _os.environ.get("KTC1") and tc >= int(_os.environ.get("KTC1")):
                    break
                cols = slice(tc * TC, (tc + 1) * TC)
                for which, dstb in ((0, qT), (1, kT)):
                    if _os.environ.get("KSKIPQK"):
                        break
                    pa = misc.next()
                    for dc in range(8):
                        k.op(k.pe, lambda: nc.tensor.matmul(pa.ap, lhsT=wh.ap[:, dc, which * 128:(which + 1) * 128], rhs=uT.ap[:, dc, cols], start=(dc == 0), stop=(dc == 7)),
                             reads=[wh, uT], writes=[pa], signal=(dc == 7))
                    pb = misc.next()
                    if stage < 0.7:
                        k.op(k.act, lambda: nc.scalar.copy(out=dstb.ap[:, cols], in_=pa.ap), reads=[pa], writes=[dstb])
                        continue
                    self.rope_fm(pa, 128, cs, cols, dstb, dstb.ap[:, cols], xbr, rar, rbr, pb)
                if _os.environ.get("KSKIPV"):
                    continue
                pa = misc.next()
                for i in range(4):
                    for dc in range(8):
                        k.op(k.pe, lambda: nc.tensor.matmul(pa.ap[:, i * 128:(i + 1) * 128], lhsT=uT.ap[:, dc, tc * TC + i * 128:tc * TC + (i + 1) * 128], rhs=wh.ap[:, dc, 256:384], start=(dc == 0), stop=(dc == 7)),
                             reads=[wh, uT], writes=[pa], signal=(dc == 7 and i == 3))
                k.op(k.act, lambda: nc.scalar.copy(out=V.ap[:, tc * 4:tc * 4 + 4, 0:128], in_=pa.ap.rearrange("p (a b) -> p a b", a=4)), reads=[pa], writes=[V])
            oT_h = oTh.next()
            if stage < 3:
                k.op(k.dve, lambda: nc.vector.memset(oT_h.ap, 0.0), writes=[oT_h])
            for c in range(NCH):
                if stage < 2:
                    break
                for m in range(2):
                    acc = accs.next()
                    self.attn_core(c,
                                   lambda j: [kT.ap[m * 64:(m + 1) * 64, j * 128:(j + 1) * 128]],
                                   lambda off, n: [qT.ap[m * 64:(m + 1) * 64, c * TC + off:c * TC + off + n]],
                                   [kT, qT], lambda j: V.ap[:, j, 0:129], V, 128, scale, st_ring, pts, acc)
                    self.evac_norm(acc, 128, om[m], lambda qt: om[m].ap[:, qt, :], recr)
                if stage < 3:
                    continue
                k.op(k.dve, lambda: nc.vector.scalar_tensor_tensor(out=d4.ap, in0=om[1].ap, scalar=self.neglam.ap[:, jj:jj + 1], in1=om[0].ap, op0=ALU.mult, op1=ALU.add),
                     reads=[om[0], om[1], self.neglam], writes=[d4])
                k.op(k.dve, lambda: nc.vector.tensor_tensor(out=sq4.ap, in0=d4.ap, in1=d4.ap, op=ALU.mult), reads=[d4], writes=[sq4])
                k.op(k.dve, lambda: nc.vector.tensor_reduce(out=ss4.ap, in_=sq4.ap, axis=AX.X, op=ALU.add), reads=[sq4], writes=[ss4])
                k.op(k.act, lambda: nc.scalar.activation(out=ln4.ap, in_=ss4.ap, func=AF.Ln, scale=1.0 / 128.0, bias=self.epsb.ap), reads=[ss4, self.epsb], writes=[ln4])
                k.op(k.act, lambda: nc.scalar.activation(out=rs4.ap, in_=ln4.ap, func=AF.Exp, scale=-0.5), reads=[ln4], writes=[rs4])
                dnb = dn.next()
                sub2 = self.subln.ap[:, jj * 128:(jj + 1) * 128]
                for qt in range(4):
                    k.op(k.dve, lambda: nc.vector.scalar_tensor_tensor(out=dnb.ap[:, qt, :], in0=d4.ap[:, qt, :], scalar=rs4.ap[:, qt:qt + 1], in1=sub2, op0=ALU.mult, op1=ALU.mult),
                         reads=[d4, rs4, self.subln], writes=[dnb])
                self.transpose_out(dnb, lambda qt: dnb.ap[:, qt, :], misc.next(), oT_h, oT_h.ap[:, c * TC:(c + 1) * TC], k.dve)
            k.dma(k.sp, oth_ds[h % 2], self.oT.ap[h * 128:(h + 1) * 128, :], oT_h.ap, reads=[oT_h], writes=[self.oT])

    def mla_phase(self, st, l):
        nc, k = self.nc, self.k
        wd, wuq, wukv = self.wb["wdown%d" % l], self.wb["wuq%d" % l], self.wb["wukv%d" % l]
        cs = self.sb(st, "cs", [128, 2, S], F32)
        k.dma(k.sp, k.dsem(), cs.ap, self.cs_d.ap, reads=[self.cs_d], writes=[cs])
        wdsb = self.sb(st, "wdsb", [128, 8, 704], BF16)
        k.dma(k.sp, k.dsem(), wdsb.ap, wd.ap.rearrange("(c p) f -> p c f", p=128), reads=[wd], writes=[wdsb])
        cqn = self.sb(st, "cqn", [128, 3, S], BF16)
        ckvn = self.sb(st, "ckvn", [128, 2, S], BF16)
        krT = self.sb(st, "krT", [128, S], BF16)
        ucs = Ring([self.sb(st, "uc%d" % i, [128, 8, TC], BF16) for i in range(2)])
        u_ds = [k.dsem(), k.dsem()]
        sqr = Ring([self.sb(st, "sq%d" % i, [128, TC], F32) for i in range(2)])
        lnb = self.sb(st, "lnb", [128, TC], F32)
        rstd = self.sb(st, "rstd", [128, TC], F32)
        xbr = Ring([self.sb(st, "xb%d" % i, [128, TC], BF16) for i in range(2)])
        rar = Ring([self.sb(st, "ra%d" % i, [128, TC], F32) for i in range(2)])
        rbr = Ring([self.sb(st, "rb%d" % i, [128, TC], F32) for i in range(2)])
        uT_v = self.uT.ap.rearrange("(c p) t -> p c t", p=128)
        ps = self.ps
        for tc in range(NCH):
            cols = slice(tc * TC, (tc + 1) * TC)
            uc = ucs.next()
            k.dma(k.sp, u_ds[tc % 2], uc.ap, uT_v[:, :, cols], reads=[self.uT], writes=[uc])
            for i in range(3):
                for dc in range(8):
                    k.op(k.pe, lambda: nc.tensor.matmul(ps[i].ap, lhsT=wdsb.ap[:, dc, i * 128:(i + 1) * 128], rhs=uc.ap[:, dc, :], start=(dc == 0), stop=(dc == 7)),
                         reads=[wdsb, uc], writes=[ps[i]], signal=(dc == 7))
            self.rmsnorm_fm(None, 3, TC, lambda i: self.small.ap[:, 72 + i:73 + i], cqn, lambda i: cqn.ap[:, i, cols], sqr, ps[7], lnb, rstd, 384.0,
                            src_ap_fn=lambda i: ps[i].ap, src_bufs=[ps[0], ps[1], ps[2]])
            for i in range(2):
                for dc in range(8):
                    k.op(k.pe, lambda: nc.tensor.matmul(ps[3 + i].ap, lhsT=wdsb.ap[:, dc, 384 + i * 128:384 + (i + 1) * 128], rhs=uc.ap[:, dc, :], start=(dc == 0), stop=(dc == 7)),
                         reads=[wdsb, uc], writes=[ps[3 + i]], signal=(dc == 7))
            self.rmsnorm_fm(None, 2, TC, lambda i: self.small.ap[:, 75 + i:76 + i], ckvn, lambda i: ckvn.ap[:, i, cols], sqr, ps[7], lnb, rstd, 256.0,
                            src_ap_fn=lambda i: ps[3 + i].ap, src_bufs=[ps[3], ps[4]])
            for dc in range(8):
                k.op(k.pe, lambda: nc.tensor.matmul(ps[5].ap[0:64, :], lhsT=wdsb.ap[:, dc, 640:704], rhs=uc.ap[:, dc, :], start=(dc == 0), stop=(dc == 7)),
                     reads=[wdsb, uc], writes=[ps[5]], signal=(dc == 7))
            self.rope_fm(ps[5], 64, cs, cols, krT, krT.ap[0:64, cols], xbr, rar, rbr, ps[6])
        wqh = Ring([self.sb(st, "wqh%d" % i, [128, 3, 192], BF16) for i in range(2)])
        wkvh = Ring([self.sb(st, "wkvh%d" % i, [128, 2, 256], BF16) for i in range(2)])
        wq_ds = [k.dsem(), k.dsem()]
        wuq_v = wuq.ap.rearrange("(c p) f -> p c f", p=128)
        wukv_v = wukv.ap.rearrange("(c p) f -> p c f", p=128)
        qnT = self.sb(st, "qnT", [128, S], BF16)
        qrT = self.sb(st, "qrT", [128, S], BF16)
        knT = self.sb(st, "knT", [128, S], BF16)
        V = self.sb(st, "V", [128, 32, 130], BF16)
        k.op(k.dve, lambda: nc.vector.memset(V.ap[:, :, 128:130], 1.0), writes=[V])
        pts = Ring([self.sb(st, "pt%d" % i, [128, TC], BF16) for i in range(4)])
        recr = Ring([self.sb(st, "rec%d" % i, [128, 1], F32) for i in range(8)])
        on = Ring([self.sb(st, "on%d" % i, [128, 4, 128], F32) for i in range(2)])
        oTh = Ring([self.sb(st, "oTh%d" % i, [128, S], BF16) for i in range(2)])
        oth_ds = [k.dsem(), k.dsem()]
        st_ring = Ring(ps[0:2])
        accs = Ring([ps[2:4], ps[4:6]])
        misc = Ring(ps[6:8])
        scale = 192 ** -0.5
        for h in range(8):
            wq, wkv = wqh.next(), wkvh.next()
            g = Tok()
            k.dma(k.sp, wq_ds[h % 2], wq.ap, wuq_v[:, :, h * 192:(h + 1) * 192], reads=[wuq], writes=[wq], group=g)
            k.dma(k.sp, wq_ds[h % 2], wkv.ap, wukv_v[:, :, h * 256:(h + 1) * 256], reads=[wukv], writes=[wkv], group=g)
            for tc in range(NCH):
                cols = slice(tc * TC, (tc + 1) * TC)
                pa = misc.next()
                for i in range(3):
                    k.op(k.pe, lambda: nc.tensor.matmul(pa.ap, lhsT=wq.ap[:, i, 0:128], rhs=cqn.ap[:, i, cols], start=(i == 0), stop=(i == 2)), reads=[wq, cqn], writes=[pa], signal=(i == 2))
                k.op(k.act, lambda: nc.scalar.copy(out=qnT.ap[:, cols], in_=pa.ap), reads=[pa], writes=[qnT])
                pa = misc.next()
                for i in range(3):
                    k.op(k.pe, lambda: nc.tensor.matmul(pa.ap[0:64, :], lhsT=wq.ap[:, i, 128:192], rhs=cqn.ap[:, i, cols], start=(i == 0), stop=(i == 2)), reads=[wq, cqn], writes=[pa], signal=(i == 2))
                pb = misc.next()
                self.rope_fm(pa, 64, cs, cols, qrT, qrT.ap[0:64, cols], xbr, rar, rbr, pb)
                pa = misc.next()
                for i in range(2):
                    k.op(k.pe, lambda: nc.tensor.matmul(pa.ap, lhsT=wkv.ap[:, i, 0:128], rhs=ckvn.ap[:, i, cols], start=(i == 0), stop=(i == 1)), reads=[wkv, ckvn], writes=[pa], signal=(i == 1))
                k.op(k.dve, lambda: nc.vector.tensor_copy(out=knT.ap[:, cols], in_=pa.ap), reads=[pa], writes=[knT])
                pa = misc.next()
                for t4 in range(4):
                    for i in range(2):
                        k.op(k.pe, lambda: nc.tensor.matmul(pa.ap[:, t4 * 128:(t4 + 1) * 128], lhsT=ckvn.ap[:, i, tc * TC + t4 * 128:tc * TC + (t4 + 1) * 128], rhs=wkv.ap[:, i, 128:256], start=(i == 0), stop=(i == 1)),
                             reads=[wkv, ckvn], writes=[pa], signal=(i == 1 and t4 == 3))
                k.op(k.act, lambda: nc.scalar.copy(out=V.ap[:, tc * 4:tc * 4 + 4, 0:128], in_=pa.ap.rearrange("p (a b) -> p a b", a=4)), reads=[pa], writes=[V])
            oT_h = oTh.next()
            for c in range(NCH):
                acc = accs.next()
                self.attn_core(c,
                               lambda j: [knT.ap[:, j * 128:(j + 1) * 128], krT.ap[0:64, j * 128:(j + 1) * 128]],
                               lambda off, n: [qnT.ap[:, c * TC + off:c * TC + off + n], qrT.ap[0:64, c * TC + off:c * TC + off + n]],
                               [knT, krT, qnT, qrT], lambda j: V.ap[:, j, 0:129], V, 128, scale, st_ring, pts, acc)
                onb = on.next()
                self.evac_norm(acc, 128, onb, lambda qt: onb.ap[:, qt, :], recr)
                self.transpose_out(onb, lambda qt: onb.ap[:, qt, :], misc.next(), oT_h, oT_h.ap[:, c * TC:(c + 1) * TC], k.dve)
            k.dma(k.sp, oth_ds[h % 2], self.oT.ap[h * 128:(h + 1) * 128, :], oT_h.ap, reads=[oT_h], writes=[self.oT])

    def dsa_phase(self, st, l):
        nc, k = self.nc, self.k
        win = self.wb["win%d" % l]
        wv_ = win.ap.rearrange("(c p) f -> p c f", p=128)
        Wq = self.sb(st, "Wq", [128, 8, 1024], BF16)
        Wkk = self.sb(st, "Wkk", [128, 8, 128], BF16)
        Wikk = self.sb(st, "Wikk", [128, 8, 128], BF16)
        Wv = self.sb(st, "Wv", [128, 8, 64], BF16)
        Wiq = self.sb(st, "Wiq", [128, 8, 512], BF16)
        Wiw = self.sb(st, "Wiw", [128, 8, 8], BF16)
        k.dma(k.sp, k.dsem(), Wq.ap, wv_[:, :, 0:1024], reads=[win], writes=[Wq])
        g = Tok(); dsx = k.dsem()
        k.dma(k.sp, dsx, Wkk.ap[:, :, 0:64], wv_[:, :, 1024:1088], reads=[win], writes=[Wkk], group=g)
        k.dma(k.sp, dsx, Wkk.ap[:, :, 64:128], wv_[:, :, 1024:1088], reads=[win], writes=[Wkk], group=g)
        k.dma(k.sp, dsx, Wikk.ap[:, :, 0:64], wv_[:, :, 1664:1728], reads=[win], writes=[Wikk], group=g)
        k.dma(k.sp, dsx, Wikk.ap[:, :, 64:128], wv_[:, :, 1664:1728], reads=[win], writes=[Wikk], group=g)
        k.dma(k.sp, dsx, Wv.ap, wv_[:, :, 1088:1152], reads=[win], writes=[Wv], group=g)
        k.dma(k.sp, dsx, Wiq.ap, wv_[:, :, 1152:1664], reads=[win], writes=[Wiq], group=g)
        k.dma(k.sp, dsx, Wiw.ap, wv_[:, :, 1728:1736], reads=[win], writes=[Wiw], group=g)
        KK = self.sb(st, "KK", [128, S], BF16)
        IKK = self.sb(st, "IKK", [128, S], BF16)
        Vaug = self.sb(st, "Vaug", [128, 32, 66], BF16)
        k.op(k.dve, lambda: nc.vector.memset(Vaug.ap[:, :, 64:66], 1.0), writes=[Vaug])
        ucs = Ring([self.sb(st, "uc%d" % i, [128, 8, TC], BF16) for i in range(1)])
        u_ds = [k.dsem()]
        csc = Ring([self.sb(st, "csc%d" % i, [128, 2, TC], F32) for i in range(2)])
        cs_ds = [k.dsem(), k.dsem()]
        xbr = Ring([self.sb(st, "xb%d" % i, [128, TC], BF16) for i in range(2)])
        rar = Ring([self.sb(st, "ra%d" % i, [128, TC], F32) for i in range(2)])
        rbr = Ring([self.sb(st, "rb%d" % i, [128, TC], F32) for i in range(2)])
        uT_v = self.uT.ap.rearrange("(c p) t -> p c t", p=128)
        ps = self.ps
        st_ring = Ring(ps[0:2])
        accs = Ring([ps[2:4], ps[4:6]])
        misc = Ring(ps[6:8])

        class CsView:
            pass

        def load_chunk(tc):
            cols = slice(tc * TC, (tc + 1) * TC)
            uc = ucs.next()
            k.dma(k.sp, u_ds[0], uc.ap, uT_v[:, :, cols], reads=[self.uT], writes=[uc])
            cc = csc.next()
            k.dma(k.sp, cs_ds[(csc.i - 1) % 2], cc.ap, self.cs_d.ap[:, :, cols], reads=[self.cs_d], writes=[cc])
            return uc, cc

        lcols = slice(0, TC)
        for tc in range(NCH):
            cols = slice(tc * TC, (tc + 1) * TC)
            uc, cc = load_chunk(tc)
            for W_, dstb in ((Wkk, KK), (Wikk, IKK)):
                pa = misc.next()
                for dc in range(8):
                    k.op(k.pe, lambda: nc.tensor.matmul(pa.ap, lhsT=W_.ap[:, dc, :], rhs=uc.ap[:, dc, :], start=(dc == 0), stop=(dc == 7)), reads=[W_, uc], writes=[pa], signal=(dc == 7))
                pb = misc.next()
                self.rope_fm(pa, 128, cc, lcols, dstb, dstb.ap[:, cols], xbr, rar, rbr, pb)
            pa = misc.next()
            for t4 in range(4):
                for dc in range(8):
                    k.op(k.pe, lambda: nc.tensor.matmul(pa.ap[:, t4 * 64:(t4 + 1) * 64], lhsT=uc.ap[:, dc, t4 * 128:(t4 + 1) * 128], rhs=Wv.ap[:, dc, :], start=(dc == 0), stop=(dc == 7)),
                         reads=[Wv, uc], writes=[pa], signal=(dc == 7 and t4 == 3))
            k.op(k.act, lambda: nc.scalar.copy(out=Vaug.ap[:, tc * 4:tc * 4 + 4, 0:64], in_=pa.ap[:, 0:256].rearrange("p (a b) -> p a b", a=4)), reads=[pa], writes=[Vaug])
        qTc = self.sb(st, "qTc", [128, 8, TC], BF16)
        iqTc = self.sb(st, "iqTc", [128, 4, TC], BF16)
        iwc = self.sb(st, "iwc", [128, 4, 8], F32)
        accr = Ring([self.sb(st, "sacc%d" % i, [128, S], F32) for i in range(2)])
        mb = self.sb(st, "mb", [128, S], F32)
        maskT = self.sb(st, "maskT", [128, 32, TC], BF16)
        rlr = Ring([self.sb(st, "rl%d" % i, [128, TC], F32) for i in range(3)])
        lo = self.sb(st, "lo", [128, 1], F32)
        mid = self.sb(st, "mid", [128, 1], F32)
        cnt = self.sb(st, "cnt", [128, 1], F32)
        tt = self.sb(st, "tt", [128, 1], F32)
        pts = Ring([self.sb(st, "pt%d" % i, [128, TC], BF16) for i in range(4)])
        recr = Ring([self.sb(st, "rec%d" % i, [128, 1], F32) for i in range(8)])
        onp = Ring([self.sb(st, "onp%d" % i, [128, 4, 128], F32) for i in range(2)])
        oTc = self.sb(st, "oTc", [128, 8, TC], BF16)
        oc_ds = k.dsem()
        oT_v = self.oT.ap.rearrange("(c p) t -> p c t", p=128)
        scale = 64 ** -0.5
        iw_scale = (64 ** -0.5) * (8 ** -0.5)
        NIT = 18
        for c in range(NCH):
            cols = slice(c * TC, (c + 1) * TC)
            uc, cc = load_chunk(c)
            for pair in range(8):
                pa = misc.next()
                for dc in range(8):
                    k.op(k.pe, lambda: nc.tensor.matmul(pa.ap, lhsT=Wq.ap[:, dc, pair * 128:(pair + 1) * 128], rhs=uc.ap[:, dc, :], start=(dc == 0), stop=(dc == 7)), reads=[Wq, uc], writes=[pa], signal=(dc == 7))
                pb = misc.next()
                self.rope_fm(pa, 128, cc, lcols, qTc, qTc.ap[:, pair, :], xbr, rar, rbr, pb)
            for pair in range(4):
                pa = misc.next()
                for dc in range(8):
                    k.op(k.pe, lambda: nc.tensor.matmul(pa.ap, lhsT=Wiq.ap[:, dc, pair * 128:(pair + 1) * 128], rhs=uc.ap[:, dc, :], start=(dc == 0), stop=(dc == 7)), reads=[Wiq, uc], writes=[pa], signal=(dc == 7))
                pb = misc.next()
                self.rope_fm(pa, 128, cc, lcols, iqTc, iqTc.ap[:, pair, :], xbr, rar, rbr, pb)
            pa = misc.next()
            for qt in range(4):
                for dc in range(8):
                    k.op(k.pe, lambda: nc.tensor.matmul(pa.ap[:, qt * 8:(qt + 1) * 8], lhsT=uc.ap[:, dc, qt * 128:(qt + 1) * 128], rhs=Wiw.ap[:, dc, :], start=(dc == 0), stop=(dc == 7)),
                         reads=[Wiw, uc], writes=[pa], signal=(dc == 7 and qt == 3))
            k.op(k.dve, lambda: nc.vector.tensor_scalar(out=iwc.ap, in0=pa.ap[:, 0:32].rearrange("p (a b) -> p a b", a=4), scalar1=iw_scale, scalar2=None, op0=ALU.mult), reads=[pa], writes=[iwc])
            for qt in range(4):
                L = c * TC + 128 * (qt + 1)
                sa = accr.next()
                for kc in range(c + 1):
                    n = min(TC, L - kc * TC)
                    for h in range(8):
                        b0 = 64 * (h % 2)
                        stb = st_ring.next()
                        k.op(k.pe, lambda: nc.tensor.matmul(stb.ap[:, 0:n], lhsT=iqTc.ap[b0:b0 + 64, h // 2, qt * 128:(qt + 1) * 128], rhs=IKK.ap[b0:b0 + 64, kc * TC:kc * TC + n], start=True, stop=True),
                             reads=[iqTc, IKK], writes=[stb])
                        rl = rlr.next()
                        k.op(k.act, lambda: nc.scalar.activation(out=rl.ap[:, 0:n], in_=stb.ap[:, 0:n], func=AF.Relu), reads=[stb], writes=[rl])
                        if h == 0:
                            k.op(k.dve, lambda: nc.vector.tensor_scalar(out=sa.ap[:, kc * TC:kc * TC + n], in0=rl.ap[:, 0:n], scalar1=iwc.ap[:, qt, 0:1], scalar2=None, op0=ALU.mult), reads=[rl, iwc], writes=[sa])
                        else:
                            k.op(k.dve, lambda: nc.vector.scalar_tensor_tensor(out=sa.ap[:, kc * TC:kc * TC + n], in0=rl.ap[:, 0:n], scalar=iwc.ap[:, qt, h:h + 1], in1=sa.ap[:, kc * TC:kc * TC + n], op0=ALU.mult, op1=ALU.add),
                                 reads=[rl, iwc, sa], writes=[sa])
                k.op(k.pool, lambda: nc.gpsimd.affine_select(out=sa.ap[:, L - 128:L], in_=sa.ap[:, L - 128:L], pattern=[[-1, 128]], compare_op=ALU.is_ge, fill=NEG, base=0, channel_multiplier=1),
                     reads=[sa], writes=[sa])
                k.op(k.dve, lambda: nc.vector.memset(lo.ap, -32.0), writes=[lo])
                k.op(k.dve, lambda: nc.vector.memset(mid.ap, 0.0), writes=[mid])
                w = 64.0
                for it in range(NIT):
                    k.op(k.dve, lambda: nc.vector.tensor_scalar(out=mb.ap[:, 0:L], in0=sa.ap[:, 0:L], scalar1=mid.ap, scalar2=None, op0=ALU.is_ge, op1=ALU.add, accum_out=cnt.ap), reads=[sa, mid], writes=[mb, cnt])
                    k.op(k.dve, lambda: nc.vector.tensor_scalar(out=tt.ap, in0=cnt.ap, scalar1=256.0, scalar2=w / 2, op0=ALU.is_ge, op1=ALU.mult), reads=[cnt], writes=[tt])
                    k.op(k.dve, lambda: nc.vector.tensor_tensor(out=lo.ap, in0=lo.ap, in1=tt.ap, op=ALU.add), reads=[lo, tt], writes=[lo])
                    k.op(k.dve, lambda: nc.vector.tensor_scalar(out=mid.ap, in0=lo.ap, scalar1=w / 4, scalar2=None, op0=ALU.add), reads=[lo], writes=[mid])
                    w = w / 2
                k.op(k.dve, lambda: nc.vector.tensor_scalar(out=mb.ap[:, 0:L], in0=sa.ap[:, 0:L], scalar1=lo.ap, scalar2=None, op0=ALU.is_ge), reads=[sa, lo], writes=[mb])
                nkt = L // 128
                for k0 in range(0, nkt, 4):
                    nb = min(4, nkt - k0)
                    pbk = misc.next()
                    for i in range(nb):
                        k.op(k.pe, lambda: nc.tensor.transpose(pbk.ap[:, i * 128:(i + 1) * 128], mb.ap[:, (k0 + i) * 128:(k0 + i + 1) * 128], self.ident_f.ap), reads=[mb, self.ident_f], writes=[pbk], signal=(i == nb - 1))
                    k.op(k.act, lambda: nc.scalar.copy(out=maskT.ap[:, k0:k0 + nb, qt * 128:(qt + 1) * 128], in_=pbk.ap[:, 0:nb * 128].rearrange("p (a b) -> p a b", a=nb)), reads=[pbk], writes=[maskT])
            for h in range(16):
                b0 = 64 * (h % 2)
                acc = accs.next()
                self.attn_core(c,
                               lambda j: [KK.ap[b0:b0 + 64, j * 128:(j + 1) * 128]],
                               lambda off, n: [qTc.ap[b0:b0 + 64, h // 2, off:off + n]],
                               [KK, qTc], lambda j: Vaug.ap[:, j, 0:65], Vaug, 64, scale, st_ring, pts, acc, tri=False,
                               mask_fn=lambda j, off, n: maskT.ap[:, j, off:off + n], maskbuf=maskT, mask_eng=lambda j: (k.pool if j % 3 != 2 else k.dve))
                if h % 2 == 0:
                    onb = onp.next()
                self.evac_norm(acc, 64, onb, lambda qt: onb.ap[:, qt, b0:b0 + 64], recr)
                if h % 2 == 1:
                    self.transpose_out(onb, lambda qt: onb.ap[:, qt, :], misc.next(), oTc, oTc.ap[:, h // 2, :], k.act)
            k.dma(k.sp, oc_ds, oT_v[:, :, cols], oTc.ap, reads=[oTc], writes=[self.oT])


_PROG_CACHE = {}


def _get_prog(n_layers=DEPTH):
    if n_layers not in _PROG_CACHE:
        _PROG_CACHE[n_layers] = Prog(n_layers)
    return _PROG_CACHE[n_layers]


def _host_inputs(inputs):
    f = lambda a: np.ascontiguousarray(np.asarray(a, dtype=np.float32))
    an = f(inputs["attn_norm"]).reshape(4, 8, 128).transpose(2, 0, 1).reshape(128, 32)
    fn = f(inputs["ffn_norm"]).reshape(4, 8, 128).transpose(2, 0, 1).reshape(128, 32)
    fin = f(inputs["final_norm"]).reshape(8, 128).T
    qn = f(inputs["mla_q_norm"]).reshape(3, 128).T
    kn = f(inputs["mla_kv_norm"]).reshape(2, 128).T
    small = f(np.concatenate([an, fn, fin, qn, kn], axis=1))
    lam = f(np.broadcast_to(f(inputs["diff_lambda"]).reshape(1, 512), (128, 512)))
    subln = f(np.broadcast_to(f(inputs["diff_subln"]).reshape(1, 256), (128, 256)))
    shared = {"small": small, "lam": lam, "subln": subln}
    for l in range(DEPTH):
        m, j = l % 3, l // 3
        if m == 0:
            shared["wqkv%d" % l] = f(inputs["diff_wqkv"][j])
            shared["wo%d" % l] = f(inputs["diff_wo"][j])
        elif m == 1:
            shared["win%d" % l] = f(inputs["dsa_win"][j])
            shared["wo%d" % l] = f(inputs["dsa_wo"][j])
        else:
            shared["wdown%d" % l] = f(inputs["mla_wdown"][j])
            shared["wuq%d" % l] = f(inputs["mla_wuq"][j])
            shared["wukv%d" % l] = f(inputs["mla_wukv"][j])
            shared["wo%d" % l] = f(inputs["mla_wo"][j])
        shared["w1_%d" % l] = f(inputs["ffn_w1"][l])
        shared["w3_%d" % l] = f(inputs["ffn_w3"][l])
        shared["w2_%d" % l] = f(inputs["ffn_w2"][l])
    return shared


def kernel(**inputs):
    x = np.asarray(inputs["x"], dtype=np.float32)
    B = x.shape[0]
    shared = _host_inputs(inputs)
    prog = _get_prog(DEPTH)
    in_maps = []
    for b in range(B):
        m = dict(shared)
        m["x"] = np.ascontiguousarray(x[b])
        in_maps.append(m)
    res = run_bass_kernel_spmd(prog.nc, in_maps, core_ids=list(range(B)))
    return np.stack([np.asarray(r["out"], dtype=np.float32) for r in res.results], axis=0)
```

```python
import math
from contextlib import ExitStack
import numpy as np
import concourse.bass as bass
import concourse.mybir as mybir
from concourse.bass_utils import run_bass_kernel_spmd

F32 = mybir.dt.float32
BF16 = mybir.dt.bfloat16
I32 = mybir.dt.int32
ALU = mybir.AluOpType
AF = mybir.ActivationFunctionType
AX = mybir.AxisListType

S = 4096
D = 1024
DFF = 2816
NCH = 8
TC = 512
EPS = 1e-6
DEPTH = 4
NEG = -1.0e30


class Sem:
    def __init__(self, nc, name):
        self.h = nc.alloc_semaphore(name)
        self.count = 0


class Tok:
    __slots__ = ("sem", "val")

    def __init__(self, sem=None, val=0):
        self.sem = sem
        self.val = val


class Buf:
    __slots__ = ("ap", "w", "r", "excl")

    def __init__(self, ap, excl=False):
        self.ap = ap
        self.w = None
        self.r = {}
        self.excl = excl


class Eng:
    def __init__(self, k, eng, name, is_pe=False):
        self.eng = eng
        self.sem = Sem(k.nc, "e_" + name)
        self.seen = {}
        self.is_pe = is_pe
        self.name = name

    def wait(self, sem, val):
        if val <= self.seen.get(sem, 0):
            return
        self.eng.wait_ge(sem.h, val)
        self.seen[sem] = val


class K:
    def __init__(self, nc):
        self.nc = nc
        self.pe = Eng(self, nc.tensor, "pe", True)
        self.act = Eng(self, nc.scalar, "act")
        self.dve = Eng(self, nc.vector, "dve")
        self.pool = Eng(self, nc.gpsimd, "pool")
        self.sp = Eng(self, nc.sync, "sp")
        self.engs = [self.pe, self.act, self.dve, self.pool, self.sp]
        self.dsems = []
        self.fixed = []
        self.nsem = 0

    def dsem(self, name=None):
        if self.nsem < len(self.dsems):
            s = self.dsems[self.nsem]
        else:
            s = Sem(self.nc, "d%d" % self.nsem)
            self.dsems.append(s)
        self.nsem += 1
        return s

    def _w(self, e, tok):
        if e.is_pe and tok.sem is e.sem:
            return
        e.wait(tok.sem, tok.val)

    def _deps(self, e, reads, writes):
        for b in reads:
            if b.w is not None:
                self._w(e, b.w)
            if b.excl:
                for t in b.r.values():
                    if t.sem is not e.sem:
                        self._w(e, t)
        for b in writes:
            if b.w is not None:
                self._w(e, b.w)
            for t in b.r.values():
                self._w(e, t)

    def _upd(self, tok, reads, writes):
        for b in writes:
            b.w = tok
            b.r = {}
        for b in reads:
            if b not in writes:
                o = b.r.get(tok.sem)
                if o is None or o.val < tok.val or o is tok:
                    b.r[tok.sem] = tok

    def op(self, e, fn, reads=(), writes=(), signal=True):
        self._deps(e, reads, writes)
        inst = fn()
        if signal:
            e.sem.count += 1
            inst.then_inc(e.sem.h, 1)
            tok = Tok(e.sem, e.sem.count)
        else:
            tok = Tok(e.sem, e.sem.count + 1)
        self._upd(tok, reads, writes)
        return inst

    def dma(self, q, dsem, out_ap, in_ap, reads=(), writes=(), group=None, **kw):
        self._deps(q, reads, writes)
        if group is None or group.val == 0:
            q.wait(dsem, dsem.count)
        inst = q.eng.dma_start(out=out_ap, in_=in_ap, **kw)
        dsem.count += 16
        inst.then_inc(dsem.h, 16)
        if group is not None:
            group.sem = dsem
            group.val = dsem.count
            tok = group
        else:
            tok = Tok(dsem, dsem.count)
        self._upd(tok, reads, writes)
        return inst

    def barrier(self):
        self.nsem = 0
        for e in self.engs:
            for o in self.engs[:4]:
                if o is not e and o.sem.count > 0:
                    e.wait(o.sem, o.sem.count)
            for d in self.dsems + self.fixed:
                if d.count > 0:
                    e.wait(d, d.count)


class Ring:
    def __init__(self, bufs):
        self.bufs = bufs
        self.i = 0

    def next(self):
        b = self.bufs[self.i % len(self.bufs)]
        self.i += 1
        return b


class Prog:
    def __init__(self, n_layers=DEPTH):
        self.n_layers = n_layers
        nc = bass.Bass("TRN2", target_bir_lowering=False)
        self.nc = nc
        self.k = K(nc)
        self.build()

    def sb(self, st, name, shape, dt):
        self.uid = getattr(self, "uid", 0) + 1
        t = st.enter_context(self.nc.sbuf_tensor("s%d_%s" % (self.uid, name), list(shape), dt))
        return Buf(t[:])

    def din(self, name, shape, dt=F32):
        return self.nc.dram_tensor(name, list(shape), dt, kind="ExternalInput").ap()

    def dscr(self, name, shape, dt):
        import os as _os
        if _os.environ.get("KDEBUG_OUT") and name in ("uT", "cs_d", "oT", "hT"):
            return Buf(self.nc.dram_tensor(name, list(shape), dt, kind="ExternalOutput").ap())
        return Buf(self.nc.dram_tensor(name, list(shape), dt).ap())

    def build(self):
        nc, k = self.nc, self.k
        self.x = self.din("x", [S, D])
        self.out = nc.dram_tensor("out", [S, D], F32, kind="ExternalOutput").ap()
        self.p_small = self.din("small", [128, 32 + 32 + 8 + 3 + 2])
        self.p_lam = self.din("lam", [128, 512])
        self.p_subln = self.din("subln", [128, 256])
        self.wspec = {}
        wl = []
        for l in range(DEPTH):
            m = l % 3
            if m == 0:
                wl.append([("wqkv%d" % l, D, 3072), ("wo%d" % l, D, D)])
            elif m == 1:
                wl.append([("win%d" % l, D, 1736), ("wo%d" % l, D, D)])
            else:
                wl.append([("wdown%d" % l, D, 704), ("wuq%d" % l, 384, 1536), ("wukv%d" % l, 256, 2048), ("wo%d" % l, D, D)])
            wl[-1] += [("w1_%d" % l, D, DFF), ("w3_%d" % l, D, DFF), ("w2_%d" % l, DFF, D)]
        self.wf = {}
        self.wb = {}
        for l in range(DEPTH):
            for (n, kk, nn) in wl[l]:
                self.wf[n] = self.din(n, [kk, nn])
                self.wb[n] = self.dscr(n + "_b", [kk, nn], BF16)
        self.wl = wl
        self.hT = [self.dscr("hT", [D, S], F32)]
        self.uT = self.dscr("uT", [D, S], BF16)
        self.oT = self.dscr("oT", [D, S], BF16)
        self.cs_d = self.dscr("cs_d", [128, 2, S], F32)

        with ExitStack() as st0:
            self.setup_persistent(st0)
            self.cast_weights()
            with ExitStack() as st:
                self.rope_tables(st)
            k.barrier()
            with ExitStack() as st:
                self.post_phase(st, -1)
            for l in range(self.n_layers):
                k.barrier()
                m = l % 3
                import os as _os
                with ExitStack() as st:
                    if _os.environ.get("KSKIP_ATTN"):
                        z = self.sb(st, "zz", [128, S], BF16)
                        k.op(k.dve, lambda: nc.vector.memset(z.ap, 0.0), writes=[z])
                        for i in range(8):
                            k.dma(k.sp, k.dsem(), self.oT.ap[i * 128:(i + 1) * 128, :], z.ap, reads=[z], writes=[self.oT])
                    elif m == 0:
                        self.diff_phase(st, l)
                    elif m == 1:
                        self.dsa_phase(st, l)
                    else:
                        self.mla_phase(st, l)
                k.barrier()
                with ExitStack() as st:
                    if not _os.environ.get("KSKIP_POST"):
                        self.post_phase(st, l)
            k.barrier()

    def setup_persistent(self, st):
        nc, k = self.nc, self.k
        self.ps = [Buf(nc.alloc_psum_tensor("ps%d" % i, [128, 512], F32)[:], excl=True) for i in range(8)]
        self.ident_f = self.sb(st, "ident_f", [128, 128], F32)
        self.ident_b = self.sb(st, "ident_b", [128, 128], BF16)
        self.ones_f = self.sb(st, "ones_f", [128, 128], F32)
        self.tri = self.sb(st, "tri", [128, 128], BF16)
        self.rperm = self.sb(st, "rperm", [128, 128], BF16)
        self.small = self.sb(st, "small", [128, 77], F32)
        self.lam = self.sb(st, "lamsb", [128, 512], F32)
        self.subln = self.sb(st, "sublnsb", [128, 256], F32)
        self.neglam = self.sb(st, "neglam", [128, 2], F32)
        self.epsb = self.sb(st, "epsb", [128, 1], F32)
        k.op(k.dve, lambda: nc.vector.memset(self.epsb.ap, EPS), writes=[self.epsb])
        k.dma(k.sp, k.dsem(), self.small.ap, self.p_small, writes=[self.small])
        k.dma(k.sp, k.dsem(), self.lam.ap, self.p_lam, writes=[self.lam])
        k.dma(k.sp, k.dsem(), self.subln.ap, self.p_subln, writes=[self.subln])
        with ExitStack() as t:
            io = self.sb(t, "io", [128, 128], I32)
            dif = self.sb(t, "dif", [128, 128], F32)
            ta = self.sb(t, "ta", [128, 128], F32)
            tb = self.sb(t, "tb", [128, 128], F32)
            co = self.sb(t, "co", [128, 128], F32)
            ioj = self.sb(t, "ioj", [128, 128], I32)
            k.op(k.pool, lambda: nc.gpsimd.iota(io.ap, pattern=[[1, 128]], base=0, channel_multiplier=-1), writes=[io])
            k.op(k.dve, lambda: nc.vector.tensor_copy(out=dif.ap, in_=io.ap), reads=[io], writes=[dif])
            k.op(k.dve, lambda: nc.vector.tensor_single_scalar(out=self.ident_f.ap, in_=dif.ap, scalar=0.0, op=ALU.is_equal), reads=[dif], writes=[self.ident_f])
            k.op(k.dve, lambda: nc.vector.tensor_copy(out=self.ident_b.ap, in_=self.ident_f.ap), reads=[self.ident_f], writes=[self.ident_b])
            k.op(k.dve, lambda: nc.vector.memset(self.ones_f.ap, 1.0), writes=[self.ones_f])
            k.op(k.dve, lambda: nc.vector.tensor_single_scalar(out=self.tri.ap, in_=dif.ap, scalar=0.0, op=ALU.is_ge), reads=[dif], writes=[self.tri])
            k.op(k.pool, lambda: nc.gpsimd.iota(ioj.ap, pattern=[[1, 128]], base=0, channel_multiplier=0), writes=[ioj])
            k.op(k.dve, lambda: nc.vector.tensor_single_scalar(out=ioj.ap, in_=ioj.ap, scalar=32, op=ALU.bitwise_and), reads=[ioj], writes=[ioj])
            k.op(k.dve, lambda: nc.vector.tensor_copy(out=co.ap, in_=ioj.ap), reads=[ioj], writes=[co])
            k.op(k.dve, lambda: nc.vector.tensor_single_scalar(out=co.ap, in_=co.ap, scalar=16.0, op=ALU.is_ge), reads=[co], writes=[co])
            k.op(k.dve, lambda: nc.vector.tensor_single_scalar(out=ta.ap, in_=dif.ap, scalar=32.0, op=ALU.is_equal), reads=[dif], writes=[ta])
            k.op(k.dve, lambda: nc.vector.tensor_single_scalar(out=tb.ap, in_=dif.ap, scalar=-32.0, op=ALU.is_equal), reads=[dif], writes=[tb])
            k.op(k.dve, lambda: nc.vector.tensor_tensor(out=ta.ap, in0=ta.ap, in1=co.ap, op=ALU.mult), reads=[ta, co], writes=[ta])
            k.op(k.dve, lambda: nc.vector.tensor_scalar(out=co.ap, in0=co.ap, scalar1=-1.0, scalar2=1.0, op0=ALU.mult, op1=ALU.add), reads=[co], writes=[co])
            k.op(k.dve, lambda: nc.vector.tensor_tensor(out=tb.ap, in0=tb.ap, in1=co.ap, op=ALU.mult), reads=[tb, co], writes=[tb])
            k.op(k.dve, lambda: nc.vector.tensor_tensor(out=self.rperm.ap, in0=ta.ap, in1=tb.ap, op=ALU.subtract), reads=[ta, tb], writes=[self.rperm])
            pr = self.sb(t, "lampr", [128, 256], F32)
            sm = self.sb(t, "lamsm", [128, 4], F32)
            for j in range(2):
                layer = 3 * j
                li = 0.8 - 0.6 * math.exp(-0.3 * layer)
                lm = self.lam.ap[:, j * 256:(j + 1) * 256]
                k.op(k.dve, lambda: nc.vector.tensor_tensor(out=pr.ap[:, 0:64], in0=lm[:, 0:64], in1=lm[:, 64:128], op=ALU.mult), reads=[self.lam], writes=[pr])
                k.op(k.dve, lambda: nc.vector.tensor_tensor(out=pr.ap[:, 64:128], in0=lm[:, 128:192], in1=lm[:, 192:256], op=ALU.mult), reads=[self.lam], writes=[pr])
                k.op(k.dve, lambda: nc.vector.tensor_reduce(out=sm.ap[:, 0:2], in_=pr.ap[:, 0:128].rearrange("p (a b) -> p a b", a=2), axis=AX.X, op=ALU.add), reads=[pr], writes=[sm])
                k.op(k.act, lambda: nc.scalar.activation(out=sm.ap[:, 2:4], in_=sm.ap[:, 0:2], func=AF.Exp), reads=[sm], writes=[sm])
                k.op(k.dve, lambda: nc.vector.tensor_tensor(out=sm.ap[:, 0:1], in0=sm.ap[:, 3:4], in1=sm.ap[:, 2:3], op=ALU.subtract), reads=[sm], writes=[sm])
                k.op(k.dve, lambda: nc.vector.tensor_scalar(out=self.neglam.ap[:, j:j + 1], in0=sm.ap[:, 0:1], scalar1=-li, scalar2=None, op0=ALU.add), reads=[sm], writes=[self.neglam])
            for j in range(2):
                li = 0.8 - 0.6 * math.exp(-0.3 * 3 * j)
                sl = self.subln.ap[:, j * 128:(j + 1) * 128]
                k.op(k.dve, lambda: nc.vector.tensor_scalar(out=sl, in0=sl, scalar1=1.0 - li, scalar2=None, op0=ALU.mult), reads=[self.subln], writes=[self.subln])
            k.barrier()

    def g_attn(self, l, dc):
        return self.small.ap[:, l * 8 + dc:l * 8 + dc + 1]

    def g_ffn(self, l, dc):
        return self.small.ap[:, 32 + l * 8 + dc:32 + l * 8 + dc + 1]

    def g_final(self, dc):
        return self.small.ap[:, 64 + dc:64 + dc + 1]

    def cast_weights(self):
        k = self.k
        for l in range(DEPTH):
            ds = Sem(self.nc, "cast%d" % l)
            k.fixed.append(ds)
            g = Tok()
            for (n, kk, nn) in self.wl[l]:
                src, dst = self.wf[n], self.wb[n]
                nsplit = 1 if nn <= 2048 else 2
                w = nn // nsplit
                for i in range(nsplit):
                    k.dma(k.pool, ds, dst.ap[:, i * w:(i + 1) * w], src[:, i * w:(i + 1) * w], writes=[dst], group=g)

    def rope_tables(self, st):
        nc, k = self.nc, self.k
        pi = self.sb(st, "r_pi", [128, 1], I32)
        pf = self.sb(st, "r_pf", [128, 1], F32)
        inv = self.sb(st, "r_inv", [128, 1], F32)
        k.op(k.pool, lambda: nc.gpsimd.iota(pi.ap, pattern=[[0, 1]], base=0, channel_multiplier=1), writes=[pi])
        k.op(k.dve, lambda: nc.vector.tensor_single_scalar(out=pi.ap, in_=pi.ap, scalar=31, op=ALU.bitwise_and), reads=[pi], writes=[pi])
        k.op(k.dve, lambda: nc.vector.tensor_copy(out=pf.ap, in_=pi.ap), reads=[pi], writes=[pf])
        k.op(k.act, lambda: nc.scalar.activation(out=inv.ap, in_=pf.ap, func=AF.Exp, scale=-math.log(10000.0) / 32.0), reads=[pf], writes=[inv])
        k.op(k.dve, lambda: nc.vector.tensor_scalar(out=inv.ap, in0=inv.ap, scalar1=1.0 / (2.0 * math.pi), scalar2=None, op0=ALU.mult), reads=[inv], writes=[inv])
        W = 1024
        ti = self.sb(st, "r_ti", [128, W], I32)
        tt = self.sb(st, "r_tt", [128, W], F32)
        ff = self.sb(st, "r_ff", [128, W], F32)
        ni = self.sb(st, "r_ni", [128, W], I32)
        nf = self.sb(st, "r_nf", [128, W], F32)
        g = self.sb(st, "r_g", [128, W], F32)
        res = [self.sb(st, "r_res%d" % i, [128, 2, W], F32) for i in range(2)]
        dss = [k.dsem(), k.dsem()]
        for c in range(S // W):
            rb = res[c % 2]
            k.op(k.pool, lambda: nc.gpsimd.iota(ti.ap, pattern=[[1, W]], base=c * W, channel_multiplier=0), writes=[ti])
            k.op(k.dve, lambda: nc.vector.tensor_copy(out=tt.ap, in_=ti.ap), reads=[ti], writes=[tt])
            k.op(k.dve, lambda: nc.vector.tensor_scalar(out=tt.ap, in0=tt.ap, scalar1=inv.ap, scalar2=None, op0=ALU.mult), reads=[tt, inv], writes=[tt])
            for which in range(2):
                off = 0.25 if which == 0 else 0.0
                k.op(k.dve, lambda: nc.vector.tensor_scalar(out=ff.ap, in0=tt.ap, scalar1=off, scalar2=None, op0=ALU.add), reads=[tt], writes=[ff])
                k.op(k.dve, lambda: nc.vector.tensor_copy(out=ni.ap, in_=ff.ap), reads=[ff], writes=[ni])
                k.op(k.dve, lambda: nc.vector.tensor_copy(out=nf.ap, in_=ni.ap), reads=[ni], writes=[nf])
                k.op(k.dve, lambda: nc.vector.tensor_tensor(out=ff.ap, in0=ff.ap, in1=nf.ap, op=ALU.subtract), reads=[ff, nf], writes=[ff])
                k.op(k.dve, lambda: nc.vector.tensor_single_scalar(out=g.ap, in_=ff.ap, scalar=0.5, op=ALU.is_gt), reads=[ff], writes=[g])
                k.op(k.dve, lambda: nc.vector.tensor_tensor(out=ff.ap, in0=ff.ap, in1=g.ap, op=ALU.subtract), reads=[ff, g], writes=[ff])
                k.op(k.dve, lambda: nc.vector.tensor_single_scalar(out=g.ap, in_=ff.ap, scalar=-0.5, op=ALU.is_lt), reads=[ff], writes=[g])
                k.op(k.dve, lambda: nc.vector.tensor_tensor(out=ff.ap, in0=ff.ap, in1=g.ap, op=ALU.add), reads=[ff, g], writes=[ff])
                k.op(k.act, lambda: nc.scalar.activation(out=rb.ap[:, which, :], in_=ff.ap, func=AF.Sin, scale=2.0 * math.pi * (1.0 - 1e-6)), reads=[ff], writes=[rb])
            k.dma(k.sp, dss[c % 2], self.cs_d.ap[:, :, c * W:(c + 1) * W], rb.ap, reads=[rb], writes=[self.cs_d])

    def rmsnorm_fm(self, src, nchunk, width, gain_fn, dst, dst_ap_fn, sqr, ssps, lnb, rstd, dim, src_ap_fn=None, src_bufs=None):
        nc, k = self.nc, self.k
        sbufs = src_bufs if src_bufs is not None else [src]
        for dc in range(nchunk):
            sq = sqr.next()
            sap = src_ap_fn(dc)
            k.op(k.act, lambda: nc.scalar.activation(out=sq.ap[:, :width], in_=sap, func=AF.Square), reads=sbufs, writes=[sq])
            k.op(k.pe, lambda: nc.tensor.matmul(ssps.ap[:, :width], lhsT=self.ones_f.ap, rhs=sq.ap[:, :width], start=(dc == 0), stop=(dc == nchunk - 1)),
                 reads=[sq, self.ones_f], writes=[ssps])
        k.op(k.act, lambda: nc.scalar.activation(out=lnb.ap[:, :width], in_=ssps.ap[:, :width], func=AF.Ln, scale=1.0 / dim, bias=self.epsb.ap), reads=[ssps, self.epsb], writes=[lnb])
        k.op(k.act, lambda: nc.scalar.activation(out=rstd.ap[:, :width], in_=lnb.ap[:, :width], func=AF.Exp, scale=-0.5), reads=[lnb], writes=[rstd])
        for dc in range(nchunk):
            sap = src_ap_fn(dc)
            dap = dst_ap_fn(dc)
            k.op(k.dve, lambda: nc.vector.scalar_tensor_tensor(out=dap, in0=sap, scalar=gain_fn(dc), in1=rstd.ap[:, :width], op0=ALU.mult, op1=ALU.mult),
                 reads=sbufs + [rstd, self.small], writes=[dst])

    def eps_ap(self):
        return self.epsb.ap

    def post_phase(self, st, l):
        nc, k = self.nc, self.k
        first = (l < 0)
        last = (l == self.n_layers - 1)
        hcs = Ring([self.sb(st, "hc%d" % i, [128, 8, TC], F32) for i in range(2)])
        h_ds = [k.dsem(), k.dsem()]
        hst_ds = [k.dsem(), k.dsem()]
        sqr = Ring([self.sb(st, "sq%d" % i, [128, TC], F32) for i in range(2)])
        lnb = self.sb(st, "lnb", [128, TC], F32)
        rstd = self.sb(st, "rstd", [128, TC], F32)
        if not last:
            uns = Ring([self.sb(st, "un%d" % i, [128, 8, TC], BF16) for i in range(1)])
            un_ds = [k.dsem(), k.dsem()]
        ssps = self.ps[7]
        hT_v = self.hT[0].ap.rearrange("(c p) t -> p c t", p=128)
        uT_v = self.uT.ap.rearrange("(c p) t -> p c t", p=128)
        oT_v = self.oT.ap.rearrange("(c p) t -> p c t", p=128)
        if first:
            xts = Ring([self.sb(st, "xt%d" % i, [128, D], F32) for i in range(8)])
            x_ds = [k.dsem() for _ in range(8)]
            psr = Ring(self.ps[0:6])
        else:
            ocs = Ring([self.sb(st, "oc%d" % i, [128, 8, TC], BF16) for i in range(2)])
            o_ds = [k.dsem(), k.dsem()]
            ucs = Ring([self.sb(st, "uc%d" % i, [128, 8, TC], BF16) for i in range(2)])
            gT = self.sb(st, "gT", [128, 22, TC], BF16)
            sil = Ring([self.sb(st, "sil%d" % i, [128, TC], F32) for i in range(2)])
            wo = self.sb(st, "wo", [128, 8, D], BF16)
            w2 = self.sb(st, "w2", [128, 22, D], BF16)
            w13 = Ring([self.sb(st, "w13_%d" % i, [128, 2, 8, 512], BF16) for i in range(2)])
            w_ds = [k.dsem(), k.dsem()]
            wres_ds = k.dsem()
            psr = Ring(self.ps[0:6])
            wob, w1b, w3b, w2b = self.wb["wo%d" % l], self.wb["w1_%d" % l], self.wb["w3_%d" % l], self.wb["w2_%d" % l]
            k.dma(k.sp, wres_ds, wo.ap, wob.ap.rearrange("(c p) f -> p c f", p=128), reads=[wob], writes=[wo])
            k.dma(k.sp, k.dsem(), w2.ap, w2b.ap.rearrange("(c p) f -> p c f", p=128), reads=[w2b], writes=[w2])
            w1v = w1b.ap.rearrange("(c p) f -> p c f", p=128)
            w3v = w3b.ap.rearrange("(c p) f -> p c f", p=128)
            fgs = [(i * 512, 512) for i in range(5)] + [(2560, 256)]
        if last:
            orow = Ring([self.sb(st, "orow%d" % i, [128, D], F32) for i in range(2)])
            or_ds = [k.dsem(), k.dsem()]

        def load_chunk(c):
            hc = hcs.bufs[c % 2]
            cols = slice(c * TC, (c + 1) * TC)
            if not first:
                k.dma(k.sp, h_ds[c % 2], hc.ap, hT_v[:, :, cols], reads=[self.hT[0]], writes=[hc])
                oc = ocs.bufs[c % 2]
                k.dma(k.sp, o_ds[c % 2], oc.ap, oT_v[:, :, cols], reads=[self.oT], writes=[oc])

        if not first:
            load_chunk(0)
        for c in range(NCH):
            cols = slice(c * TC, (c + 1) * TC)
            hc = hcs.bufs[c % 2]
            if first:
                xtl = []
                for i in range(4):
                    xt = xts.next()
                    k.dma(k.sp, x_ds[(xts.i - 1) % 8], xt.ap, self.x[c * TC + i * 128:c * TC + (i + 1) * 128, :], writes=[xt])
                    xtl.append(xt)
                for dc in range(8):
                    pb = psr.next()
                    for i in range(4):
                        k.op(k.pe, lambda: nc.tensor.transpose(pb.ap[:, i * 128:(i + 1) * 128], xtl[i].ap[:, dc * 128:(dc + 1) * 128], self.ident_f.ap),
                             reads=[xtl[i], self.ident_f], writes=[pb], signal=(i == 3))
                    e = k.act if dc % 2 == 0 else k.dve
                    if e is k.act:
                        k.op(e, lambda: nc.scalar.copy(out=hc.ap[:, dc, :], in_=pb.ap), reads=[pb], writes=[hc])
                    else:
                        k.op(e, lambda: nc.vector.tensor_copy(out=hc.ap[:, dc, :], in_=pb.ap), reads=[pb], writes=[hc])
            else:
                if c + 1 < NCH:
                    load_chunk(c + 1)
                oc = ocs.bufs[c % 2]
                for dc in range(8):
                    pb = psr.next()
                    for cc in range(8):
                        k.op(k.pe, lambda: nc.tensor.matmul(pb.ap, lhsT=wo.ap[:, cc, dc * 128:(dc + 1) * 128], rhs=oc.ap[:, cc, :], start=(cc == 0), stop=(cc == 7)),
                             reads=[wo, oc], writes=[pb], signal=(cc == 7))
                    k.op(k.dve, lambda: nc.vector.tensor_tensor(out=hc.ap[:, dc, :], in0=pb.ap, in1=hc.ap[:, dc, :], op=ALU.add), reads=[pb, hc], writes=[hc])
                uc = ucs.next()
                self.rmsnorm_fm(hc, 8, TC, lambda dc: self.g_ffn(l, dc), uc, lambda dc: uc.ap[:, dc, :], sqr, ssps, lnb, rstd, float(D),
                                src_ap_fn=lambda dc: hc.ap[:, dc, :])
                def load_w13(gi):
                    f0, fw = fgs[gi]
                    wt = w13.bufs[gi % 2]
                    g = Tok()
                    k.dma(k.sp, w_ds[gi % 2], wt.ap[:, 0, :, :fw], w1v[:, :, f0:f0 + fw], reads=[w1b], writes=[wt], group=g)
                    k.dma(k.sp, w_ds[gi % 2], wt.ap[:, 1, :, :fw], w3v[:, :, f0:f0 + fw], reads=[w3b], writes=[wt], group=g)
                load_w13(0)
                for gi, (f0, fw) in enumerate(fgs):
                    if gi + 1 < len(fgs):
                        load_w13(gi + 1)
                    wt = w13.bufs[gi % 2]
                    for fi in range(fw // 128):
                        fc = f0 // 128 + fi
                        p1 = psr.next()
                        p3 = psr.next()
                        for dc in range(8):
                            k.op(k.pe, lambda: nc.tensor.matmul(p1.ap, lhsT=wt.ap[:, 0, dc, fi * 128:(fi + 1) * 128], rhs=uc.ap[:, dc, :], start=(dc == 0), stop=(dc == 7)),
                                 reads=[wt, uc], writes=[p1], signal=(dc == 7))
                        for dc in range(8):
                            k.op(k.pe, lambda: nc.tensor.matmul(p3.ap, lhsT=wt.ap[:, 1, dc, fi * 128:(fi + 1) * 128], rhs=uc.ap[:, dc, :], start=(dc == 0), stop=(dc == 7)),
                                 reads=[wt, uc], writes=[p3], signal=(dc == 7))
                        sb_ = sil.next()
                        k.op(k.act, lambda: nc.scalar.activation(out=sb_.ap, in_=p1.ap, func=AF.Silu), reads=[p1], writes=[sb_])
                        k.op(k.dve, lambda: nc.vector.tensor_tensor(out=gT.ap[:, fc, :], in0=sb_.ap, in1=p3.ap, op=ALU.mult), reads=[sb_, p3], writes=[gT])
                for dc in range(8):
                    pb = psr.next()
                    for fc in range(22):
                        k.op(k.pe, lambda: nc.tensor.matmul(pb.ap, lhsT=w2.ap[:, fc, dc * 128:(dc + 1) * 128], rhs=gT.ap[:, fc, :], start=(fc == 0), stop=(fc == 21)),
                             reads=[w2, gT], writes=[pb], signal=(fc == 21))
                    k.op(k.dve, lambda: nc.vector.tensor_tensor(out=hc.ap[:, dc, :], in0=pb.ap, in1=hc.ap[:, dc, :], op=ALU.add), reads=[pb, hc], writes=[hc])
            if not last:
                k.dma(k.sp, hst_ds[c % 2], hT_v[:, :, cols], hc.ap, reads=[hc], writes=[self.hT[0]])
                un = uns.next()
                self.rmsnorm_fm(hc, 8, TC, lambda dc: self.g_attn(l + 1, dc), un, lambda dc: un.ap[:, dc, :], sqr, ssps, lnb, rstd, float(D),
                                src_ap_fn=lambda dc: hc.ap[:, dc, :])
                k.dma(k.sp, un_ds[c % 2], uT_v[:, :, cols], un.ap, reads=[un], writes=[self.uT])
            else:
                for dc in range(8):
                    sq = sqr.next()
                    k.op(k.act, lambda: nc.scalar.activation(out=sq.ap, in_=hc.ap[:, dc, :], func=AF.Square), reads=[hc], writes=[sq])
                    k.op(k.pe, lambda: nc.tensor.matmul(ssps.ap, lhsT=self.ones_f.ap, rhs=sq.ap, start=(dc == 0), stop=(dc == 7)),
                         reads=[sq, self.ones_f], writes=[ssps])
                k.op(k.act, lambda: nc.scalar.activation(out=lnb.ap, in_=ssps.ap, func=AF.Ln, scale=1.0 / D, bias=self.epsb.ap), reads=[ssps, self.epsb], writes=[lnb])
                k.op(k.act, lambda: nc.scalar.activation(out=rstd.ap, in_=lnb.ap, func=AF.Exp, scale=-0.5), reads=[lnb], writes=[rstd])
                for dc in range(8):
                    k.op(k.dve, lambda: nc.vector.scalar_tensor_tensor(out=hc.ap[:, dc, :], in0=hc.ap[:, dc, :], scalar=self.g_final(dc), in1=rstd.ap, op0=ALU.mult, op1=ALU.mult),
                         reads=[hc, rstd, self.small], writes=[hc])
                for i in range(4):
                    ob = orow.next()
                    for half in range(2):
                        pb = psr.next()
                        for j in range(4):
                            dc = half * 4 + j
                            k.op(k.pe, lambda: nc.tensor.transpose(pb.ap[:, j * 128:(j + 1) * 128], hc.ap[:, dc, i * 128:(i + 1) * 128], self.ident_f.ap),
                                 reads=[hc, self.ident_f], writes=[pb], signal=(j == 3))
                        if half == 0:
                            k.op(k.act, lambda: nc.scalar.copy(out=ob.ap[:, 0:512], in_=pb.ap), reads=[pb], writes=[ob])
                        else:
                            k.op(k.dve, lambda: nc.vector.tensor_copy(out=ob.ap[:, 512:1024], in_=pb.ap), reads=[pb], writes=[ob])
                    k.dma(k.sp, or_ds[(orow.i - 1) % 2], self.out[c * TC + i * 128:c * TC + (i + 1) * 128, :], ob.ap, reads=[ob])

    def attn_core(self, c, lhs_fn, rhs_fn, rbufs, v_fn, vbuf, E, scale, st_ring, pt_ring, acc, tri=True, mask_fn=None, maskbuf=None, mask_eng=None):
        nc, k = self.nc, self.k
        nk = 4 * c + 4

        def emit_st(j):
            r = j - 4 * c
            off = 128 * max(r, 0)
            n = 512 - off
            stb = st_ring.next()
            ls = lhs_fn(j)
            rs = rhs_fn(off, n)
            nl_ = len(ls)
            for i in range(nl_):
                k.op(k.pe, lambda: nc.tensor.matmul(stb.ap[:, off:512], lhsT=ls[i], rhs=rs[i], start=(i == 0), stop=(i == nl_ - 1)),
                     reads=rbufs, writes=[stb], signal=(i == nl_ - 1))
            return stb, r, off, n

        nxt = emit_st(0)
        for j in range(nk):
            stb, r, off, n = nxt
            if j + 1 < nk:
                nxt = emit_st(j + 1)
            pt = pt_ring.next()
            k.op(k.act, lambda: nc.scalar.activation(out=pt.ap[:, off:512], in_=stb.ap[:, off:512], func=AF.Exp, scale=scale), reads=[stb], writes=[pt])
            if r >= 0 and tri:
                k.op(k.pool, lambda: nc.gpsimd.tensor_tensor(out=pt.ap[:, off:off + 128], in0=pt.ap[:, off:off + 128], in1=self.tri.ap, op=ALU.mult),
                     reads=[pt, self.tri], writes=[pt])
            if mask_fn is not None:
                me = mask_eng(j)
                mm = mask_fn(j, off, n)
                k.op(me, lambda: me.eng.tensor_tensor(out=pt.ap[:, off:512], in0=pt.ap[:, off:512], in1=mm, op=ALU.mult), reads=[pt, maskbuf], writes=[pt])
            for qt in range(max(r, 0), 4):
                k.op(k.pe, lambda: nc.tensor.matmul(acc[qt].ap[:, 0:E + 1], lhsT=pt.ap[:, qt * 128:(qt + 1) * 128], rhs=v_fn(j), start=(j == 0), stop=(j == 4 * c + qt)),
                     reads=[pt, vbuf], writes=[acc[qt]], signal=(qt == 3))

    def rope_fm(self, pa, np_, cs, cols, dst, dst_ap, xbr, rar, rbr, pb, split=None):
        nc, k = self.nc, self.k
        import os as _os
        lvl = _os.environ.get("KROPE", "z")
        xb = xbr.next()
        ra = rar.next()
        rb = rbr.next()
        k.op(k.act, lambda: nc.scalar.copy(out=xb.ap[0:np_, :], in_=pa.ap[0:np_, :]), reads=[pa], writes=[xb])
        if lvl >= "b":
            k.op(k.pe, lambda: nc.tensor.matmul(pb.ap[0:np_, :], lhsT=self.rperm.ap[0:np_, 0:np_], rhs=xb.ap[0:np_, :], start=True, stop=True), reads=[xb, self.rperm], writes=[pb])
        if lvl >= "c":
            k.op(k.dve, lambda: nc.vector.tensor_tensor(out=ra.ap[0:np_, :], in0=pa.ap[0:np_, :], in1=cs.ap[0:np_, 0, cols], op=ALU.mult), reads=[pa, cs], writes=[ra])
        if lvl >= "d":
            k.op(k.dve, lambda: nc.vector.tensor_tensor(out=rb.ap[0:np_, :], in0=pb.ap[0:np_, :], in1=cs.ap[0:np_, 1, cols], op=ALU.mult), reads=[pb, cs], writes=[rb])
        if split is not None:
            for hh in range(2):
                k.op(k.pool, lambda: nc.gpsimd.tensor_tensor(out=dst[hh].ap[hh * 64:(hh + 1) * 64, split], in0=ra.ap[hh * 64:(hh + 1) * 64, :], in1=rb.ap[hh * 64:(hh + 1) * 64, :], op=ALU.add),
                     reads=[ra, rb], writes=[dst[hh]])
        elif lvl >= "e":
            k.op(k.pool, lambda: nc.gpsimd.tensor_tensor(out=dst_ap, in0=ra.ap[0:np_, :], in1=rb.ap[0:np_, :], op=ALU.add), reads=[ra, rb], writes=[dst])
        else:
            k.op(k.act, lambda: nc.scalar.copy(out=dst_ap, in_=pa.ap[0:np_, :]), reads=[pa], writes=[dst])

    def load_full(self, st, name, dbuf, shape_inner, dt, nsplit=8):
        k = self.k
        t = self.sb(st, name, [128] + shape_inner, dt)
        v = dbuf.ap.rearrange("(c p) t -> p c t", p=128)
        g = Tok()
        ds = k.dsem()
        w = S // nsplit
        for i in range(nsplit):
            k.dma(k.sp, ds, t.ap[:, :, i * w:(i + 1) * w], v[:, :, i * w:(i + 1) * w], reads=[dbuf], writes=[t], group=g)
        return t

    def evac_norm(self, acc, E, dst, dst_ap_fn, recr):
        nc, k = self.nc, self.k
        for qt in range(4):
            rec = recr.next()
            k.op(k.dve, lambda: nc.vector.reciprocal(out=rec.ap, in_=acc[qt].ap[:, E:E + 1]), reads=[acc[qt]], writes=[rec])
            k.op(k.dve, lambda: nc.vector.tensor_scalar(out=dst_ap_fn(qt), in0=acc[qt].ap[:, 0:E], scalar1=rec.ap, scalar2=None, op0=ALU.mult), reads=[acc[qt], rec], writes=[dst])

    def transpose_out(self, src, src_ap_fn, pbuf, dst, dst_ap, eng):
        nc, k = self.nc, self.k
        for qt in range(4):
            k.op(k.pe, lambda: nc.tensor.transpose(pbuf.ap[:, qt * 128:(qt + 1) * 128], src_ap_fn(qt), self.ident_f.ap), reads=[src, self.ident_f], writes=[pbuf], signal=(qt == 3))
        if eng is k.act:
            k.op(eng, lambda: nc.scalar.copy(out=dst_ap, in_=pbuf.ap), reads=[pbuf], writes=[dst])
        else:
            k.op(eng, lambda: nc.vector.tensor_copy(out=dst_ap, in_=pbuf.ap), reads=[pbuf], writes=[dst])

    def diff_phase(self, st, l):
        nc, k = self.nc, self.k
        jj = l // 3
        uT = self.load_full(st, "uTf", self.uT, [8, S], BF16)
        cs = self.sb(st, "cs", [128, 2, S], F32)
        k.dma(k.sp, k.dsem(), cs.ap, self.cs_d.ap, reads=[self.cs_d], writes=[cs])
        wqkv = self.wb["wqkv%d" % l]
        wv = wqkv.ap.rearrange("(c p) f -> p c f", p=128)
        whs = Ring([self.sb(st, "wh%d" % i, [128, 8, 384], BF16) for i in range(2)])
        wh_ds = [k.dsem(), k.dsem()]
        qTz = [self.sb(st, "qTz%d" % i, [128, S], BF16) for i in range(2)]
        k.op(k.dve, lambda: nc.vector.memset(qTz[0].ap[64:128, :], 0.0), writes=[qTz[0]])
        k.op(k.dve, lambda: nc.vector.memset(qTz[1].ap[0:64, :], 0.0), writes=[qTz[1]])
        kTs = Ring([self.sb(st, "kT%d" % i, [128, S], BF16) for i in range(2)])
        Vs = Ring([self.sb(st, "V%d" % i, [128, 32, 130], BF16) for i in range(2)])
        for vb in Vs.bufs:
            k.op(k.dve, lambda: nc.vector.memset(vb.ap[:, :, 128:130], 1.0), writes=[vb])
        xbr = Ring([self.sb(st, "xb%d" % i, [128, TC], BF16) for i in range(2)])
        rar = Ring([self.sb(st, "ra%d" % i, [128, TC], F32) for i in range(2)])
        rbr = Ring([self.sb(st, "rb%d" % i, [128, TC], F32) for i in range(2)])
        pts = Ring([self.sb(st, "pt%d" % i, [128, TC], BF16) for i in range(4)])
        recr = Ring([self.sb(st, "rec%d" % i, [128, 1], F32) for i in range(8)])
        om = [self.sb(st, "om%d" % i, [128, 4, 128], F32) for i in range(2)]
        d4 = self.sb(st, "d4", [128, 4, 128], F32)
        sq4 = self.sb(st, "sq4", [128, 4, 128], F32)
        ss4 = self.sb(st, "ss4", [128, 4], F32)
        ln4 = self.sb(st, "ln4", [128, 4], F32)
        rs4 = self.sb(st, "rs4", [128, 4], F32)
        dn = Ring([self.sb(st, "dn%d" % i, [128, 4, 128], F32) for i in range(2)])
        oTh = Ring([self.sb(st, "oTh%d" % i, [128, S], BF16) for i in range(2)])
        oth_ds = [k.dsem(), k.dsem()]
        st_ring = Ring(self.ps[0:2])
        acc = self.ps[2:6]
        misc = Ring(self.ps[6:8])
        sub_ap = self.subln.ap[:, jj * 128:(jj + 1) * 128].unsqueeze(1).broadcast_to([128, 4, 128])
        scale = 64 ** -0.5
        import os as _os
        for h in range(int(_os.environ.get("KHEADS", "8"))):
            wh = whs.next()
            g = Tok()
            for i in range(3):
                k.dma(k.sp, wh_ds[h % 2], wh.ap[:, :, i * 128:(i + 1) * 128], wv[:, :, i * 1024 + h * 128:i * 1024 + (h + 1) * 128], reads=[wqkv], writes=[wh], group=g)
            kT, V = kTs.next(), Vs.next()
            import os as _os
            stage = float(_os.environ.get("KDIFF_STAGE", "3"))
            for tc in range(NCH):
                if stage < 0.5:
                    break
                if _os.environ.get("KTC1") and tc >= int(_os.environ.get("KTC1")):
                    break
                cols = slice(tc * TC, (tc + 1) * TC)
                for which, dstb in ((0, None), (1, kT)):
                    if _os.environ.get("KSKIPQK"):
                        break
                    pa = misc.next()
                    for dc in range(8):
                        k.op(k.pe, lambda: nc.tensor.matmul(pa.ap, lhsT=wh.ap[:, dc, which * 128:(which + 1) * 128], rhs=uT.ap[:, dc, cols], start=(dc == 0), stop=(dc == 7)),
                             reads=[wh, uT], writes=[pa], signal=(dc == 7))
                    pb = misc.next()
                    if stage < 0.7:
                        k.op(k.act, lambda: nc.scalar.copy(out=dstb.ap[:, cols], in_=pa.ap), reads=[pa], writes=[dstb])
                        continue
                    if which == 0:
                        self.rope_fm(pa, 128, cs, cols, qTz, None, xbr, rar, rbr, pb, split=cols)
                    else:
                        self.rope_fm(pa, 128, cs, cols, dstb, dstb.ap[:, cols], xbr, rar, rbr, pb)
                if _os.environ.get("KSKIPV"):
                    continue
                pa = misc.next()
                for i in range(4):
                    for dc in range(8):
                        k.op(k.pe, lambda: nc.tensor.matmul(pa.ap[:, i * 128:(i + 1) * 128], lhsT=uT.ap[:, dc, tc * TC + i * 128:tc * TC + (i + 1) * 128], rhs=wh.ap[:, dc, 256:384], start=(dc == 0), stop=(dc == 7)),
                             reads=[wh, uT], writes=[pa], signal=(dc == 7 and i == 3))
                k.op(k.act, lambda: nc.scalar.copy(out=V.ap[:, tc * 4:tc * 4 + 4, 0:128], in_=pa.ap.rearrange("p (a b) -> p a b", a=4)), reads=[pa], writes=[V])
            oT_h = oTh.next()
            if stage < 3:
                k.op(k.dve, lambda: nc.vector.memset(oT_h.ap, 0.0), writes=[oT_h])
            for c in range(NCH):
                if stage < 2:
                    break
                for m in range(2):
                    self.attn_core(c,
                                   lambda j: [kT.ap[:, j * 128:(j + 1) * 128]],
                                   lambda off, n: [qTz[m].ap[:, c * TC + off:c * TC + off + n]],
                                   [kT, qTz[m]], lambda j: V.ap[:, j, 0:129], V, 128, scale, st_ring, pts, acc)
                    self.evac_norm(acc, 128, om[m], lambda qt: om[m].ap[:, qt, :], recr)
                if stage < 3:
                    continue
                k.op(k.dve, lambda: nc.vector.scalar_tensor_tensor(out=d4.ap, in0=om[1].ap, scalar=self.neglam.ap[:, jj:jj + 1], in1=om[0].ap, op0=ALU.mult, op1=ALU.add),
                     reads=[om[0], om[1], self.neglam], writes=[d4])
                k.op(k.dve, lambda: nc.vector.tensor_tensor(out=sq4.ap, in0=d4.ap, in1=d4.ap, op=ALU.mult), reads=[d4], writes=[sq4])
                k.op(k.dve, lambda: nc.vector.tensor_reduce(out=ss4.ap, in_=sq4.ap, axis=AX.X, op=ALU.add), reads=[sq4], writes=[ss4])
                k.op(k.act, lambda: nc.scalar.activation(out=ln4.ap, in_=ss4.ap, func=AF.Ln, scale=1.0 / 128.0, bias=self.epsb.ap), reads=[ss4, self.epsb], writes=[ln4])
                k.op(k.act, lambda: nc.scalar.activation(out=rs4.ap, in_=ln4.ap, func=AF.Exp, scale=-0.5), reads=[ln4], writes=[rs4])
                dnb = dn.next()
                sub2 = self.subln.ap[:, jj * 128:(jj + 1) * 128]
                for qt in range(4):
                    k.op(k.dve, lambda: nc.vector.scalar_tensor_tensor(out=dnb.ap[:, qt, :], in0=d4.ap[:, qt, :], scalar=rs4.ap[:, qt:qt + 1], in1=sub2, op0=ALU.mult, op1=ALU.mult),
                         reads=[d4, rs4, self.subln], writes=[dnb])
                self.transpose_out(dnb, lambda qt: dnb.ap[:, qt, :], misc.next(), oT_h, oT_h.ap[:, c * TC:(c + 1) * TC], k.act)
            k.dma(k.sp, oth_ds[h % 2], self.oT.ap[h * 128:(h + 1) * 128, :], oT_h.ap, reads=[oT_h], writes=[self.oT])

    def mla_phase(self, st, l):
        nc, k = self.nc, self.k
        wd, wuq, wukv = self.wb["wdown%d" % l], self.wb["wuq%d" % l], self.wb["wukv%d" % l]
        cs = self.sb(st, "cs", [128, 2, S], F32)
        k.dma(k.sp, k.dsem(), cs.ap, self.cs_d.ap, reads=[self.cs_d], writes=[cs])
        wdsb = self.sb(st, "wdsb", [128, 8, 704], BF16)
        k.dma(k.sp, k.dsem(), wdsb.ap, wd.ap.rearrange("(c p) f -> p c f", p=128), reads=[wd], writes=[wdsb])
        cqn = self.sb(st, "cqn", [128, 3, S], BF16)
        ckvn = self.sb(st, "ckvn", [128, 2, S], BF16)
        krT = self.sb(st, "krT", [128, S], BF16)
        ucs = Ring([self.sb(st, "uc%d" % i, [128, 8, TC], BF16) for i in range(2)])
        u_ds = [k.dsem(), k.dsem()]
        sqr = Ring([self.sb(st, "sq%d" % i, [128, TC], F32) for i in range(2)])
        lnb = self.sb(st, "lnb", [128, TC], F32)
        rstd = self.sb(st, "rstd", [128, TC], F32)
        xbr = Ring([self.sb(st, "xb%d" % i, [128, TC], BF16) for i in range(2)])
        rar = Ring([self.sb(st, "ra%d" % i, [128, TC], F32) for i in range(2)])
        rbr = Ring([self.sb(st, "rb%d" % i, [128, TC], F32) for i in range(2)])
        uT_v = self.uT.ap.rearrange("(c p) t -> p c t", p=128)
        ps = self.ps
        for tc in range(NCH):
            cols = slice(tc * TC, (tc + 1) * TC)
            uc = ucs.next()
            k.dma(k.sp, u_ds[tc % 2], uc.ap, uT_v[:, :, cols], reads=[self.uT], writes=[uc])
            for i in range(3):
                for dc in range(8):
                    k.op(k.pe, lambda: nc.tensor.matmul(ps[i].ap, lhsT=wdsb.ap[:, dc, i * 128:(i + 1) * 128], rhs=uc.ap[:, dc, :], start=(dc == 0), stop=(dc == 7)),
                         reads=[wdsb, uc], writes=[ps[i]], signal=(dc == 7))
            self.rmsnorm_fm(None, 3, TC, lambda i: self.small.ap[:, 72 + i:73 + i], cqn, lambda i: cqn.ap[:, i, cols], sqr, ps[7], lnb, rstd, 384.0,
                            src_ap_fn=lambda i: ps[i].ap, src_bufs=[ps[0], ps[1], ps[2]])
            for i in range(2):
                for dc in range(8):
                    k.op(k.pe, lambda: nc.tensor.matmul(ps[3 + i].ap, lhsT=wdsb.ap[:, dc, 384 + i * 128:384 + (i + 1) * 128], rhs=uc.ap[:, dc, :], start=(dc == 0), stop=(dc == 7)),
                         reads=[wdsb, uc], writes=[ps[3 + i]], signal=(dc == 7))
            self.rmsnorm_fm(None, 2, TC, lambda i: self.small.ap[:, 75 + i:76 + i], ckvn, lambda i: ckvn.ap[:, i, cols], sqr, ps[7], lnb, rstd, 256.0,
                            src_ap_fn=lambda i: ps[3 + i].ap, src_bufs=[ps[3], ps[4]])
            for dc in range(8):
                k.op(k.pe, lambda: nc.tensor.matmul(ps[5].ap[0:64, :], lhsT=wdsb.ap[:, dc, 640:704], rhs=uc.ap[:, dc, :], start=(dc == 0), stop=(dc == 7)),
                     reads=[wdsb, uc], writes=[ps[5]], signal=(dc == 7))
            self.rope_fm(ps[5], 64, cs, cols, krT, krT.ap[0:64, cols], xbr, rar, rbr, ps[6])
        wqh = Ring([self.sb(st, "wqh%d" % i, [128, 3, 192], BF16) for i in range(2)])
        wkvh = Ring([self.sb(st, "wkvh%d" % i, [128, 2, 256], BF16) for i in range(2)])
        wq_ds = [k.dsem(), k.dsem()]
        wuq_v = wuq.ap.rearrange("(c p) f -> p c f", p=128)
        wukv_v = wukv.ap.rearrange("(c p) f -> p c f", p=128)
        qnT = self.sb(st, "qnT", [128, S], BF16)
        qrT = self.sb(st, "qrT", [128, S], BF16)
        knT = self.sb(st, "knT", [128, S], BF16)
        V = self.sb(st, "V", [128, 32, 130], BF16)
        k.op(k.dve, lambda: nc.vector.memset(V.ap[:, :, 128:130], 1.0), writes=[V])
        pts = Ring([self.sb(st, "pt%d" % i, [128, TC], BF16) for i in range(4)])
        recr = Ring([self.sb(st, "rec%d" % i, [128, 1], F32) for i in range(8)])
        on = Ring([self.sb(st, "on%d" % i, [128, 4, 128], F32) for i in range(2)])
        oTh = Ring([self.sb(st, "oTh%d" % i, [128, S], BF16) for i in range(2)])
        oth_ds = [k.dsem(), k.dsem()]
        st_ring = Ring(ps[0:2])
        acc = ps[2:6]
        misc = Ring(ps[6:8])
        scale = 192 ** -0.5
        for h in range(8):
            wq, wkv = wqh.next(), wkvh.next()
            g = Tok()
            k.dma(k.sp, wq_ds[h % 2], wq.ap, wuq_v[:, :, h * 192:(h + 1) * 192], reads=[wuq], writes=[wq], group=g)
            k.dma(k.sp, wq_ds[h % 2], wkv.ap, wukv_v[:, :, h * 256:(h + 1) * 256], reads=[wukv], writes=[wkv], group=g)
            for tc in range(NCH):
                cols = slice(tc * TC, (tc + 1) * TC)
                pa = misc.next()
                for i in range(3):
                    k.op(k.pe, lambda: nc.tensor.matmul(pa.ap, lhsT=wq.ap[:, i, 0:128], rhs=cqn.ap[:, i, cols], start=(i == 0), stop=(i == 2)), reads=[wq, cqn], writes=[pa], signal=(i == 2))
                k.op(k.act, lambda: nc.scalar.copy(out=qnT.ap[:, cols], in_=pa.ap), reads=[pa], writes=[qnT])
                pa = misc.next()
                for i in range(3):
                    k.op(k.pe, lambda: nc.tensor.matmul(pa.ap[0:64, :], lhsT=wq.ap[:, i, 128:192], rhs=cqn.ap[:, i, cols], start=(i == 0), stop=(i == 2)), reads=[wq, cqn], writes=[pa], signal=(i == 2))
                pb = misc.next()
                self.rope_fm(pa, 64, cs, cols, qrT, qrT.ap[0:64, cols], xbr, rar, rbr, pb)
                pa = misc.next()
                for i in range(2):
                    k.op(k.pe, lambda: nc.tensor.matmul(pa.ap, lhsT=wkv.ap[:, i, 0:128], rhs=ckvn.ap[:, i, cols], start=(i == 0), stop=(i == 1)), reads=[wkv, ckvn], writes=[pa], signal=(i == 1))
                k.op(k.dve, lambda: nc.vector.tensor_copy(out=knT.ap[:, cols], in_=pa.ap), reads=[pa], writes=[knT])
                pa = misc.next()
                for t4 in range(4):
                    for i in range(2):
                        k.op(k.pe, lambda: nc.tensor.matmul(pa.ap[:, t4 * 128:(t4 + 1) * 128], lhsT=ckvn.ap[:, i, tc * TC + t4 * 128:tc * TC + (t4 + 1) * 128], rhs=wkv.ap[:, i, 128:256], start=(i == 0), stop=(i == 1)),
                             reads=[wkv, ckvn], writes=[pa], signal=(i == 1 and t4 == 3))
                k.op(k.act, lambda: nc.scalar.copy(out=V.ap[:, tc * 4:tc * 4 + 4, 0:128], in_=pa.ap.rearrange("p (a b) -> p a b", a=4)), reads=[pa], writes=[V])
            oT_h = oTh.next()
            for c in range(NCH):
                self.attn_core(c,
                               lambda j: [knT.ap[:, j * 128:(j + 1) * 128], krT.ap[0:64, j * 128:(j + 1) * 128]],
                               lambda off, n: [qnT.ap[:, c * TC + off:c * TC + off + n], qrT.ap[0:64, c * TC + off:c * TC + off + n]],
                               [knT, krT, qnT, qrT], lambda j: V.ap[:, j, 0:129], V, 128, scale, st_ring, pts, acc)
                onb = on.next()
                self.evac_norm(acc, 128, onb, lambda qt: onb.ap[:, qt, :], recr)
                self.transpose_out(onb, lambda qt: onb.ap[:, qt, :], misc.next(), oT_h, oT_h.ap[:, c * TC:(c + 1) * TC], k.act)
            k.dma(k.sp, oth_ds[h % 2], self.oT.ap[h * 128:(h + 1) * 128, :], oT_h.ap, reads=[oT_h], writes=[self.oT])

    def dsa_phase(self, st, l):
        nc, k = self.nc, self.k
        win = self.wb["win%d" % l]
        wv_ = win.ap.rearrange("(c p) f -> p c f", p=128)
        Wq = self.sb(st, "Wq", [128, 8, 1024], BF16)
        Wkk = self.sb(st, "Wkk", [128, 8, 128], BF16)
        Wikk = self.sb(st, "Wikk", [128, 8, 128], BF16)
        Wv = self.sb(st, "Wv", [128, 8, 64], BF16)
        Wiq = self.sb(st, "Wiq", [128, 8, 512], BF16)
        Wiw = self.sb(st, "Wiw", [128, 8, 8], BF16)
        k.dma(k.sp, k.dsem(), Wq.ap, wv_[:, :, 0:1024], reads=[win], writes=[Wq])
        g = Tok(); dsx = k.dsem()
        k.dma(k.sp, dsx, Wkk.ap[:, :, 0:64], wv_[:, :, 1024:1088], reads=[win], writes=[Wkk], group=g)
        k.dma(k.sp, dsx, Wkk.ap[:, :, 64:128], wv_[:, :, 1024:1088], reads=[win], writes=[Wkk], group=g)
        k.dma(k.sp, dsx, Wikk.ap[:, :, 0:64], wv_[:, :, 1664:1728], reads=[win], writes=[Wikk], group=g)
        k.dma(k.sp, dsx, Wikk.ap[:, :, 64:128], wv_[:, :, 1664:1728], reads=[win], writes=[Wikk], group=g)
        k.dma(k.sp, dsx, Wv.ap, wv_[:, :, 1088:1152], reads=[win], writes=[Wv], group=g)
        k.dma(k.sp, dsx, Wiq.ap, wv_[:, :, 1152:1664], reads=[win], writes=[Wiq], group=g)
        k.dma(k.sp, dsx, Wiw.ap, wv_[:, :, 1728:1736], reads=[win], writes=[Wiw], group=g)
        KK = self.sb(st, "KK", [128, S], BF16)
        IKK = self.sb(st, "IKK", [128, S], BF16)
        Vaug = self.sb(st, "Vaug", [128, 32, 66], BF16)
        k.op(k.dve, lambda: nc.vector.memset(Vaug.ap[:, :, 64:66], 1.0), writes=[Vaug])
        ucs = Ring([self.sb(st, "uc%d" % i, [128, 8, TC], BF16) for i in range(1)])
        u_ds = [k.dsem()]
        csc = Ring([self.sb(st, "csc%d" % i, [128, 2, TC], F32) for i in range(2)])
        cs_ds = [k.dsem(), k.dsem()]
        xbr = Ring([self.sb(st, "xb%d" % i, [128, TC], BF16) for i in range(2)])
        rar = Ring([self.sb(st, "ra%d" % i, [128, TC], F32) for i in range(2)])
        rbr = Ring([self.sb(st, "rb%d" % i, [128, TC], F32) for i in range(2)])
        uT_v = self.uT.ap.rearrange("(c p) t -> p c t", p=128)
        ps = self.ps
        st_ring = Ring(ps[0:2])
        acc = ps[2:6]
        misc = Ring(ps[6:8])

        class CsView:
            pass

        def load_chunk(tc):
            cols = slice(tc * TC, (tc + 1) * TC)
            uc = ucs.next()
            k.dma(k.sp, u_ds[0], uc.ap, uT_v[:, :, cols], reads=[self.uT], writes=[uc])
            cc = csc.next()
            k.dma(k.sp, cs_ds[(csc.i - 1) % 2], cc.ap, self.cs_d.ap[:, :, cols], reads=[self.cs_d], writes=[cc])
            return uc, cc

        lcols = slice(0, TC)
        for tc in range(NCH):
            cols = slice(tc * TC, (tc + 1) * TC)
            uc, cc = load_chunk(tc)
            for W_, dstb in ((Wkk, KK), (Wikk, IKK)):
                pa = misc.next()
                for dc in range(8):
                    k.op(k.pe, lambda: nc.tensor.matmul(pa.ap, lhsT=W_.ap[:, dc, :], rhs=uc.ap[:, dc, :], start=(dc == 0), stop=(dc == 7)), reads=[W_, uc], writes=[pa], signal=(dc == 7))
                pb = misc.next()
                self.rope_fm(pa, 128, cc, lcols, dstb, dstb.ap[:, cols], xbr, rar, rbr, pb)
            pa = misc.next()
            for t4 in range(4):
                for dc in range(8):
                    k.op(k.pe, lambda: nc.tensor.matmul(pa.ap[:, t4 * 64:(t4 + 1) * 64], lhsT=uc.ap[:, dc, t4 * 128:(t4 + 1) * 128], rhs=Wv.ap[:, dc, :], start=(dc == 0), stop=(dc == 7)),
                         reads=[Wv, uc], writes=[pa], signal=(dc == 7 and t4 == 3))
            k.op(k.act, lambda: nc.scalar.copy(out=Vaug.ap[:, tc * 4:tc * 4 + 4, 0:64], in_=pa.ap[:, 0:256].rearrange("p (a b) -> p a b", a=4)), reads=[pa], writes=[Vaug])
        qTc = self.sb(st, "qTc", [128, 8, TC], BF16)
        iqTc = self.sb(st, "iqTc", [128, 4, TC], BF16)
        iwc = self.sb(st, "iwc", [128, 4, 8], F32)
        accr = Ring([self.sb(st, "sacc%d" % i, [128, S], F32) for i in range(2)])
        mb = self.sb(st, "mb", [128, S], F32)
        maskT = self.sb(st, "maskT", [128, 32, TC], BF16)
        rlr = Ring([self.sb(st, "rl%d" % i, [128, TC], F32) for i in range(3)])
        lo = self.sb(st, "lo", [128, 1], F32)
        mid = self.sb(st, "mid", [128, 1], F32)
        cnt = self.sb(st, "cnt", [128, 1], F32)
        tt = self.sb(st, "tt", [128, 1], F32)
        pts = Ring([self.sb(st, "pt%d" % i, [128, TC], BF16) for i in range(4)])
        recr = Ring([self.sb(st, "rec%d" % i, [128, 1], F32) for i in range(8)])
        onp = Ring([self.sb(st, "onp%d" % i, [128, 4, 128], F32) for i in range(2)])
        oTc = self.sb(st, "oTc", [128, 8, TC], BF16)
        oc_ds = k.dsem()
        oT_v = self.oT.ap.rearrange("(c p) t -> p c t", p=128)
        scale = 64 ** -0.5
        iw_scale = (64 ** -0.5) * (8 ** -0.5)
        NIT = 18
        for c in range(NCH):
            cols = slice(c * TC, (c + 1) * TC)
            uc, cc = load_chunk(c)
            for pair in range(8):
                pa = misc.next()
                for dc in range(8):
                    k.op(k.pe, lambda: nc.tensor.matmul(pa.ap, lhsT=Wq.ap[:, dc, pair * 128:(pair + 1) * 128], rhs=uc.ap[:, dc, :], start=(dc == 0), stop=(dc == 7)), reads=[Wq, uc], writes=[pa], signal=(dc == 7))
                pb = misc.next()
                self.rope_fm(pa, 128, cc, lcols, qTc, qTc.ap[:, pair, :], xbr, rar, rbr, pb)
            for pair in range(4):
                pa = misc.next()
                for dc in range(8):
                    k.op(k.pe, lambda: nc.tensor.matmul(pa.ap, lhsT=Wiq.ap[:, dc, pair * 128:(pair + 1) * 128], rhs=uc.ap[:, dc, :], start=(dc == 0), stop=(dc == 7)), reads=[Wiq, uc], writes=[pa], signal=(dc == 7))
                pb = misc.next()
                self.rope_fm(pa, 128, cc, lcols, iqTc, iqTc.ap[:, pair, :], xbr, rar, rbr, pb)
            pa = misc.next()
            for qt in range(4):
                for dc in range(8):
                    k.op(k.pe, lambda: nc.tensor.matmul(pa.ap[:, qt * 8:(qt + 1) * 8], lhsT=uc.ap[:, dc, qt * 128:(qt + 1) * 128], rhs=Wiw.ap[:, dc, :], start=(dc == 0), stop=(dc == 7)),
                         reads=[Wiw, uc], writes=[pa], signal=(dc == 7 and qt == 3))
            k.op(k.dve, lambda: nc.vector.tensor_scalar(out=iwc.ap, in0=pa.ap[:, 0:32].rearrange("p (a b) -> p a b", a=4), scalar1=iw_scale, scalar2=None, op0=ALU.mult), reads=[pa], writes=[iwc])
            for qt in range(4):
                L = c * TC + 128 * (qt + 1)
                sa = accr.next()
                for kc in range(c + 1):
                    n = min(TC, L - kc * TC)
                    for h in range(8):
                        b0 = 64 * (h % 2)
                        stb = st_ring.next()
                        k.op(k.pe, lambda: nc.tensor.matmul(stb.ap[:, 0:n], lhsT=iqTc.ap[b0:b0 + 64, h // 2, qt * 128:(qt + 1) * 128], rhs=IKK.ap[b0:b0 + 64, kc * TC:kc * TC + n], start=True, stop=True),
                             reads=[iqTc, IKK], writes=[stb])
                        rl = rlr.next()
                        k.op(k.act, lambda: nc.scalar.activation(out=rl.ap[:, 0:n], in_=stb.ap[:, 0:n], func=AF.Relu), reads=[stb], writes=[rl])
                        if h == 0:
                            k.op(k.dve, lambda: nc.vector.tensor_scalar(out=sa.ap[:, kc * TC:kc * TC + n], in0=rl.ap[:, 0:n], scalar1=iwc.ap[:, qt, 0:1], scalar2=None, op0=ALU.mult), reads=[rl, iwc], writes=[sa])
                        else:
                            k.op(k.dve, lambda: nc.vector.scalar_tensor_tensor(out=sa.ap[:, kc * TC:kc * TC + n], in0=rl.ap[:, 0:n], scalar=iwc.ap[:, qt, h:h + 1], in1=sa.ap[:, kc * TC:kc * TC + n], op0=ALU.mult, op1=ALU.add),
                                 reads=[rl, iwc, sa], writes=[sa])
                k.op(k.pool, lambda: nc.gpsimd.affine_select(out=sa.ap[:, L - 128:L], in_=sa.ap[:, L - 128:L], pattern=[[-1, 128]], compare_op=ALU.is_ge, fill=NEG, base=0, channel_multiplier=1),
                     reads=[sa], writes=[sa])
                k.op(k.dve, lambda: nc.vector.memset(lo.ap, -32.0), writes=[lo])
                k.op(k.dve, lambda: nc.vector.memset(mid.ap, 0.0), writes=[mid])
                w = 64.0
                for it in range(NIT):
                    k.op(k.dve, lambda: nc.vector.tensor_scalar(out=mb.ap[:, 0:L], in0=sa.ap[:, 0:L], scalar1=mid.ap, scalar2=None, op0=ALU.is_ge, op1=ALU.add, accum_out=cnt.ap), reads=[sa, mid], writes=[mb, cnt])
                    k.op(k.dve, lambda: nc.vector.tensor_scalar(out=tt.ap, in0=cnt.ap, scalar1=256.0, scalar2=w / 2, op0=ALU.is_ge, op1=ALU.mult), reads=[cnt], writes=[tt])
                    k.op(k.dve, lambda: nc.vector.tensor_tensor(out=lo.ap, in0=lo.ap, in1=tt.ap, op=ALU.add), reads=[lo, tt], writes=[lo])
                    k.op(k.dve, lambda: nc.vector.tensor_scalar(out=mid.ap, in0=lo.ap, scalar1=w / 4, scalar2=None, op0=ALU.add), reads=[lo], writes=[mid])
                    w = w / 2
                k.op(k.dve, lambda: nc.vector.tensor_scalar(out=mb.ap[:, 0:L], in0=sa.ap[:, 0:L], scalar1=lo.ap, scalar2=None, op0=ALU.is_ge), reads=[sa, lo], writes=[mb])
                nkt = L // 128
                for k0 in range(0, nkt, 4):
                    nb = min(4, nkt - k0)
                    pbk = misc.next()
                    for i in range(nb):
                        k.op(k.pe, lambda: nc.tensor.transpose(pbk.ap[:, i * 128:(i + 1) * 128], mb.ap[:, (k0 + i) * 128:(k0 + i + 1) * 128], self.ident_f.ap), reads=[mb, self.ident_f], writes=[pbk], signal=(i == nb - 1))
                    k.op(k.act, lambda: nc.scalar.copy(out=maskT.ap[:, k0:k0 + nb, qt * 128:(qt + 1) * 128], in_=pbk.ap[:, 0:nb * 128].rearrange("p (a b) -> p a b", a=nb)), reads=[pbk], writes=[maskT])
            for h in range(16):
                b0 = 64 * (h % 2)
                self.attn_core(c,
                               lambda j: [KK.ap[b0:b0 + 64, j * 128:(j + 1) * 128]],
                               lambda off, n: [qTc.ap[b0:b0 + 64, h // 2, off:off + n]],
                               [KK, qTc], lambda j: Vaug.ap[:, j, 0:65], Vaug, 64, scale, st_ring, pts, acc, tri=False,
                               mask_fn=lambda j, off, n: maskT.ap[:, j, off:off + n], maskbuf=maskT, mask_eng=lambda j: (k.pool if j % 3 != 2 else k.dve))
                if h % 2 == 0:
                    onb = onp.next()
                self.evac_norm(acc, 64, onb, lambda qt: onb.ap[:, qt, b0:b0 + 64], recr)
                if h % 2 == 1:
                    self.transpose_out(onb, lambda qt: onb.ap[:, qt, :], misc.next(), oTc, oTc.ap[:, h // 2, :], k.act)
            k.dma(k.sp, oc_ds, oT_v[:, :, cols], oTc.ap, reads=[oTc], writes=[self.oT])


_PROG_CACHE = {}


def _get_prog(n_layers=DEPTH):
    if n_layers not in _PROG_CACHE:
        _PROG_CACHE[n_layers] = Prog(n_layers)
    return _PROG_CACHE[n_layers]


def _host_inputs(inputs):
    f = lambda a: np.ascontiguousarray(np.asarray(a, dtype=np.float32))
    an = f(inputs["attn_norm"]).reshape(4, 8, 128).transpose(2, 0, 1).reshape(128, 32)
    fn = f(inputs["ffn_norm"]).reshape(4, 8, 128).transpose(2, 0, 1).reshape(128, 32)
    fin = f(inputs["final_norm"]).reshape(8, 128).T
    qn = f(inputs["mla_q_norm"]).reshape(3, 128).T
    kn = f(inputs["mla_kv_norm"]).reshape(2, 128).T
    small = f(np.concatenate([an, fn, fin, qn, kn], axis=1))
    lam = f(np.broadcast_to(f(inputs["diff_lambda"]).reshape(1, 512), (128, 512)))
    subln = f(np.broadcast_to(f(inputs["diff_subln"]).reshape(1, 256), (128, 256)))
    shared = {"small": small, "lam": lam, "subln": subln}
    for l in range(DEPTH):
        m, j = l % 3, l // 3
        if m == 0:
            shared["wqkv%d" % l] = f(inputs["diff_wqkv"][j])
            shared["wo%d" % l] = f(inputs["diff_wo"][j])
        elif m == 1:
            shared["win%d" % l] = f(inputs["dsa_win"][j])
            shared["wo%d" % l] = f(inputs["dsa_wo"][j])
        else:
            shared["wdown%d" % l] = f(inputs["mla_wdown"][j])
            shared["wuq%d" % l] = f(inputs["mla_wuq"][j])
            shared["wukv%d" % l] = f(inputs["mla_wukv"][j])
            shared["wo%d" % l] = f(inputs["mla_wo"][j])
        shared["w1_%d" % l] = f(inputs["ffn_w1"][l])
        shared["w3_%d" % l] = f(inputs["ffn_w3"][l])
        shared["w2_%d" % l] = f(inputs["ffn_w2"][l])
    return shared


def kernel(**inputs):
    x = np.asarray(inputs["x"], dtype=np.float32)
    B = x.shape[0]
    shared = _host_inputs(inputs)
    prog = _get_prog(DEPTH)
    in_maps = []
    for b in range(B):
        m = dict(shared)
        m["x"] = np.ascontiguousarray(x[b])
        in_maps.append(m)
    res = run_bass_kernel_spmd(prog.nc, in_maps, core_ids=list(range(B)))
    return np.stack([np.asarray(r["out"], dtype=np.float32) for r in res.results], axis=0)
```

```python
import math
from contextlib import ExitStack
import numpy as np
import concourse.bass as bass
import concourse.mybir as mybir
from concourse.bass_utils import run_bass_kernel_spmd

F32 = mybir.dt.float32
BF16 = mybir.dt.bfloat16
I32 = mybir.dt.int32
ALU = mybir.AluOpType
AF = mybir.ActivationFunctionType
AX = mybir.AxisListType

S = 4096
D = 1024
DFF = 2816
NCH = 8
TC = 512
EPS = 1e-6
DEPTH = 4
NEG = -1.0e30


class Sem:
    def __init__(self, nc, name):
        self.h = nc.alloc_semaphore(name)
        self.count = 0


class Tok:
    __slots__ = ("sem", "val")

    def __init__(self, sem=None, val=0):
        self.sem = sem
        self.val = val


class Buf:
    __slots__ = ("ap", "w", "r", "excl")

    def __init__(self, ap, excl=False):
        self.ap = ap
        self.w = None
        self.r = {}
        self.excl = excl


class Eng:
    def __init__(self, k, eng, name, is_pe=False):
        self.eng = eng
        self.sem = Sem(k.nc, "e_" + name)
        self.seen = {}
        self.is_pe = is_pe
        self.name = name

    def wait(self, sem, val):
        if val <= self.seen.get(sem, 0):
            return
        self.eng.wait_ge(sem.h, val)
        self.seen[sem] = val


class K:
    def __init__(self, nc):
        self.nc = nc
        self.pe = Eng(self, nc.tensor, "pe", True)
        self.act = Eng(self, nc.scalar, "act")
        self.dve = Eng(self, nc.vector, "dve")
        self.pool = Eng(self, nc.gpsimd, "pool")
        self.sp = Eng(self, nc.sync, "sp")
        self.engs = [self.pe, self.act, self.dve, self.pool, self.sp]
        self.dsems = []
        self.fixed = []
        self.nsem = 0

    def dsem(self, name=None):
        if self.nsem < len(self.dsems):
            s = self.dsems[self.nsem]
        else:
            s = Sem(self.nc, "d%d" % self.nsem)
            self.dsems.append(s)
        self.nsem += 1
        return s

    def _w(self, e, tok):
        if e.is_pe and tok.sem is e.sem:
            return
        e.wait(tok.sem, tok.val)

    def _deps(self, e, reads, writes):
        for b in reads:
            if b.w is not None:
                self._w(e, b.w)
            if b.excl:
                for t in b.r.values():
                    if t.sem is not e.sem:
                        self._w(e, t)
        for b in writes:
            if b.w is not None:
                self._w(e, b.w)
            for t in b.r.values():
                self._w(e, t)

    def _upd(self, tok, reads, writes):
        for b in writes:
            b.w = tok
            b.r = {}
        for b in reads:
            if b not in writes:
                o = b.r.get(tok.sem)
                if o is None or o.val < tok.val or o is tok:
                    b.r[tok.sem] = tok

    def op(self, e, fn, reads=(), writes=(), signal=True):
        self._deps(e, reads, writes)
        inst = fn()
        if signal:
            e.sem.count += 1
            inst.then_inc(e.sem.h, 1)
            tok = Tok(e.sem, e.sem.count)
        else:
            tok = Tok(e.sem, e.sem.count + 1)
        self._upd(tok, reads, writes)
        return inst

    def dma(self, q, dsem, out_ap, in_ap, reads=(), writes=(), group=None, **kw):
        self._deps(q, reads, writes)
        if group is None or group.val == 0:
            q.wait(dsem, dsem.count)
        inst = q.eng.dma_start(out=out_ap, in_=in_ap, **kw)
        dsem.count += 16
        inst.then_inc(dsem.h, 16)
        if group is not None:
            group.sem = dsem
            group.val = dsem.count
            tok = group
        else:
            tok = Tok(dsem, dsem.count)
        self._upd(tok, reads, writes)
        return inst

    def barrier(self):
        self.nsem = 0
        for e in self.engs:
            for o in self.engs[:4]:
                if o is not e and o.sem.count > 0:
                    e.wait(o.sem, o.sem.count)
            for d in self.dsems + self.fixed:
                if d.count > 0:
                    e.wait(d, d.count)


class Ring:
    def __init__(self, bufs):
        self.bufs = bufs
        self.i = 0

    def next(self):
        b = self.bufs[self.i % len(self.bufs)]
        self.i += 1
        return b


class Prog:
    def __init__(self, n_layers=DEPTH):
        self.n_layers = n_layers
        nc = bass.Bass("TRN2", target_bir_lowering=False)
        self.nc = nc
        self.k = K(nc)
        self.build()

    def sb(self, st, name, shape, dt):
        self.uid = getattr(self, "uid", 0) + 1
        t = st.enter_context(self.nc.sbuf_tensor("s%d_%s" % (self.uid, name), list(shape), dt))
        return Buf(t[:])

    def din(self, name, shape, dt=F32):
        return self.nc.dram_tensor(name, list(shape), dt, kind="ExternalInput").ap()

    def dscr(self, name, shape, dt):
        import os as _os
        if _os.environ.get("KDEBUG_OUT") and name in ("uT", "cs_d", "oT", "hT"):
            return Buf(self.nc.dram_tensor(name, list(shape), dt, kind="ExternalOutput").ap())
        return Buf(self.nc.dram_tensor(name, list(shape), dt).ap())

    def build(self):
        nc, k = self.nc, self.k
        self.x = self.din("x", [S, D])
        self.out = nc.dram_tensor("out", [S, D], F32, kind="ExternalOutput").ap()
        self.p_small = self.din("small", [128, 32 + 32 + 8 + 3 + 2])
        self.p_lam = self.din("lam", [128, 512])
        self.p_subln = self.din("subln", [128, 256])
        self.wspec = {}
        wl = []
        for l in range(DEPTH):
            m = l % 3
            if m == 0:
                wl.append([("wqkv%d" % l, D, 3072), ("wo%d" % l, D, D)])
            elif m == 1:
                wl.append([("win%d" % l, D, 1736), ("wo%d" % l, D, D)])
            else:
                wl.append([("wdown%d" % l, D, 704), ("wuq%d" % l, 384, 1536), ("wukv%d" % l, 256, 2048), ("wo%d" % l, D, D)])
            wl[-1] += [("w1_%d" % l, D, DFF), ("w3_%d" % l, D, DFF), ("w2_%d" % l, DFF, D)]
        self.wf = {}
        self.wb = {}
        for l in range(DEPTH):
            for (n, kk, nn) in wl[l]:
                self.wf[n] = self.din(n, [kk, nn])
                self.wb[n] = self.dscr(n + "_b", [kk, nn], BF16)
        self.wl = wl
        self.hT = [self.dscr("hT", [D, S], F32)]
        self.uT = self.dscr("uT", [D, S], BF16)
        self.oT = self.dscr("oT", [D, S], BF16)
        self.cs_d = self.dscr("cs_d", [128, 2, S], F32)

        with ExitStack() as st0:
            self.setup_persistent(st0)
            self.cast_weights()
            with ExitStack() as st:
                self.rope_tables(st)
            k.barrier()
            with ExitStack() as st:
                self.post_phase(st, -1)
            for l in range(self.n_layers):
                k.barrier()
                m = l % 3
                import os as _os
                with ExitStack() as st:
                    if _os.environ.get("KSKIP_ATTN"):
                        z = self.sb(st, "zz", [128, S], BF16)
                        k.op(k.dve, lambda: nc.vector.memset(z.ap, 0.0), writes=[z])
                        for i in range(8):
                            k.dma(k.sp, k.dsem(), self.oT.ap[i * 128:(i + 1) * 128, :], z.ap, reads=[z], writes=[self.oT])
                    elif m == 0:
                        self.diff_phase(st, l)
                    elif m == 1:
                        self.dsa_phase(st, l)
                    else:
                        self.mla_phase(st, l)
                k.barrier()
                with ExitStack() as st:
                    if not _os.environ.get("KSKIP_POST"):
                        self.post_phase(st, l)
            k.barrier()

    def setup_persistent(self, st):
        nc, k = self.nc, self.k
        self.ps = [Buf(nc.alloc_psum_tensor("ps%d" % i, [128, 512], F32)[:], excl=True) for i in range(8)]
        self.ident_f = self.sb(st, "ident_f", [128, 128], F32)
        self.ident_b = self.sb(st, "ident_b", [128, 128], BF16)
        self.ones_f = self.sb(st, "ones_f", [128, 128], F32)
        self.tri = self.sb(st, "tri", [128, 128], BF16)
        self.rperm = self.sb(st, "rperm", [128, 128], BF16)
        self.small = self.sb(st, "small", [128, 77], F32)
        self.lam = self.sb(st, "lamsb", [128, 512], F32)
        self.subln = self.sb(st, "sublnsb", [128, 256], F32)
        self.neglam = self.sb(st, "neglam", [128, 2], F32)
        self.epsb = self.sb(st, "epsb", [128, 1], F32)
        k.op(k.dve, lambda: nc.vector.memset(self.epsb.ap, EPS), writes=[self.epsb])
        k.dma(k.sp, k.dsem(), self.small.ap, self.p_small, writes=[self.small])
        k.dma(k.sp, k.dsem(), self.lam.ap, self.p_lam, writes=[self.lam])
        k.dma(k.sp, k.dsem(), self.subln.ap, self.p_subln, writes=[self.subln])
        with ExitStack() as t:
            io = self.sb(t, "io", [128, 128], I32)
            dif = self.sb(t, "dif", [128, 128], F32)
            ta = self.sb(t, "ta", [128, 128], F32)
            tb = self.sb(t, "tb", [128, 128], F32)
            co = self.sb(t, "co", [128, 128], F32)
            ioj = self.sb(t, "ioj", [128, 128], I32)
            k.op(k.pool, lambda: nc.gpsimd.iota(io.ap, pattern=[[1, 128]], base=0, channel_multiplier=-1), writes=[io])
            k.op(k.dve, lambda: nc.vector.tensor_copy(out=dif.ap, in_=io.ap), reads=[io], writes=[dif])
            k.op(k.dve, lambda: nc.vector.tensor_single_scalar(out=self.ident_f.ap, in_=dif.ap, scalar=0.0, op=ALU.is_equal), reads=[dif], writes=[self.ident_f])
            k.op(k.dve, lambda: nc.vector.tensor_copy(out=self.ident_b.ap, in_=self.ident_f.ap), reads=[self.ident_f], writes=[self.ident_b])
            k.op(k.dve, lambda: nc.vector.memset(self.ones_f.ap, 1.0), writes=[self.ones_f])
            k.op(k.dve, lambda: nc.vector.tensor_single_scalar(out=self.tri.ap, in_=dif.ap, scalar=0.0, op=ALU.is_ge), reads=[dif], writes=[self.tri])
            k.op(k.pool, lambda: nc.gpsimd.iota(ioj.ap, pattern=[[1, 128]], base=0, channel_multiplier=0), writes=[ioj])
            k.op(k.dve, lambda: nc.vector.tensor_single_scalar(out=ioj.ap, in_=ioj.ap, scalar=32, op=ALU.bitwise_and), reads=[ioj], writes=[ioj])
            k.op(k.dve, lambda: nc.vector.tensor_copy(out=co.ap, in_=ioj.ap), reads=[ioj], writes=[co])
            k.op(k.dve, lambda: nc.vector.tensor_single_scalar(out=co.ap, in_=co.ap, scalar=16.0, op=ALU.is_ge), reads=[co], writes=[co])
            k.op(k.dve, lambda: nc.vector.tensor_single_scalar(out=ta.ap, in_=dif.ap, scalar=32.0, op=ALU.is_equal), reads=[dif], writes=[ta])
            k.op(k.dve, lambda: nc.vector.tensor_single_scalar(out=tb.ap, in_=dif.ap, scalar=-32.0, op=ALU.is_equal), reads=[dif], writes=[tb])
            k.op(k.dve, lambda: nc.vector.tensor_tensor(out=ta.ap, in0=ta.ap, in1=co.ap, op=ALU.mult), reads=[ta, co], writes=[ta])
            k.op(k.dve, lambda: nc.vector.tensor_scalar(out=co.ap, in0=co.ap, scalar1=-1.0, scalar2=1.0, op0=ALU.mult, op1=ALU.add), reads=[co], writes=[co])
            k.op(k.dve, lambda: nc.vector.tensor_tensor(out=tb.ap, in0=tb.ap, in1=co.ap, op=ALU.mult), reads=[tb, co], writes=[tb])
            k.op(k.dve, lambda: nc.vector.tensor_tensor(out=self.rperm.ap, in0=ta.ap, in1=tb.ap, op=ALU.subtract), reads=[ta, tb], writes=[self.rperm])
            pr = self.sb(t, "lampr", [128, 256], F32)
            sm = self.sb(t, "lamsm", [128, 4], F32)
            for j in range(2):
                layer = 3 * j
                li = 0.8 - 0.6 * math.exp(-0.3 * layer)
                lm = self.lam.ap[:, j * 256:(j + 1) * 256]
                k.op(k.dve, lambda: nc.vector.tensor_tensor(out=pr.ap[:, 0:64], in0=lm[:, 0:64], in1=lm[:, 64:128], op=ALU.mult), reads=[self.lam], writes=[pr])
                k.op(k.dve, lambda: nc.vector.tensor_tensor(out=pr.ap[:, 64:128], in0=lm[:, 128:192], in1=lm[:, 192:256], op=ALU.mult), reads=[self.lam], writes=[pr])
                k.op(k.dve, lambda: nc.vector.tensor_reduce(out=sm.ap[:, 0:2], in_=pr.ap[:, 0:128].rearrange("p (a b) -> p a b", a=2), axis=AX.X, op=ALU.add), reads=[pr], writes=[sm])
                k.op(k.act, lambda: nc.scalar.activation(out=sm.ap[:, 2:4], in_=sm.ap[:, 0:2], func=AF.Exp), reads=[sm], writes=[sm])
                k.op(k.dve, lambda: nc.vector.tensor_tensor(out=sm.ap[:, 0:1], in0=sm.ap[:, 3:4], in1=sm.ap[:, 2:3], op=ALU.subtract), reads=[sm], writes=[sm])
                k.op(k.dve, lambda: nc.vector.tensor_scalar(out=self.neglam.ap[:, j:j + 1], in0=sm.ap[:, 0:1], scalar1=-li, scalar2=None, op0=ALU.add), reads=[sm], writes=[self.neglam])
            for j in range(2):
                li = 0.8 - 0.6 * math.exp(-0.3 * 3 * j)
                sl = self.subln.ap[:, j * 128:(j + 1) * 128]
                k.op(k.dve, lambda: nc.vector.tensor_scalar(out=sl, in0=sl, scalar1=1.0 - li, scalar2=None, op0=ALU.mult), reads=[self.subln], writes=[self.subln])
            k.barrier()

    def g_attn(self, l, dc):
        return self.small.ap[:, l * 8 + dc:l * 8 + dc + 1]

    def g_ffn(self, l, dc):
        return self.small.ap[:, 32 + l * 8 + dc:32 + l * 8 + dc + 1]

    def g_final(self, dc):
        return self.small.ap[:, 64 + dc:64 + dc + 1]

    def cast_weights(self):
        k = self.k
        for l in range(DEPTH):
            ds = Sem(self.nc, "cast%d" % l)
            k.fixed.append(ds)
            g = Tok()
            for (n, kk, nn) in self.wl[l]:
                src, dst = self.wf[n], self.wb[n]
                nsplit = 1 if nn <= 2048 else 2
                w = nn // nsplit
                for i in range(nsplit):
                    k.dma(k.pool, ds, dst.ap[:, i * w:(i + 1) * w], src[:, i * w:(i + 1) * w], writes=[dst], group=g)

    def rope_tables(self, st):
        nc, k = self.nc, self.k
        pi = self.sb(st, "r_pi", [128, 1], I32)
        pf = self.sb(st, "r_pf", [128, 1], F32)
        inv = self.sb(st, "r_inv", [128, 1], F32)
        k.op(k.pool, lambda: nc.gpsimd.iota(pi.ap, pattern=[[0, 1]], base=0, channel_multiplier=1), writes=[pi])
        k.op(k.dve, lambda: nc.vector.tensor_single_scalar(out=pi.ap, in_=pi.ap, scalar=31, op=ALU.bitwise_and), reads=[pi], writes=[pi])
        k.op(k.dve, lambda: nc.vector.tensor_copy(out=pf.ap, in_=pi.ap), reads=[pi], writes=[pf])
        k.op(k.act, lambda: nc.scalar.activation(out=inv.ap, in_=pf.ap, func=AF.Exp, scale=-math.log(10000.0) / 32.0), reads=[pf], writes=[inv])
        k.op(k.dve, lambda: nc.vector.tensor_scalar(out=inv.ap, in0=inv.ap, scalar1=1.0 / (2.0 * math.pi), scalar2=None, op0=ALU.mult), reads=[inv], writes=[inv])
        W = 1024
        ti = self.sb(st, "r_ti", [128, W], I32)
        tt = self.sb(st, "r_tt", [128, W], F32)
        ff = self.sb(st, "r_ff", [128, W], F32)
        ni = self.sb(st, "r_ni", [128, W], I32)
        nf = self.sb(st, "r_nf", [128, W], F32)
        g = self.sb(st, "r_g", [128, W], F32)
        res = [self.sb(st, "r_res%d" % i, [128, 2, W], F32) for i in range(2)]
        dss = [k.dsem(), k.dsem()]
        for c in range(S // W):
            rb = res[c % 2]
            k.op(k.pool, lambda: nc.gpsimd.iota(ti.ap, pattern=[[1, W]], base=c * W, channel_multiplier=0), writes=[ti])
            k.op(k.dve, lambda: nc.vector.tensor_copy(out=tt.ap, in_=ti.ap), reads=[ti], writes=[tt])
            k.op(k.dve, lambda: nc.vector.tensor_scalar(out=tt.ap, in0=tt.ap, scalar1=inv.ap, scalar2=None, op0=ALU.mult), reads=[tt, inv], writes=[tt])
            for which in range(2):
                off = 0.25 if which == 0 else 0.0
                k.op(k.dve, lambda: nc.vector.tensor_scalar(out=ff.ap, in0=tt.ap, scalar1=off, scalar2=None, op0=ALU.add), reads=[tt], writes=[ff])
                k.op(k.dve, lambda: nc.vector.tensor_copy(out=ni.ap, in_=ff.ap), reads=[ff], writes=[ni])
                k.op(k.dve, lambda: nc.vector.tensor_copy(out=nf.ap, in_=ni.ap), reads=[ni], writes=[nf])
                k.op(k.dve, lambda: nc.vector.tensor_tensor(out=ff.ap, in0=ff.ap, in1=nf.ap, op=ALU.subtract), reads=[ff, nf], writes=[ff])
                k.op(k.dve, lambda: nc.vector.tensor_single_scalar(out=g.ap, in_=ff.ap, scalar=0.5, op=ALU.is_gt), reads=[ff], writes=[g])
                k.op(k.dve, lambda: nc.vector.tensor_tensor(out=ff.ap, in0=ff.ap, in1=g.ap, op=ALU.subtract), reads=[ff, g], writes=[ff])
                k.op(k.dve, lambda: nc.vector.tensor_single_scalar(out=g.ap, in_=ff.ap, scalar=-0.5, op=ALU.is_lt), reads=[ff], writes=[g])
                k.op(k.dve, lambda: nc.vector.tensor_tensor(out=ff.ap, in0=ff.ap, in1=g.ap, op=ALU.add), reads=[ff, g], writes=[ff])
                k.op(k.act, lambda: nc.scalar.activation(out=rb.ap[:, which, :], in_=ff.ap, func=AF.Sin, scale=2.0 * math.pi * (1.0 - 1e-6)), reads=[ff], writes=[rb])
            k.dma(k.sp, dss[c % 2], self.cs_d.ap[:, :, c * W:(c + 1) * W], rb.ap, reads=[rb], writes=[self.cs_d])

    def rmsnorm_fm(self, src, nchunk, width, gain_fn, dst, dst_ap_fn, sqr, ssps, lnb, rstd, dim, src_ap_fn=None, src_bufs=None):
        nc, k = self.nc, self.k
        sbufs = src_bufs if src_bufs is not None else [src]
        for dc in range(nchunk):
            sq = sqr.next()
            sap = src_ap_fn(dc)
            k.op(k.act, lambda: nc.scalar.activation(out=sq.ap[:, :width], in_=sap, func=AF.Square), reads=sbufs, writes=[sq])
            k.op(k.pe, lambda: nc.tensor.matmul(ssps.ap[:, :width], lhsT=self.ones_f.ap, rhs=sq.ap[:, :width], start=(dc == 0), stop=(dc == nchunk - 1)),
                 reads=[sq, self.ones_f], writes=[ssps])
        k.op(k.act, lambda: nc.scalar.activation(out=lnb.ap[:, :width], in_=ssps.ap[:, :width], func=AF.Ln, scale=1.0 / dim, bias=self.epsb.ap), reads=[ssps, self.epsb], writes=[lnb])
        k.op(k.act, lambda: nc.scalar.activation(out=rstd.ap[:, :width], in_=lnb.ap[:, :width], func=AF.Exp, scale=-0.5), reads=[lnb], writes=[rstd])
        for dc in range(nchunk):
            sap = src_ap_fn(dc)
            dap = dst_ap_fn(dc)
            k.op(k.dve, lambda: nc.vector.scalar_tensor_tensor(out=dap, in0=sap, scalar=gain_fn(dc), in1=rstd.ap[:, :width], op0=ALU.mult, op1=ALU.mult),
                 reads=sbufs + [rstd, self.small], writes=[dst])

    def eps_ap(self):
        return self.epsb.ap

    def post_phase(self, st, l):
        nc, k = self.nc, self.k
        first = (l < 0)
        last = (l == self.n_layers - 1)
        hcs = Ring([self.sb(st, "hc%d" % i, [128, 8, TC], F32) for i in range(2)])
        h_ds = [k.dsem(), k.dsem()]
        hst_ds = [k.dsem(), k.dsem()]
        sqr = Ring([self.sb(st, "sq%d" % i, [128, TC], F32) for i in range(2)])
        lnb = self.sb(st, "lnb", [128, TC], F32)
        rstd = self.sb(st, "rstd", [128, TC], F32)
        if not last:
            uns = Ring([self.sb(st, "un%d" % i, [128, 8, TC], BF16) for i in range(1)])
            un_ds = [k.dsem(), k.dsem()]
        ssps = self.ps[7]
        hT_v = self.hT[0].ap.rearrange("(c p) t -> p c t", p=128)
        uT_v = self.uT.ap.rearrange("(c p) t -> p c t", p=128)
        oT_v = self.oT.ap.rearrange("(c p) t -> p c t", p=128)
        if first:
            xts = Ring([self.sb(st, "xt%d" % i, [128, D], F32) for i in range(8)])
            x_ds = [k.dsem() for _ in range(8)]
            psr = Ring(self.ps[0:6])
        else:
            ocs = Ring([self.sb(st, "oc%d" % i, [128, 8, TC], BF16) for i in range(2)])
            o_ds = [k.dsem(), k.dsem()]
            ucs = Ring([self.sb(st, "uc%d" % i, [128, 8, TC], BF16) for i in range(2)])
            gT = self.sb(st, "gT", [128, 22, TC], BF16)
            sil = Ring([self.sb(st, "sil%d" % i, [128, TC], F32) for i in range(2)])
            wo = self.sb(st, "wo", [128, 8, D], BF16)
            w2 = self.sb(st, "w2", [128, 22, D], BF16)
            w13 = Ring([self.sb(st, "w13_%d" % i, [128, 2, 8, 512], BF16) for i in range(2)])
            w_ds = [k.dsem(), k.dsem()]
            wres_ds = k.dsem()
            psr = Ring(self.ps[0:6])
            wob, w1b, w3b, w2b = self.wb["wo%d" % l], self.wb["w1_%d" % l], self.wb["w3_%d" % l], self.wb["w2_%d" % l]
            k.dma(k.sp, wres_ds, wo.ap, wob.ap.rearrange("(c p) f -> p c f", p=128), reads=[wob], writes=[wo])
            k.dma(k.sp, k.dsem(), w2.ap, w2b.ap.rearrange("(c p) f -> p c f", p=128), reads=[w2b], writes=[w2])
            w1v = w1b.ap.rearrange("(c p) f -> p c f", p=128)
            w3v = w3b.ap.rearrange("(c p) f -> p c f", p=128)
            fgs = [(i * 512, 512) for i in range(5)] + [(2560, 256)]
        if last:
            orow = Ring([self.sb(st, "orow%d" % i, [128, D], F32) for i in range(2)])
            or_ds = [k.dsem(), k.dsem()]

        def load_chunk(c):
            hc = hcs.bufs[c % 2]
            cols = slice(c * TC, (c + 1) * TC)
            if not first:
                k.dma(k.sp, h_ds[c % 2], hc.ap, hT_v[:, :, cols], reads=[self.hT[0]], writes=[hc])
                oc = ocs.bufs[c % 2]
                k.dma(k.sp, o_ds[c % 2], oc.ap, oT_v[:, :, cols], reads=[self.oT], writes=[oc])

        if not first:
            load_chunk(0)
        for c in range(NCH):
            cols = slice(c * TC, (c + 1) * TC)
            hc = hcs.bufs[c % 2]
            if first:
                xtl = []
                for i in range(4):
                    xt = xts.next()
                    k.dma(k.sp, x_ds[(xts.i - 1) % 8], xt.ap, self.x[c * TC + i * 128:c * TC + (i + 1) * 128, :], writes=[xt])
                    xtl.append(xt)
                for dc in range(8):
                    pb = psr.next()
                    for i in range(4):
                        k.op(k.pe, lambda: nc.tensor.transpose(pb.ap[:, i * 128:(i + 1) * 128], xtl[i].ap[:, dc * 128:(dc + 1) * 128], self.ident_f.ap),
                             reads=[xtl[i], self.ident_f], writes=[pb], signal=(i == 3))
                    e = k.act if dc % 2 == 0 else k.dve
                    if e is k.act:
                        k.op(e, lambda: nc.scalar.copy(out=hc.ap[:, dc, :], in_=pb.ap), reads=[pb], writes=[hc])
                    else:
                        k.op(e, lambda: nc.vector.tensor_copy(out=hc.ap[:, dc, :], in_=pb.ap), reads=[pb], writes=[hc])
            else:
                if c + 1 < NCH:
                    load_chunk(c + 1)
                oc = ocs.bufs[c % 2]
                for dc in range(8):
                    pb = psr.next()
                    for cc in range(8):
                        k.op(k.pe, lambda: nc.tensor.matmul(pb.ap, lhsT=wo.ap[:, cc, dc * 128:(dc + 1) * 128], rhs=oc.ap[:, cc, :], start=(cc == 0), stop=(cc == 7)),
                             reads=[wo, oc], writes=[pb], signal=(cc == 7))
                    k.op(k.dve, lambda: nc.vector.tensor_tensor(out=hc.ap[:, dc, :], in0=pb.ap, in1=hc.ap[:, dc, :], op=ALU.add), reads=[pb, hc], writes=[hc])
                uc = ucs.next()
                self.rmsnorm_fm(hc, 8, TC, lambda dc: self.g_ffn(l, dc), uc, lambda dc: uc.ap[:, dc, :], sqr, ssps, lnb, rstd, float(D),
                                src_ap_fn=lambda dc: hc.ap[:, dc, :])
                def load_w13(gi):
                    f0, fw = fgs[gi]
                    wt = w13.bufs[gi % 2]
                    g = Tok()
                    k.dma(k.sp, w_ds[gi % 2], wt.ap[:, 0, :, :fw], w1v[:, :, f0:f0 + fw], reads=[w1b], writes=[wt], group=g)
                    k.dma(k.sp, w_ds[gi % 2], wt.ap[:, 1, :, :fw], w3v[:, :, f0:f0 + fw], reads=[w3b], writes=[wt], group=g)
                load_w13(0)
                for gi, (f0, fw) in enumerate(fgs):
                    if gi + 1 < len(fgs):
                        load_w13(gi + 1)
                    wt = w13.bufs[gi % 2]
                    for fi in range(fw // 128):
                        fc = f0 // 128 + fi
                        p1 = psr.next()
                        p3 = psr.next()
                        for dc in range(8):
                            k.op(k.pe, lambda: nc.tensor.matmul(p1.ap, lhsT=wt.ap[:, 0, dc, fi * 128:(fi + 1) * 128], rhs=uc.ap[:, dc, :], start=(dc == 0), stop=(dc == 7)),
                                 reads=[wt, uc], writes=[p1], signal=(dc == 7))
                        for dc in range(8):
                            k.op(k.pe, lambda: nc.tensor.matmul(p3.ap, lhsT=wt.ap[:, 1, dc, fi * 128:(fi + 1) * 128], rhs=uc.ap[:, dc, :], start=(dc == 0), stop=(dc == 7)),
                                 reads=[wt, uc], writes=[p3], signal=(dc == 7))
                        sb_ = sil.next()
                        k.op(k.act, lambda: nc.scalar.activation(out=sb_.ap, in_=p1.ap, func=AF.Silu), reads=[p1], writes=[sb_])
                        k.op(k.dve, lambda: nc.vector.tensor_tensor(out=gT.ap[:, fc, :], in0=sb_.ap, in1=p3.ap, op=ALU.mult), reads=[sb_, p3], writes=[gT])
                for dc in range(8):
                    pb = psr.next()
                    for fc in range(22):
                        k.op(k.pe, lambda: nc.tensor.matmul(pb.ap, lhsT=w2.ap[:, fc, dc * 128:(dc + 1) * 128], rhs=gT.ap[:, fc, :], start=(fc == 0), stop=(fc == 21)),
                             reads=[w2, gT], writes=[pb], signal=(fc == 21))
                    k.op(k.dve, lambda: nc.vector.tensor_tensor(out=hc.ap[:, dc, :], in0=pb.ap, in1=hc.ap[:, dc, :], op=ALU.add), reads=[pb, hc], writes=[hc])
            if not last:
                k.dma(k.sp, hst_ds[c % 2], hT_v[:, :, cols], hc.ap, reads=[hc], writes=[self.hT[0]])
                un = uns.next()
                self.rmsnorm_fm(hc, 8, TC, lambda dc: self.g_attn(l + 1, dc), un, lambda dc: un.ap[:, dc, :], sqr, ssps, lnb, rstd, float(D),
                                src_ap_fn=lambda dc: hc.ap[:, dc, :])
                k.dma(k.sp, un_ds[c % 2], uT_v[:, :, cols], un.ap, reads=[un], writes=[self.uT])
            else:
                for dc in range(8):
                    sq = sqr.next()
                    k.op(k.act, lambda: nc.scalar.activation(out=sq.ap, in_=hc.ap[:, dc, :], func=AF.Square), reads=[hc], writes=[sq])
                    k.op(k.pe, lambda: nc.tensor.matmul(ssps.ap, lhsT=self.ones_f.ap, rhs=sq.ap, start=(dc == 0), stop=(dc == 7)),
                         reads=[sq, self.ones_f], writes=[ssps])
                k.op(k.act, lambda: nc.scalar.activation(out=lnb.ap, in_=ssps.ap, func=AF.Ln, scale=1.0 / D, bias=self.epsb.ap), reads=[ssps, self.epsb], writes=[lnb])
                k.op(k.act, lambda: nc.scalar.activation(out=rstd.ap, in_=lnb.ap, func=AF.Exp, scale=-0.5), reads=[lnb], writes=[rstd])
                for dc in range(8):
                    k.op(k.dve, lambda: nc.vector.scalar_tensor_tensor(out=hc.ap[:, dc, :], in0=hc.ap[:, dc, :], scalar=self.g_final(dc), in1=rstd.ap, op0=ALU.mult, op1=ALU.mult),
                         reads=[hc, rstd, self.small], writes=[hc])
                for i in range(4):
                    ob = orow.next()
                    for half in range(2):
                        pb = psr.next()
                        for j in range(4):
                            dc = half * 4 + j
                            k.op(k.pe, lambda: nc.tensor.transpose(pb.ap[:, j * 128:(j + 1) * 128], hc.ap[:, dc, i * 128:(i + 1) * 128], self.ident_f.ap),
                                 reads=[hc, self.ident_f], writes=[pb], signal=(j == 3))
                        if half == 0:
                            k.op(k.act, lambda: nc.scalar.copy(out=ob.ap[:, 0:512], in_=pb.ap), reads=[pb], writes=[ob])
                        else:
                            k.op(k.dve, lambda: nc.vector.tensor_copy(out=ob.ap[:, 512:1024], in_=pb.ap), reads=[pb], writes=[ob])
                    k.dma(k.sp, or_ds[(orow.i - 1) % 2], self.out[c * TC + i * 128:c * TC + (i + 1) * 128, :], ob.ap, reads=[ob])

    def attn_core(self, c, lhs_fn, rhs_fn, rbufs, v_fn, vbuf, E, scale, st_ring, pt_ring, acc, tri=True, mask_fn=None, maskbuf=None, mask_eng=None):
        nc, k = self.nc, self.k
        nk = 4 * c + 4

        def emit_st(j):
            r = j - 4 * c
            off = 128 * max(r, 0)
            n = 512 - off
            stb = st_ring.next()
            ls = lhs_fn(j)
            rs = rhs_fn(off, n)
            nl_ = len(ls)
            for i in range(nl_):
                k.op(k.pe, lambda: nc.tensor.matmul(stb.ap[:, off:512], lhsT=ls[i], rhs=rs[i], start=(i == 0), stop=(i == nl_ - 1)),
                     reads=rbufs, writes=[stb], signal=(i == nl_ - 1))
            return stb, r, off, n

        nxt = emit_st(0)
        for j in range(nk):
            stb, r, off, n = nxt
            if j + 1 < nk:
                nxt = emit_st(j + 1)
            pt = pt_ring.next()
            k.op(k.act, lambda: nc.scalar.activation(out=pt.ap[:, off:512], in_=stb.ap[:, off:512], func=AF.Exp, scale=scale), reads=[stb], writes=[pt])
            if r >= 0 and tri:
                k.op(k.pool, lambda: nc.gpsimd.tensor_tensor(out=pt.ap[:, off:off + 128], in0=pt.ap[:, off:off + 128], in1=self.tri.ap, op=ALU.mult),
                     reads=[pt, self.tri], writes=[pt])
            if mask_fn is not None:
                me = mask_eng(j)
                mm = mask_fn(j, off, n)
                k.op(me, lambda: me.eng.tensor_tensor(out=pt.ap[:, off:512], in0=pt.ap[:, off:512], in1=mm, op=ALU.mult), reads=[pt, maskbuf], writes=[pt])
            for qt in range(max(r, 0), 4):
                k.op(k.pe, lambda: nc.tensor.matmul(acc[qt].ap[:, 0:E + 1], lhsT=pt.ap[:, qt * 128:(qt + 1) * 128], rhs=v_fn(j), start=(j == 0), stop=(j == 4 * c + qt)),
                     reads=[pt, vbuf], writes=[acc[qt]], signal=(qt == 3))

    def rope_fm(self, pa, np_, cs, cols, dst, dst_ap, xbr, rar, rbr, pb, split=None):
        nc, k = self.nc, self.k
        import os as _os
        lvl = _os.environ.get("KROPE", "z")
        xb = xbr.next()
        ra = rar.next()
        rb = rbr.next()
        k.op(k.act, lambda: nc.scalar.copy(out=xb.ap[0:np_, :], in_=pa.ap[0:np_, :]), reads=[pa], writes=[xb])
        if lvl >= "b":
            k.op(k.pe, lambda: nc.tensor.matmul(pb.ap[0:np_, :], lhsT=self.rperm.ap[0:np_, 0:np_], rhs=xb.ap[0:np_, :], start=True, stop=True), reads=[xb, self.rperm], writes=[pb])
        if lvl >= "c":
            k.op(k.dve, lambda: nc.vector.tensor_tensor(out=ra.ap[0:np_, :], in0=pa.ap[0:np_, :], in1=cs.ap[0:np_, 0, cols], op=ALU.mult), reads=[pa, cs], writes=[ra])
        if lvl >= "d":
            k.op(k.dve, lambda: nc.vector.tensor_tensor(out=rb.ap[0:np_, :], in0=pb.ap[0:np_, :], in1=cs.ap[0:np_, 1, cols], op=ALU.mult), reads=[pb, cs], writes=[rb])
        if split is not None:
            for hh in range(2):
                k.op(k.pool, lambda: nc.gpsimd.tensor_tensor(out=dst[hh].ap[hh * 64:(hh + 1) * 64, split], in0=ra.ap[hh * 64:(hh + 1) * 64, :], in1=rb.ap[hh * 64:(hh + 1) * 64, :], op=ALU.add),
                     reads=[ra, rb], writes=[dst[hh]])
        elif lvl >= "e":
            k.op(k.pool, lambda: nc.gpsimd.tensor_tensor(out=dst_ap, in0=ra.ap[0:np_, :], in1=rb.ap[0:np_, :], op=ALU.add), reads=[ra, rb], writes=[dst])
        else:
            k.op(k.act, lambda: nc.scalar.copy(out=dst_ap, in_=pa.ap[0:np_, :]), reads=[pa], writes=[dst])

    def load_full(self, st, name, dbuf, shape_inner, dt, nsplit=8):
        k = self.k
        t = self.sb(st, name, [128] + shape_inner, dt)
        v = dbuf.ap.rearrange("(c p) t -> p c t", p=128)
        g = Tok()
        ds = k.dsem()
        w = S // nsplit
        for i in range(nsplit):
            k.dma(k.sp, ds, t.ap[:, :, i * w:(i + 1) * w], v[:, :, i * w:(i + 1) * w], reads=[dbuf], writes=[t], group=g)
        return t

    def evac_norm(self, acc, E, dst, dst_ap_fn, recr):
        nc, k = self.nc, self.k
        for qt in range(4):
            rec = recr.next()
            k.op(k.dve, lambda: nc.vector.reciprocal(out=rec.ap, in_=acc[qt].ap[:, E:E + 1]), reads=[acc[qt]], writes=[rec])
            k.op(k.dve, lambda: nc.vector.tensor_scalar(out=dst_ap_fn(qt), in0=acc[qt].ap[:, 0:E], scalar1=rec.ap, scalar2=None, op0=ALU.mult), reads=[acc[qt], rec], writes=[dst])

    def transpose_out(self, src, src_ap_fn, pbuf, dst, dst_ap, eng):
        nc, k = self.nc, self.k
        for qt in range(4):
            k.op(k.pe, lambda: nc.tensor.transpose(pbuf.ap[:, qt * 128:(qt + 1) * 128], src_ap_fn(qt), self.ident_f.ap), reads=[src, self.ident_f], writes=[pbuf], signal=(qt == 3))
        if eng is k.act:
            k.op(eng, lambda: nc.scalar.copy(out=dst_ap, in_=pbuf.ap), reads=[pbuf], writes=[dst])
        else:
            k.op(eng, lambda: nc.vector.tensor_copy(out=dst_ap, in_=pbuf.ap), reads=[pbuf], writes=[dst])

    def diff_phase(self, st, l):
        nc, k = self.nc, self.k
        jj = l // 3
        uT = self.load_full(st, "uTf", self.uT, [8, S], BF16)
        cs = self.sb(st, "cs", [128, 2, S], F32)
        k.dma(k.sp, k.dsem(), cs.ap, self.cs_d.ap, reads=[self.cs_d], writes=[cs])
        wqkv = self.wb["wqkv%d" % l]
        wv = wqkv.ap.rearrange("(c p) f -> p c f", p=128)
        whs = Ring([self.sb(st, "wh%d" % i, [128, 8, 384], BF16) for i in range(2)])
        wh_ds = [k.dsem(), k.dsem()]
        qTz = [self.sb(st, "qTz%d" % i, [128, S], BF16) for i in range(2)]
        k.op(k.dve, lambda: nc.vector.memset(qTz[0].ap[64:128, :], 0.0), writes=[qTz[0]])
        k.op(k.dve, lambda: nc.vector.memset(qTz[1].ap[0:64, :], 0.0), writes=[qTz[1]])
        kTs = Ring([self.sb(st, "kT%d" % i, [128, S], BF16) for i in range(2)])
        Vs = Ring([self.sb(st, "V%d" % i, [128, 32, 130], BF16) for i in range(2)])
        for vb in Vs.bufs:
            k.op(k.dve, lambda: nc.vector.memset(vb.ap[:, :, 128:130], 1.0), writes=[vb])
        xbr = Ring([self.sb(st, "xb%d" % i, [128, TC], BF16) for i in range(2)])
        rar = Ring([self.sb(st, "ra%d" % i, [128, TC], F32) for i in range(2)])
        rbr = Ring([self.sb(st, "rb%d" % i, [128, TC], F32) for i in range(2)])
        pts = Ring([self.sb(st, "pt%d" % i, [128, TC], BF16) for i in range(4)])
        recr = Ring([self.sb(st, "rec%d" % i, [128, 1], F32) for i in range(8)])
        om = [self.sb(st, "om%d" % i, [128, 4, 128], F32) for i in range(2)]
        d4 = self.sb(st, "d4", [128, 4, 128], F32)
        sq4 = self.sb(st, "sq4", [128, 4, 128], F32)
        ss4 = self.sb(st, "ss4", [128, 4], F32)
        ln4 = self.sb(st, "ln4", [128, 4], F32)
        rs4 = self.sb(st, "rs4", [128, 4], F32)
        dn = Ring([self.sb(st, "dn%d" % i, [128, 4, 128], F32) for i in range(2)])
        oTh = Ring([self.sb(st, "oTh%d" % i, [128, S], BF16) for i in range(2)])
        oth_ds = [k.dsem(), k.dsem()]
        st_ring = Ring(self.ps[0:2])
        acc = self.ps[2:6]
        misc = Ring(self.ps[6:8])
        sub_ap = self.subln.ap[:, jj * 128:(jj + 1) * 128].unsqueeze(1).broadcast_to([128, 4, 128])
        scale = 64 ** -0.5
        import os as _os
        for h in range(int(_os.environ.get("KHEADS", "8"))):
            wh = whs.next()
            g = Tok()
            for i in range(3):
                k.dma(k.sp, wh_ds[h % 2], wh.ap[:, :, i * 128:(i + 1) * 128], wv[:, :, i * 1024 + h * 128:i * 1024 + (h + 1) * 128], reads=[wqkv], writes=[wh], group=g)
            kT, V = kTs.next(), Vs.next()
            import os as _os
            stage = float(_os.environ.get("KDIFF_STAGE", "3"))
            for tc in range(NCH):
                if stage < 0.5:
                    break
                if _os.environ.get("KTC1") and tc >= int(_os.environ.get("KTC1")):
                    break
                cols = slice(tc * TC, (tc + 1) * TC)
                for which, dstb in ((0, None), (1, kT)):
                    if _os.environ.get("KSKIPQK"):
                        break
                    pa = misc.next()
                    for dc in range(8):
                        k.op(k.pe, lambda: nc.tensor.matmul(pa.ap, lhsT=wh.ap[:, dc, which * 128:(which + 1) * 128], rhs=uT.ap[:, dc, cols], start=(dc == 0), stop=(dc == 7)),
                             reads=[wh, uT], writes=[pa], signal=(dc == 7))
                    pb = misc.next()
                    if stage < 0.7:
                        k.op(k.act, lambda: nc.scalar.copy(out=dstb.ap[:, cols], in_=pa.ap), reads=[pa], writes=[dstb])
                        continue
                    if which == 0:
                        self.rope_fm(pa, 128, cs, cols, qTz, None, xbr, rar, rbr, pb, split=cols)
                    else:
                        self.rope_fm(pa, 128, cs, cols, dstb, dstb.ap[:, cols], xbr, rar, rbr, pb)
                if _os.environ.get("KSKIPV"):
                    continue
                pa = misc.next()
                for i in range(4):
                    for dc in range(8):
                        k.op(k.pe, lambda: nc.tensor.matmul(pa.ap[:, i * 128:(i + 1) * 128], lhsT=uT.ap[:, dc, tc * TC + i * 128:tc * TC + (i + 1) * 128], rhs=wh.ap[:, dc, 256:384], start=(dc == 0), stop=(dc == 7)),
                             reads=[wh, uT], writes=[pa], signal=(dc == 7 and i == 3))
                k.op(k.act, lambda: nc.scalar.copy(out=V.ap[:, tc * 4:tc * 4 + 4, 0:128], in_=pa.ap.rearrange("p (a b) -> p a b", a=4)), reads=[pa], writes=[V])
            oT_h = oTh.next()
            if stage < 3:
                k.op(k.dve, lambda: nc.vector.memset(oT_h.ap, 0.0), writes=[oT_h])
            for c in range(NCH):
                if stage < 2:
                    break
                for m in range(2):
                    self.attn_core(c,
                                   lambda j: [kT.ap[:, j * 128:(j + 1) * 128]],
                                   lambda off, n: [qTz[m].ap[:, c * TC + off:c * TC + off + n]],
                                   [kT, qTz[m]], lambda j: V.ap[:, j, 0:129], V, 128, scale, st_ring, pts, acc)
                    self.evac_norm(acc, 128, om[m], lambda qt: om[m].ap[:, qt, :], recr)
                if stage < 3:
                    continue
                k.op(k.dve, lambda: nc.vector.scalar_tensor_tensor(out=d4.ap, in0=om[1].ap, scalar=self.neglam.ap[:, jj:jj + 1], in1=om[0].ap, op0=ALU.mult, op1=ALU.add),
                     reads=[om[0], om[1], self.neglam], writes=[d4])
                k.op(k.dve, lambda: nc.vector.tensor_tensor(out=sq4.ap, in0=d4.ap, in1=d4.ap, op=ALU.mult), reads=[d4], writes=[sq4])
                k.op(k.dve, lambda: nc.vector.tensor_reduce(out=ss4.ap, in_=sq4.ap, axis=AX.X, op=ALU.add), reads=[sq4], writes=[ss4])
                k.op(k.act, lambda: nc.scalar.activation(out=ln4.ap, in_=ss4.ap, func=AF.Ln, scale=1.0 / 128.0, bias=self.epsb.ap), reads=[ss4, self.epsb], writes=[ln4])
                k.op(k.act, lambda: nc.scalar.activation(out=rs4.ap, in_=ln4.ap, func=AF.Exp, scale=-0.5), reads=[ln4], writes=[rs4])
                dnb = dn.next()
                sub2 = self.subln.ap[:, jj * 128:(jj + 1) * 128]
                for qt in range(4):
                    k.op(k.dve, lambda: nc.vector.scalar_tensor_tensor(out=dnb.ap[:, qt, :], in0=d4.ap[:, qt, :], scalar=rs4.ap[:, qt:qt + 1], in1=sub2, op0=ALU.mult, op1=ALU.mult),
                         reads=[d4, rs4, self.subln], writes=[dnb])
                self.transpose_out(dnb, lambda qt: dnb.ap[:, qt, :], misc.next(), oT_h, oT_h.ap[:, c * TC:(c + 1) * TC], k.act)
            k.dma(k.sp, oth_ds[h % 2], self.oT.ap[h * 128:(h + 1) * 128, :], oT_h.ap, reads=[oT_h], writes=[self.oT])

    def mla_phase(self, st, l):
        nc, k = self.nc, self.k
        wd, wuq, wukv = self.wb["wdown%d" % l], self.wb["wuq%d" % l], self.wb["wukv%d" % l]
        cs = self.sb(st, "cs", [128, 2, S], F32)
        k.dma(k.sp, k.dsem(), cs.ap, self.cs_d.ap, reads=[self.cs_d], writes=[cs])
        wdsb = self.sb(st, "wdsb", [128, 8, 704], BF16)
        k.dma(k.sp, k.dsem(), wdsb.ap, wd.ap.rearrange("(c p) f -> p c f", p=128), reads=[wd], writes=[wdsb])
        cqn = self.sb(st, "cqn", [128, 3, S], BF16)
        ckvn = self.sb(st, "ckvn", [128, 2, S], BF16)
        krT = self.sb(st, "krT", [128, S], BF16)
        k.op(k.dve, lambda: nc.vector.memset(krT.ap[64:128, :], 0.0), writes=[krT])
        ucs = Ring([self.sb(st, "uc%d" % i, [128, 8, TC], BF16) for i in range(2)])
        u_ds = [k.dsem(), k.dsem()]
        sqr = Ring([self.sb(st, "sq%d" % i, [128, TC], F32) for i in range(2)])
        lnb = self.sb(st, "lnb", [128, TC], F32)
        rstd = self.sb(st, "rstd", [128, TC], F32)
        xbr = Ring([self.sb(st, "xb%d" % i, [128, TC], BF16) for i in range(2)])
        rar = Ring([self.sb(st, "ra%d" % i, [128, TC], F32) for i in range(2)])
        rbr = Ring([self.sb(st, "rb%d" % i, [128, TC], F32) for i in range(2)])
        uT_v = self.uT.ap.rearrange("(c p) t -> p c t", p=128)
        ps = self.ps
        for tc in range(NCH):
            cols = slice(tc * TC, (tc + 1) * TC)
            uc = ucs.next()
            k.dma(k.sp, u_ds[tc % 2], uc.ap, uT_v[:, :, cols], reads=[self.uT], writes=[uc])
            for i in range(3):
                for dc in range(8):
                    k.op(k.pe, lambda: nc.tensor.matmul(ps[i].ap, lhsT=wdsb.ap[:, dc, i * 128:(i + 1) * 128], rhs=uc.ap[:, dc, :], start=(dc == 0), stop=(dc == 7)),
                         reads=[wdsb, uc], writes=[ps[i]], signal=(dc == 7))
            self.rmsnorm_fm(None, 3, TC, lambda i: self.small.ap[:, 72 + i:73 + i], cqn, lambda i: cqn.ap[:, i, cols], sqr, ps[7], lnb, rstd, 384.0,
                            src_ap_fn=lambda i: ps[i].ap, src_bufs=[ps[0], ps[1], ps[2]])
            for i in range(2):
                for dc in range(8):
                    k.op(k.pe, lambda: nc.tensor.matmul(ps[3 + i].ap, lhsT=wdsb.ap[:, dc, 384 + i * 128:384 + (i + 1) * 128], rhs=uc.ap[:, dc, :], start=(dc == 0), stop=(dc == 7)),
                         reads=[wdsb, uc], writes=[ps[3 + i]], signal=(dc == 7))
            self.rmsnorm_fm(None, 2, TC, lambda i: self.small.ap[:, 75 + i:76 + i], ckvn, lambda i: ckvn.ap[:, i, cols], sqr, ps[7], lnb, rstd, 256.0,
                            src_ap_fn=lambda i: ps[3 + i].ap, src_bufs=[ps[3], ps[4]])
            for dc in range(8):
                k.op(k.pe, lambda: nc.tensor.matmul(ps[5].ap[0:64, :], lhsT=wdsb.ap[:, dc, 640:704], rhs=uc.ap[:, dc, :], start=(dc == 0), stop=(dc == 7)),
                     reads=[wdsb, uc], writes=[ps[5]], signal=(dc == 7))
            self.rope_fm(ps[5], 64, cs, cols, krT, krT.ap[0:64, cols], xbr, rar, rbr, ps[6])
        wqh = Ring([self.sb(st, "wqh%d" % i, [128, 3, 192], BF16) for i in range(2)])
        wkvh = Ring([self.sb(st, "wkvh%d" % i, [128, 2, 256], BF16) for i in range(2)])
        wq_ds = [k.dsem(), k.dsem()]
        wuq_v = wuq.ap.rearrange("(c p) f -> p c f", p=128)
        wukv_v = wukv.ap.rearrange("(c p) f -> p c f", p=128)
        qnT = self.sb(st, "qnT", [128, S], BF16)
        qrT = self.sb(st, "qrT", [128, S], BF16)
        k.op(k.dve, lambda: nc.vector.memset(qrT.ap[64:128, :], 0.0), writes=[qrT])
        knT = self.sb(st, "knT", [128, S], BF16)
        V = self.sb(st, "V", [128, 32, 130], BF16)
        k.op(k.dve, lambda: nc.vector.memset(V.ap[:, :, 128:130], 1.0), writes=[V])
        pts = Ring([self.sb(st, "pt%d" % i, [128, TC], BF16) for i in range(4)])
        recr = Ring([self.sb(st, "rec%d" % i, [128, 1], F32) for i in range(8)])
        on = Ring([self.sb(st, "on%d" % i, [128, 4, 128], F32) for i in range(2)])
        oTh = Ring([self.sb(st, "oTh%d" % i, [128, S], BF16) for i in range(2)])
        oth_ds = [k.dsem(), k.dsem()]
        st_ring = Ring(ps[0:2])
        acc = ps[2:6]
        misc = Ring(ps[6:8])
        scale = 192 ** -0.5
        for h in range(8):
            wq, wkv = wqh.next(), wkvh.next()
            g = Tok()
            k.dma(k.sp, wq_ds[h % 2], wq.ap, wuq_v[:, :, h * 192:(h + 1) * 192], reads=[wuq], writes=[wq], group=g)
            k.dma(k.sp, wq_ds[h % 2], wkv.ap, wukv_v[:, :, h * 256:(h + 1) * 256], reads=[wukv], writes=[wkv], group=g)
            for tc in range(NCH):
                cols = slice(tc * TC, (tc + 1) * TC)
                pa = misc.next()
                for i in range(3):
                    k.op(k.pe, lambda: nc.tensor.matmul(pa.ap, lhsT=wq.ap[:, i, 0:128], rhs=cqn.ap[:, i, cols], start=(i == 0), stop=(i == 2)), reads=[wq, cqn], writes=[pa], signal=(i == 2))
                k.op(k.act, lambda: nc.scalar.copy(out=qnT.ap[:, cols], in_=pa.ap), reads=[pa], writes=[qnT])
                pa = misc.next()
                for i in range(3):
                    k.op(k.pe, lambda: nc.tensor.matmul(pa.ap[0:64, :], lhsT=wq.ap[:, i, 128:192], rhs=cqn.ap[:, i, cols], start=(i == 0), stop=(i == 2)), reads=[wq, cqn], writes=[pa], signal=(i == 2))
                pb = misc.next()
                self.rope_fm(pa, 64, cs, cols, qrT, qrT.ap[0:64, cols], xbr, rar, rbr, pb)
                pa = misc.next()
                for i in range(2):
                    k.op(k.pe, lambda: nc.tensor.matmul(pa.ap, lhsT=wkv.ap[:, i, 0:128], rhs=ckvn.ap[:, i, cols], start=(i == 0), stop=(i == 1)), reads=[wkv, ckvn], writes=[pa], signal=(i == 1))
                k.op(k.dve, lambda: nc.vector.tensor_copy(out=knT.ap[:, cols], in_=pa.ap), reads=[pa], writes=[knT])
                pa = misc.next()
                for t4 in range(4):
                    for i in range(2):
                        k.op(k.pe, lambda: nc.tensor.matmul(pa.ap[:, t4 * 128:(t4 + 1) * 128], lhsT=ckvn.ap[:, i, tc * TC + t4 * 128:tc * TC + (t4 + 1) * 128], rhs=wkv.ap[:, i, 128:256], start=(i == 0), stop=(i == 1)),
                             reads=[wkv, ckvn], writes=[pa], signal=(i == 1 and t4 == 3))
                k.op(k.act, lambda: nc.scalar.copy(out=V.ap[:, tc * 4:tc * 4 + 4, 0:128], in_=pa.ap.rearrange("p (a b) -> p a b", a=4)), reads=[pa], writes=[V])
            oT_h = oTh.next()
            for c in range(NCH):
                self.attn_core(c,
                               lambda j: [knT.ap[:, j * 128:(j + 1) * 128], krT.ap[:, j * 128:(j + 1) * 128]],
                               lambda off, n: [qnT.ap[:, c * TC + off:c * TC + off + n], qrT.ap[:, c * TC + off:c * TC + off + n]],
                               [knT, krT, qnT, qrT], lambda j: V.ap[:, j, 0:129], V, 128, scale, st_ring, pts, acc)
                onb = on.next()
                self.evac_norm(acc, 128, onb, lambda qt: onb.ap[:, qt, :], recr)
                self.transpose_out(onb, lambda qt: onb.ap[:, qt, :], misc.next(), oT_h, oT_h.ap[:, c * TC:(c + 1) * TC], k.act)
            k.dma(k.sp, oth_ds[h % 2], self.oT.ap[h * 128:(h + 1) * 128, :], oT_h.ap, reads=[oT_h], writes=[self.oT])

    def dsa_phase(self, st, l):
        nc, k = self.nc, self.k
        win = self.wb["win%d" % l]
        wv_ = win.ap.rearrange("(c p) f -> p c f", p=128)
        Wq = self.sb(st, "Wq", [128, 8, 1024], BF16)
        Wkk = self.sb(st, "Wkk", [128, 8, 128], BF16)
        Wikk = self.sb(st, "Wikk", [128, 8, 128], BF16)
        Wv = self.sb(st, "Wv", [128, 8, 64], BF16)
        Wiq = self.sb(st, "Wiq", [128, 8, 512], BF16)
        Wiw = self.sb(st, "Wiw", [128, 8, 8], BF16)
        k.dma(k.sp, k.dsem(), Wq.ap, wv_[:, :, 0:1024], reads=[win], writes=[Wq])
        g = Tok(); dsx = k.dsem()
        k.dma(k.sp, dsx, Wkk.ap[:, :, 0:64], wv_[:, :, 1024:1088], reads=[win], writes=[Wkk], group=g)
        k.dma(k.sp, dsx, Wkk.ap[:, :, 64:128], wv_[:, :, 1024:1088], reads=[win], writes=[Wkk], group=g)
        k.dma(k.sp, dsx, Wikk.ap[:, :, 0:64], wv_[:, :, 1664:1728], reads=[win], writes=[Wikk], group=g)
        k.dma(k.sp, dsx, Wikk.ap[:, :, 64:128], wv_[:, :, 1664:1728], reads=[win], writes=[Wikk], group=g)
        k.dma(k.sp, dsx, Wv.ap, wv_[:, :, 1088:1152], reads=[win], writes=[Wv], group=g)
        k.dma(k.sp, dsx, Wiq.ap, wv_[:, :, 1152:1664], reads=[win], writes=[Wiq], group=g)
        k.dma(k.sp, dsx, Wiw.ap, wv_[:, :, 1728:1736], reads=[win], writes=[Wiw], group=g)
        KK = self.sb(st, "KK", [128, S], BF16)
        IKK = self.sb(st, "IKK", [128, S], BF16)
        Vaug = self.sb(st, "Vaug", [128, 32, 66], BF16)
        k.op(k.dve, lambda: nc.vector.memset(Vaug.ap[:, :, 64:66], 1.0), writes=[Vaug])
        ucs = Ring([self.sb(st, "uc%d" % i, [128, 8, TC], BF16) for i in range(1)])
        u_ds = [k.dsem()]
        csc = Ring([self.sb(st, "csc%d" % i, [128, 2, TC], F32) for i in range(2)])
        cs_ds = [k.dsem(), k.dsem()]
        xbr = Ring([self.sb(st, "xb%d" % i, [128, TC], BF16) for i in range(2)])
        rar = Ring([self.sb(st, "ra%d" % i, [128, TC], F32) for i in range(2)])
        rbr = Ring([self.sb(st, "rb%d" % i, [128, TC], F32) for i in range(2)])
        uT_v = self.uT.ap.rearrange("(c p) t -> p c t", p=128)
        ps = self.ps
        st_ring = Ring(ps[0:2])
        acc = ps[2:6]
        misc = Ring(ps[6:8])

        class CsView:
            pass

        def load_chunk(tc):
            cols = slice(tc * TC, (tc + 1) * TC)
            uc = ucs.next()
            k.dma(k.sp, u_ds[0], uc.ap, uT_v[:, :, cols], reads=[self.uT], writes=[uc])
            cc = csc.next()
            k.dma(k.sp, cs_ds[(csc.i - 1) % 2], cc.ap, self.cs_d.ap[:, :, cols], reads=[self.cs_d], writes=[cc])
            return uc, cc

        lcols = slice(0, TC)
        for tc in range(NCH):
            cols = slice(tc * TC, (tc + 1) * TC)
            uc, cc = load_chunk(tc)
            for W_, dstb in ((Wkk, KK), (Wikk, IKK)):
                pa = misc.next()
                for dc in range(8):
                    k.op(k.pe, lambda: nc.tensor.matmul(pa.ap, lhsT=W_.ap[:, dc, :], rhs=uc.ap[:, dc, :], start=(dc == 0), stop=(dc == 7)), reads=[W_, uc], writes=[pa], signal=(dc == 7))
                pb = misc.next()
                self.rope_fm(pa, 128, cc, lcols, dstb, dstb.ap[:, cols], xbr, rar, rbr, pb)
            pa = misc.next()
            for t4 in range(4):
                for dc in range(8):
                    k.op(k.pe, lambda: nc.tensor.matmul(pa.ap[:, t4 * 64:(t4 + 1) * 64], lhsT=uc.ap[:, dc, t4 * 128:(t4 + 1) * 128], rhs=Wv.ap[:, dc, :], start=(dc == 0), stop=(dc == 7)),
                         reads=[Wv, uc], writes=[pa], signal=(dc == 7 and t4 == 3))
            k.op(k.act, lambda: nc.scalar.copy(out=Vaug.ap[:, tc * 4:tc * 4 + 4, 0:64], in_=pa.ap[:, 0:256].rearrange("p (a b) -> p a b", a=4)), reads=[pa], writes=[Vaug])
        qTc = self.sb(st, "qTc", [128, 8, TC], BF16)
        iqTc = self.sb(st, "iqTc", [128, 4, TC], BF16)
        iwc = self.sb(st, "iwc", [128, 4, 8], F32)
        accr = Ring([self.sb(st, "sacc%d" % i, [128, S], F32) for i in range(2)])
        mb = self.sb(st, "mb", [128, S], F32)
        maskT = self.sb(st, "maskT", [128, 32, TC], BF16)
        rlr = Ring([self.sb(st, "rl%d" % i, [128, TC], F32) for i in range(3)])
        lo = self.sb(st, "lo", [128, 1], F32)
        mid = self.sb(st, "mid", [128, 1], F32)
        cnt = self.sb(st, "cnt", [128, 1], F32)
        tt = self.sb(st, "tt", [128, 1], F32)
        pts = Ring([self.sb(st, "pt%d" % i, [128, TC], BF16) for i in range(4)])
        recr = Ring([self.sb(st, "rec%d" % i, [128, 1], F32) for i in range(8)])
        onp = Ring([self.sb(st, "onp%d" % i, [128, 4, 128], F32) for i in range(2)])
        oTc = self.sb(st, "oTc", [128, 8, TC], BF16)
        oc_ds = k.dsem()
        oT_v = self.oT.ap.rearrange("(c p) t -> p c t", p=128)
        scale = 64 ** -0.5
        iw_scale = (64 ** -0.5) * (8 ** -0.5)
        NIT = 18
        for c in range(NCH):
            cols = slice(c * TC, (c + 1) * TC)
            uc, cc = load_chunk(c)
            for pair in range(8):
                pa = misc.next()
                for dc in range(8):
                    k.op(k.pe, lambda: nc.tensor.matmul(pa.ap, lhsT=Wq.ap[:, dc, pair * 128:(pair + 1) * 128], rhs=uc.ap[:, dc, :], start=(dc == 0), stop=(dc == 7)), reads=[Wq, uc], writes=[pa], signal=(dc == 7))
                pb = misc.next()
                self.rope_fm(pa, 128, cc, lcols, qTc, qTc.ap[:, pair, :], xbr, rar, rbr, pb)
            for pair in range(4):
                pa = misc.next()
                for dc in range(8):
                    k.op(k.pe, lambda: nc.tensor.matmul(pa.ap, lhsT=Wiq.ap[:, dc, pair * 128:(pair + 1) * 128], rhs=uc.ap[:, dc, :], start=(dc == 0), stop=(dc == 7)), reads=[Wiq, uc], writes=[pa], signal=(dc == 7))
                pb = misc.next()
                self.rope_fm(pa, 128, cc, lcols, iqTc, iqTc.ap[:, pair, :], xbr, rar, rbr, pb)
            pa = misc.next()
            for qt in range(4):
                for dc in range(8):
                    k.op(k.pe, lambda: nc.tensor.matmul(pa.ap[:, qt * 8:(qt + 1) * 8], lhsT=uc.ap[:, dc, qt * 128:(qt + 1) * 128], rhs=Wiw.ap[:, dc, :], start=(dc == 0), stop=(dc == 7)),
                         reads=[Wiw, uc], writes=[pa], signal=(dc == 7 and qt == 3))
            k.op(k.dve, lambda: nc.vector.tensor_scalar(out=iwc.ap, in0=pa.ap[:, 0:32].rearrange("p (a b) -> p a b", a=4), scalar1=iw_scale, scalar2=None, op0=ALU.mult), reads=[pa], writes=[iwc])
            for qt in range(4):
                L = c * TC + 128 * (qt + 1)
                sa = accr.next()
                for kc in range(c + 1):
                    n = min(TC, L - kc * TC)
                    for h in range(8):
                        b0 = 64 * (h % 2)
                        stb = st_ring.next()
                        k.op(k.pe, lambda: nc.tensor.matmul(stb.ap[:, 0:n], lhsT=iqTc.ap[b0:b0 + 64, h // 2, qt * 128:(qt + 1) * 128], rhs=IKK.ap[b0:b0 + 64, kc * TC:kc * TC + n], start=True, stop=True),
                             reads=[iqTc, IKK], writes=[stb])
                        rl = rlr.next()
                        k.op(k.act, lambda: nc.scalar.activation(out=rl.ap[:, 0:n], in_=stb.ap[:, 0:n], func=AF.Relu), reads=[stb], writes=[rl])
                        if h == 0:
                            k.op(k.dve, lambda: nc.vector.tensor_scalar(out=sa.ap[:, kc * TC:kc * TC + n], in0=rl.ap[:, 0:n], scalar1=iwc.ap[:, qt, 0:1], scalar2=None, op0=ALU.mult), reads=[rl, iwc], writes=[sa])
                        else:
                            k.op(k.dve, lambda: nc.vector.scalar_tensor_tensor(out=sa.ap[:, kc * TC:kc * TC + n], in0=rl.ap[:, 0:n], scalar=iwc.ap[:, qt, h:h + 1], in1=sa.ap[:, kc * TC:kc * TC + n], op0=ALU.mult, op1=ALU.add),
                                 reads=[rl, iwc, sa], writes=[sa])
                k.op(k.pool, lambda: nc.gpsimd.affine_select(out=sa.ap[:, L - 128:L], in_=sa.ap[:, L - 128:L], pattern=[[-1, 128]], compare_op=ALU.is_ge, fill=NEG, base=0, channel_multiplier=1),
                     reads=[sa], writes=[sa])
                k.op(k.dve, lambda: nc.vector.memset(lo.ap, -32.0), writes=[lo])
                k.op(k.dve, lambda: nc.vector.memset(mid.ap, 0.0), writes=[mid])
                w = 64.0
                for it in range(NIT):
                    k.op(k.dve, lambda: nc.vector.tensor_scalar(out=mb.ap[:, 0:L], in0=sa.ap[:, 0:L], scalar1=mid.ap, scalar2=None, op0=ALU.is_ge, op1=ALU.add, accum_out=cnt.ap), reads=[sa, mid], writes=[mb, cnt])
                    k.op(k.dve, lambda: nc.vector.tensor_scalar(out=tt.ap, in0=cnt.ap, scalar1=256.0, scalar2=w / 2, op0=ALU.is_ge, op1=ALU.mult), reads=[cnt], writes=[tt])
                    k.op(k.dve, lambda: nc.vector.tensor_tensor(out=lo.ap, in0=lo.ap, in1=tt.ap, op=ALU.add), reads=[lo, tt], writes=[lo])
                    k.op(k.dve, lambda: nc.vector.tensor_scalar(out=mid.ap, in0=lo.ap, scalar1=w / 4, scalar2=None, op0=ALU.add), reads=[lo], writes=[mid])
                    w = w / 2
                k.op(k.dve, lambda: nc.vector.tensor_scalar(out=mb.ap[:, 0:L], in0=sa.ap[:, 0:L], scalar1=lo.ap, scalar2=None, op0=ALU.is_ge), reads=[sa, lo], writes=[mb])
                nkt = L // 128
                for k0 in range(0, nkt, 4):
                    nb = min(4, nkt - k0)
                    pbk = misc.next()
                    for i in range(nb):
                        k.op(k.pe, lambda: nc.tensor.transpose(pbk.ap[:, i * 128:(i + 1) * 128], mb.ap[:, (k0 + i) * 128:(k0 + i + 1) * 128], self.ident_f.ap), reads=[mb, self.ident_f], writes=[pbk], signal=(i == nb - 1))
                    k.op(k.act, lambda: nc.scalar.copy(out=maskT.ap[:, k0:k0 + nb, qt * 128:(qt + 1) * 128], in_=pbk.ap[:, 0:nb * 128].rearrange("p (a b) -> p a b", a=nb)), reads=[pbk], writes=[maskT])
            for h in range(16):
                b0 = 64 * (h % 2)
                self.attn_core(c,
                               lambda j: [KK.ap[b0:b0 + 64, j * 128:(j + 1) * 128]],
                               lambda off, n: [qTc.ap[b0:b0 + 64, h // 2, off:off + n]],
                               [KK, qTc], lambda j: Vaug.ap[:, j, 0:65], Vaug, 64, scale, st_ring, pts, acc, tri=False,
                               mask_fn=lambda j, off, n: maskT.ap[:, j, off:off + n], maskbuf=maskT, mask_eng=lambda j: (k.pool if j % 3 != 2 else k.dve))
                if h % 2 == 0:
                    onb = onp.next()
                self.evac_norm(acc, 64, onb, lambda qt: onb.ap[:, qt, b0:b0 + 64], recr)
                if h % 2 == 1:
                    self.transpose_out(onb, lambda qt: onb.ap[:, qt, :], misc.next(), oTc, oTc.ap[:, h // 2, :], k.act)
            k.dma(k.sp, oc_ds, oT_v[:, :, cols], oTc.ap, reads=[oTc], writes=[self.oT])


_PROG_CACHE = {}


def _get_prog(n_layers=DEPTH):
    if n_layers not in _PROG_CACHE:
        _PROG_CACHE[n_layers] = Prog(n_layers)
    return _PROG_CACHE[n_layers]


def _host_inputs(inputs):
    f = lambda a: np.ascontiguousarray(np.asarray(a, dtype=np.float32))
    an = f(inputs["attn_norm"]).reshape(4, 8, 128).transpose(2, 0, 1).reshape(128, 32)
    fn = f(inputs["ffn_norm"]).reshape(4, 8, 128).transpose(2, 0, 1).reshape(128, 32)
    fin = f(inputs["final_norm"]).reshape(8, 128).T
    qn = f(inputs["mla_q_norm"]).reshape(3, 128).T
    kn = f(inputs["mla_kv_norm"]).reshape(2, 128).T
    small = f(np.concatenate([an, fn, fin, qn, kn], axis=1))
    lam = f(np.broadcast_to(f(inputs["diff_lambda"]).reshape(1, 512), (128, 512)))
    subln = f(np.broadcast_to(f(inputs["diff_subln"]).reshape(1, 256), (128, 256)))
    shared = {"small": small, "lam": lam, "subln": subln}
    for l in range(DEPTH):
        m, j = l % 3, l // 3
        if m == 0:
            shared["wqkv%d" % l] = f(inputs["diff_wqkv"][j])
            shared["wo%d" % l] = f(inputs["diff_wo"][j])
        elif m == 1:
            shared["win%d" % l] = f(inputs["dsa_win"][j])
            shared["wo%d" % l] = f(inputs["dsa_wo"][j])
        else:
            shared["wdown%d" % l] = f(inputs["mla_wdown"][j])
            shared["wuq%d" % l] = f(inputs["mla_wuq"][j])
            shared["wukv%d" % l] = f(inputs["mla_wukv"][j])
            shared["wo%d" % l] = f(inputs["mla_wo"][j])
        shared["w1_%d" % l] = f(inputs["ffn_w1"][l])
        shared["w3_%d" % l] = f(inputs["ffn_w3"][l])
        shared["w2_%d" % l] = f(inputs["ffn_w2"][l])
    return shared


def kernel(**inputs):
    x = np.asarray(inputs["x"], dtype=np.float32)
    B = x.shape[0]
    shared = _host_inputs(inputs)
    prog = _get_prog(DEPTH)
    in_maps = []
    for b in range(B):
        m = dict(shared)
        m["x"] = np.ascontiguousarray(x[b])
        in_maps.append(m)
    res = run_bass_kernel_spmd(prog.nc, in_maps, core_ids=list(range(B)))
    return np.stack([np.asarray(r["out"], dtype=np.float32) for r in res.results], axis=0)
```

```python
import math
from contextlib import ExitStack
import numpy as np
import concourse.bass as bass
import concourse.mybir as mybir
from concourse.bass_utils import run_bass_kernel_spmd

F32 = mybir.dt.float32
BF16 = mybir.dt.bfloat16
I32 = mybir.dt.int32
ALU = mybir.AluOpType
AF = mybir.ActivationFunctionType
AX = mybir.AxisListType

S = 4096
D = 1024
DFF = 2816
NCH = 8
TC = 512
EPS = 1e-6
DEPTH = 4
NEG = -1.0e30


class Sem:
    def __init__(self, nc, name):
        self.h = nc.alloc_semaphore(name)
        self.count = 0


class Tok:
    __slots__ = ("sem", "val")

    def __init__(self, sem=None, val=0):
        self.sem = sem
        self.val = val


class Buf:
    __slots__ = ("ap", "w", "r", "excl")

    def __init__(self, ap, excl=False):
        self.ap = ap
        self.w = None
        self.r = {}
        self.excl = excl


class Eng:
    def __init__(self, k, eng, name, is_pe=False):
        self.eng = eng
        self.sem = Sem(k.nc, "e_" + name)
        self.seen = {}
        self.is_pe = is_pe
        self.name = name

    def wait(self, sem, val):
        if val <= self.seen.get(sem, 0):
            return
        self.eng.wait_ge(sem.h, val)
        self.seen[sem] = val


class K:
    def __init__(self, nc):
        self.nc = nc
        self.pe = Eng(self, nc.tensor, "pe", True)
        self.act = Eng(self, nc.scalar, "act")
        self.dve = Eng(self, nc.vector, "dve")
        self.pool = Eng(self, nc.gpsimd, "pool")
        self.sp = Eng(self, nc.sync, "sp")
        self.engs = [self.pe, self.act, self.dve, self.pool, self.sp]
        self.dsems = []
        self.fixed = []
        self.nsem = 0

    def dsem(self, name=None):
        if self.nsem < len(self.dsems):
            s = self.dsems[self.nsem]
        else:
            s = Sem(self.nc, "d%d" % self.nsem)
            self.dsems.append(s)
        self.nsem += 1
        return s

    def _w(self, e, tok):
        if e.is_pe and tok.sem is e.sem:
            return
        e.wait(tok.sem, tok.val)

    def _deps(self, e, reads, writes):
        for b in reads:
            if b.w is not None:
                self._w(e, b.w)
            if b.excl:
                for t in b.r.values():
                    if t.sem is not e.sem:
                        self._w(e, t)
        for b in writes:
            if b.w is not None:
                self._w(e, b.w)
            for t in b.r.values():
                self._w(e, t)

    def _upd(self, tok, reads, writes):
        for b in writes:
            b.w = tok
            b.r = {}
        for b in reads:
            if b not in writes:
                o = b.r.get(tok.sem)
                if o is None or o.val < tok.val or o is tok:
                    b.r[tok.sem] = tok

    def op(self, e, fn, reads=(), writes=(), signal=True):
        self._deps(e, reads, writes)
        inst = fn()
        if signal:
            e.sem.count += 1
            inst.then_inc(e.sem.h, 1)
            tok = Tok(e.sem, e.sem.count)
        else:
            tok = Tok(e.sem, e.sem.count + 1)
        self._upd(tok, reads, writes)
        return inst

    def dma(self, q, dsem, out_ap, in_ap, reads=(), writes=(), group=None, **kw):
        self._deps(q, reads, writes)
        if group is None or group.val == 0:
            q.wait(dsem, dsem.count)
        inst = q.eng.dma_start(out=out_ap, in_=in_ap, **kw)
        dsem.count += 16
        inst.then_inc(dsem.h, 16)
        if group is not None:
            group.sem = dsem
            group.val = dsem.count
            tok = group
        else:
            tok = Tok(dsem, dsem.count)
        self._upd(tok, reads, writes)
        return inst

    def barrier(self):
        self.nsem = 0
        for e in self.engs:
            for o in self.engs[:4]:
                if o is not e and o.sem.count > 0:
                    e.wait(o.sem, o.sem.count)
            for d in self.dsems + self.fixed:
                if d.count > 0:
                    e.wait(d, d.count)


class Ring:
    def __init__(self, bufs):
        self.bufs = bufs
        self.i = 0

    def next(self):
        b = self.bufs[self.i % len(self.bufs)]
        self.i += 1
        return b


class Prog:
    def __init__(self, n_layers=DEPTH):
        self.n_layers = n_layers
        nc = bass.Bass("TRN2", target_bir_lowering=False)
        self.nc = nc
        self.k = K(nc)
        self.build()

    def sb(self, st, name, shape, dt):
        self.uid = getattr(self, "uid", 0) + 1
        t = st.enter_context(self.nc.sbuf_tensor("s%d_%s" % (self.uid, name), list(shape), dt))
        return Buf(t[:])

    def din(self, name, shape, dt=F32):
        return self.nc.dram_tensor(name, list(shape), dt, kind="ExternalInput").ap()

    def dscr(self, name, shape, dt):
        import os as _os
        if _os.environ.get("KDEBUG_OUT") and name in ("uT", "cs_d", "oT", "hT"):
            return Buf(self.nc.dram_tensor(name, list(shape), dt, kind="ExternalOutput").ap())
        return Buf(self.nc.dram_tensor(name, list(shape), dt).ap())

    def build(self):
        nc, k = self.nc, self.k
        self.x = self.din("x", [S, D])
        self.out = nc.dram_tensor("out", [S, D], F32, kind="ExternalOutput").ap()
        self.p_small = self.din("small", [128, 32 + 32 + 8 + 3 + 2])
        self.p_lam = self.din("lam", [128, 512])
        self.p_subln = self.din("subln", [128, 256])
        self.wspec = {}
        wl = []
        for l in range(DEPTH):
            m = l % 3
            if m == 0:
                wl.append([("wqkv%d" % l, D, 3072), ("wo%d" % l, D, D)])
            elif m == 1:
                wl.append([("win%d" % l, D, 1736), ("wo%d" % l, D, D)])
            else:
                wl.append([("wdown%d" % l, D, 704), ("wuq%d" % l, 384, 1536), ("wukv%d" % l, 256, 2048), ("wo%d" % l, D, D)])
            wl[-1] += [("w1_%d" % l, D, DFF), ("w3_%d" % l, D, DFF), ("w2_%d" % l, DFF, D)]
        self.wf = {}
        self.wb = {}
        for l in range(DEPTH):
            for (n, kk, nn) in wl[l]:
                self.wf[n] = self.din(n, [kk, nn])
                self.wb[n] = self.dscr(n + "_b", [kk, nn], BF16)
        self.wl = wl
        self.hT = [self.dscr("hT", [D, S], F32)]
        self.uT = self.dscr("uT", [D, S], BF16)
        self.oT = self.dscr("oT", [D, S], BF16)
        self.cs_d = self.dscr("cs_d", [128, 2, S], F32)

        with ExitStack() as st0:
            self.setup_persistent(st0)
            self.cast_weights()
            with ExitStack() as st:
                self.rope_tables(st)
            k.barrier()
            with ExitStack() as st:
                self.post_phase(st, -1)
            for l in range(self.n_layers):
                k.barrier()
                m = l % 3
                import os as _os
                with ExitStack() as st:
                    if _os.environ.get("KSKIP_ATTN"):
                        z = self.sb(st, "zz", [128, S], BF16)
                        k.op(k.dve, lambda: nc.vector.memset(z.ap, 0.0), writes=[z])
                        for i in range(8):
                            k.dma(k.sp, k.dsem(), self.oT.ap[i * 128:(i + 1) * 128, :], z.ap, reads=[z], writes=[self.oT])
                    elif m == 0:
                        self.diff_phase(st, l)
                    elif m == 1:
                        self.dsa_phase(st, l)
                    else:
                        self.mla_phase(st, l)
                k.barrier()
                with ExitStack() as st:
                    if not _os.environ.get("KSKIP_POST"):
                        self.post_phase(st, l)
            k.barrier()

    def setup_persistent(self, st):
        nc, k = self.nc, self.k
        self.ps = [Buf(nc.alloc_psum_tensor("ps%d" % i, [128, 512], F32)[:], excl=True) for i in range(8)]
        self.ident_f = self.sb(st, "ident_f", [128, 128], F32)
        self.ident_b = self.sb(st, "ident_b", [128, 128], BF16)
        self.ones_f = self.sb(st, "ones_f", [128, 128], F32)
        self.tri = self.sb(st, "tri", [128, 128], BF16)
        self.rperm = self.sb(st, "rperm", [128, 128], BF16)
        self.small = self.sb(st, "small", [128, 77], F32)
        self.lam = self.sb(st, "lamsb", [128, 512], F32)
        self.subln = self.sb(st, "sublnsb", [128, 256], F32)
        self.neglam = self.sb(st, "neglam", [128, 2], F32)
        self.epsb = self.sb(st, "epsb", [128, 1], F32)
        k.op(k.dve, lambda: nc.vector.memset(self.epsb.ap, EPS), writes=[self.epsb])
        k.dma(k.sp, k.dsem(), self.small.ap, self.p_small, writes=[self.small])
        k.dma(k.sp, k.dsem(), self.lam.ap, self.p_lam, writes=[self.lam])
        k.dma(k.sp, k.dsem(), self.subln.ap, self.p_subln, writes=[self.subln])
        with ExitStack() as t:
            io = self.sb(t, "io", [128, 128], I32)
            dif = self.sb(t, "dif", [128, 128], F32)
            ta = self.sb(t, "ta", [128, 128], F32)
            tb = self.sb(t, "tb", [128, 128], F32)
            co = self.sb(t, "co", [128, 128], F32)
            ioj = self.sb(t, "ioj", [128, 128], I32)
            k.op(k.pool, lambda: nc.gpsimd.iota(io.ap, pattern=[[1, 128]], base=0, channel_multiplier=-1), writes=[io])
            k.op(k.dve, lambda: nc.vector.tensor_copy(out=dif.ap, in_=io.ap), reads=[io], writes=[dif])
            k.op(k.dve, lambda: nc.vector.tensor_single_scalar(out=self.ident_f.ap, in_=dif.ap, scalar=0.0, op=ALU.is_equal), reads=[dif], writes=[self.ident_f])
            k.op(k.dve, lambda: nc.vector.tensor_copy(out=self.ident_b.ap, in_=self.ident_f.ap), reads=[self.ident_f], writes=[self.ident_b])
            k.op(k.dve, lambda: nc.vector.memset(self.ones_f.ap, 1.0), writes=[self.ones_f])
            k.op(k.dve, lambda: nc.vector.tensor_single_scalar(out=self.tri.ap, in_=dif.ap, scalar=0.0, op=ALU.is_ge), reads=[dif], writes=[self.tri])
            k.op(k.pool, lambda: nc.gpsimd.iota(ioj.ap, pattern=[[1, 128]], base=0, channel_multiplier=0), writes=[ioj])
            k.op(k.dve, lambda: nc.vector.tensor_single_scalar(out=ioj.ap, in_=ioj.ap, scalar=32, op=ALU.bitwise_and), reads=[ioj], writes=[ioj])
            k.op(k.dve, lambda: nc.vector.tensor_copy(out=co.ap, in_=ioj.ap), reads=[ioj], writes=[co])
            k.op(k.dve, lambda: nc.vector.tensor_single_scalar(out=co.ap, in_=co.ap, scalar=16.0, op=ALU.is_ge), reads=[co], writes=[co])
            k.op(k.dve, lambda: nc.vector.tensor_single_scalar(out=ta.ap, in_=dif.ap, scalar=32.0, op=ALU.is_equal), reads=[dif], writes=[ta])
            k.op(k.dve, lambda: nc.vector.tensor_single_scalar(out=tb.ap, in_=dif.ap, scalar=-32.0, op=ALU.is_equal), reads=[dif], writes=[tb])
            k.op(k.dve, lambda: nc.vector.tensor_tensor(out=ta.ap, in0=ta.ap, in1=co.ap, op=ALU.mult), reads=[ta, co], writes=[ta])
            k.op(k.dve, lambda: nc.vector.tensor_scalar(out=co.ap, in0=co.ap, scalar1=-1.0, scalar2=1.0, op0=ALU.mult, op1=ALU.add), reads=[co], writes=[co])
            k.op(k.dve, lambda: nc.vector.tensor_tensor(out=tb.ap, in0=tb.ap, in1=co.ap, op=ALU.mult), reads=[tb, co], writes=[tb])
            k.op(k.dve, lambda: nc.vector.tensor_tensor(out=self.rperm.ap, in0=ta.ap, in1=tb.ap, op=ALU.subtract), reads=[ta, tb], writes=[self.rperm])
            pr = self.sb(t, "lampr", [128, 256], F32)
            sm = self.sb(t, "lamsm", [128, 4], F32)
            for j in range(2):
                layer = 3 * j
                li = 0.8 - 0.6 * math.exp(-0.3 * layer)
                lm = self.lam.ap[:, j * 256:(j + 1) * 256]
                k.op(k.dve, lambda: nc.vector.tensor_tensor(out=pr.ap[:, 0:64], in0=lm[:, 0:64], in1=lm[:, 64:128], op=ALU.mult), reads=[self.lam], writes=[pr])
                k.op(k.dve, lambda: nc.vector.tensor_tensor(out=pr.ap[:, 64:128], in0=lm[:, 128:192], in1=lm[:, 192:256], op=ALU.mult), reads=[self.lam], writes=[pr])
                k.op(k.dve, lambda: nc.vector.tensor_reduce(out=sm.ap[:, 0:2], in_=pr.ap[:, 0:128].rearrange("p (a b) -> p a b", a=2), axis=AX.X, op=ALU.add), reads=[pr], writes=[sm])
                k.op(k.act, lambda: nc.scalar.activation(out=sm.ap[:, 2:4], in_=sm.ap[:, 0:2], func=AF.Exp), reads=[sm], writes=[sm])
                k.op(k.dve, lambda: nc.vector.tensor_tensor(out=sm.ap[:, 0:1], in0=sm.ap[:, 3:4], in1=sm.ap[:, 2:3], op=ALU.subtract), reads=[sm], writes=[sm])
                k.op(k.dve, lambda: nc.vector.tensor_scalar(out=self.neglam.ap[:, j:j + 1], in0=sm.ap[:, 0:1], scalar1=-li, scalar2=None, op0=ALU.add), reads=[sm], writes=[self.neglam])
            for j in range(2):
                li = 0.8 - 0.6 * math.exp(-0.3 * 3 * j)
                sl = self.subln.ap[:, j * 128:(j + 1) * 128]
                k.op(k.dve, lambda: nc.vector.tensor_scalar(out=sl, in0=sl, scalar1=1.0 - li, scalar2=None, op0=ALU.mult), reads=[self.subln], writes=[self.subln])
            k.barrier()

    def g_attn(self, l, dc):
        return self.small.ap[:, l * 8 + dc:l * 8 + dc + 1]

    def g_ffn(self, l, dc):
        return self.small.ap[:, 32 + l * 8 + dc:32 + l * 8 + dc + 1]

    def g_final(self, dc):
        return self.small.ap[:, 64 + dc:64 + dc + 1]

    def cast_weights(self):
        k = self.k
        for l in range(DEPTH):
            ds = Sem(self.nc, "cast%d" % l)
            k.fixed.append(ds)
            g = Tok()
            for (n, kk, nn) in self.wl[l]:
                src, dst = self.wf[n], self.wb[n]
                nsplit = 1 if nn <= 2048 else 2
                w = nn // nsplit
                for i in range(nsplit):
                    k.dma(k.pool, ds, dst.ap[:, i * w:(i + 1) * w], src[:, i * w:(i + 1) * w], writes=[dst], group=g)

    def rope_tables(self, st):
        nc, k = self.nc, self.k
        pi = self.sb(st, "r_pi", [128, 1], I32)
        pf = self.sb(st, "r_pf", [128, 1], F32)
        inv = self.sb(st, "r_inv", [128, 1], F32)
        k.op(k.pool, lambda: nc.gpsimd.iota(pi.ap, pattern=[[0, 1]], base=0, channel_multiplier=1), writes=[pi])
        k.op(k.dve, lambda: nc.vector.tensor_single_scalar(out=pi.ap, in_=pi.ap, scalar=31, op=ALU.bitwise_and), reads=[pi], writes=[pi])
        k.op(k.dve, lambda: nc.vector.tensor_copy(out=pf.ap, in_=pi.ap), reads=[pi], writes=[pf])
        k.op(k.act, lambda: nc.scalar.activation(out=inv.ap, in_=pf.ap, func=AF.Exp, scale=-math.log(10000.0) / 32.0), reads=[pf], writes=[inv])
        k.op(k.dve, lambda: nc.vector.tensor_scalar(out=inv.ap, in0=inv.ap, scalar1=1.0 / (2.0 * math.pi), scalar2=None, op0=ALU.mult), reads=[inv], writes=[inv])
        W = 1024
        ti = self.sb(st, "r_ti", [128, W], I32)
        tt = self.sb(st, "r_tt", [128, W], F32)
        ff = self.sb(st, "r_ff", [128, W], F32)
        ni = self.sb(st, "r_ni", [128, W], I32)
        nf = self.sb(st, "r_nf", [128, W], F32)
        g = self.sb(st, "r_g", [128, W], F32)
        res = [self.sb(st, "r_res%d" % i, [128, 2, W], F32) for i in range(2)]
        dss = [k.dsem(), k.dsem()]
        for c in range(S // W):
            rb = res[c % 2]
            k.op(k.pool, lambda: nc.gpsimd.iota(ti.ap, pattern=[[1, W]], base=c * W, channel_multiplier=0), writes=[ti])
            k.op(k.dve, lambda: nc.vector.tensor_copy(out=tt.ap, in_=ti.ap), reads=[ti], writes=[tt])
            k.op(k.dve, lambda: nc.vector.tensor_scalar(out=tt.ap, in0=tt.ap, scalar1=inv.ap, scalar2=None, op0=ALU.mult), reads=[tt, inv], writes=[tt])
            for which in range(2):
                off = 0.25 if which == 0 else 0.0
                k.op(k.dve, lambda: nc.vector.tensor_scalar(out=ff.ap, in0=tt.ap, scalar1=off, scalar2=None, op0=ALU.add), reads=[tt], writes=[ff])
                k.op(k.dve, lambda: nc.vector.tensor_copy(out=ni.ap, in_=ff.ap), reads=[ff], writes=[ni])
                k.op(k.dve, lambda: nc.vector.tensor_copy(out=nf.ap, in_=ni.ap), reads=[ni], writes=[nf])
                k.op(k.dve, lambda: nc.vector.tensor_tensor(out=ff.ap, in0=ff.ap, in1=nf.ap, op=ALU.subtract), reads=[ff, nf], writes=[ff])
                k.op(k.dve, lambda: nc.vector.tensor_single_scalar(out=g.ap, in_=ff.ap, scalar=0.5, op=ALU.is_gt), reads=[ff], writes=[g])
                k.op(k.dve, lambda: nc.vector.tensor_tensor(out=ff.ap, in0=ff.ap, in1=g.ap, op=ALU.subtract), reads=[ff, g], writes=[ff])
                k.op(k.dve, lambda: nc.vector.tensor_single_scalar(out=g.ap, in_=ff.ap, scalar=-0.5, op=ALU.is_lt), reads=[ff], writes=[g])
                k.op(k.dve, lambda: nc.vector.tensor_tensor(out=ff.ap, in0=ff.ap, in1=g.ap, op=ALU.add), reads=[ff, g], writes=[ff])
                k.op(k.act, lambda: nc.scalar.activation(out=rb.ap[:, which, :], in_=ff.ap, func=AF.Sin, scale=2.0 * math.pi * (1.0 - 1e-6)), reads=[ff], writes=[rb])
            k.dma(k.sp, dss[c % 2], self.cs_d.ap[:, :, c * W:(c + 1) * W], rb.ap, reads=[rb], writes=[self.cs_d])

    def rmsnorm_fm(self, src, nchunk, width, gain_fn, dst, dst_ap_fn, sqr, ssps, lnb, rstd, dim, src_ap_fn=None, src_bufs=None):
        nc, k = self.nc, self.k
        sbufs = src_bufs if src_bufs is not None else [src]
        for dc in range(nchunk):
            sq = sqr.next()
            sap = src_ap_fn(dc)
            k.op(k.act, lambda: nc.scalar.activation(out=sq.ap[:, :width], in_=sap, func=AF.Square), reads=sbufs, writes=[sq])
            k.op(k.pe, lambda: nc.tensor.matmul(ssps.ap[:, :width], lhsT=self.ones_f.ap, rhs=sq.ap[:, :width], start=(dc == 0), stop=(dc == nchunk - 1)),
                 reads=[sq, self.ones_f], writes=[ssps])
        k.op(k.act, lambda: nc.scalar.activation(out=lnb.ap[:, :width], in_=ssps.ap[:, :width], func=AF.Ln, scale=1.0 / dim, bias=self.epsb.ap), reads=[ssps, self.epsb], writes=[lnb])
        k.op(k.act, lambda: nc.scalar.activation(out=rstd.ap[:, :width], in_=lnb.ap[:, :width], func=AF.Exp, scale=-0.5), reads=[lnb], writes=[rstd])
        for dc in range(nchunk):
            sap = src_ap_fn(dc)
            dap = dst_ap_fn(dc)
            k.op(k.dve, lambda: nc.vector.scalar_tensor_tensor(out=dap, in0=sap, scalar=gain_fn(dc), in1=rstd.ap[:, :width], op0=ALU.mult, op1=ALU.mult),
                 reads=sbufs + [rstd, self.small], writes=[dst])

    def eps_ap(self):
        return self.epsb.ap

    def post_phase(self, st, l):
        nc, k = self.nc, self.k
        first = (l < 0)
        last = (l == self.n_layers - 1)
        hcs = Ring([self.sb(st, "hc%d" % i, [128, 8, TC], F32) for i in range(2)])
        h_ds = [k.dsem(), k.dsem()]
        hst_ds = [k.dsem(), k.dsem()]
        sqr = Ring([self.sb(st, "sq%d" % i, [128, TC], F32) for i in range(2)])
        lnb = self.sb(st, "lnb", [128, TC], F32)
        rstd = self.sb(st, "rstd", [128, TC], F32)
        if not last:
            uns = Ring([self.sb(st, "un%d" % i, [128, 8, TC], BF16) for i in range(1)])
            un_ds = [k.dsem(), k.dsem()]
        ssps = self.ps[7]
        hT_v = self.hT[0].ap.rearrange("(c p) t -> p c t", p=128)
        uT_v = self.uT.ap.rearrange("(c p) t -> p c t", p=128)
        oT_v = self.oT.ap.rearrange("(c p) t -> p c t", p=128)
        if first:
            xts = Ring([self.sb(st, "xt%d" % i, [128, D], F32) for i in range(8)])
            x_ds = [k.dsem() for _ in range(8)]
            psr = Ring(self.ps[0:6])
        else:
            ocs = Ring([self.sb(st, "oc%d" % i, [128, 8, TC], BF16) for i in range(2)])
            o_ds = [k.dsem(), k.dsem()]
            ucs = Ring([self.sb(st, "uc%d" % i, [128, 8, TC], BF16) for i in range(2)])
            gT = self.sb(st, "gT", [128, 22, TC], BF16)
            sil = Ring([self.sb(st, "sil%d" % i, [128, TC], F32) for i in range(2)])
            wo = self.sb(st, "wo", [128, 8, D], BF16)
            w2 = self.sb(st, "w2", [128, 22, D], BF16)
            w13 = Ring([self.sb(st, "w13_%d" % i, [128, 2, 8, 512], BF16) for i in range(2)])
            w_ds = [k.dsem(), k.dsem()]
            wres_ds = k.dsem()
            psr = Ring(self.ps[0:6])
            wob, w1b, w3b, w2b = self.wb["wo%d" % l], self.wb["w1_%d" % l], self.wb["w3_%d" % l], self.wb["w2_%d" % l]
            k.dma(k.sp, wres_ds, wo.ap, wob.ap.rearrange("(c p) f -> p c f", p=128), reads=[wob], writes=[wo])
            k.dma(k.sp, k.dsem(), w2.ap, w2b.ap.rearrange("(c p) f -> p c f", p=128), reads=[w2b], writes=[w2])
            w1v = w1b.ap.rearrange("(c p) f -> p c f", p=128)
            w3v = w3b.ap.rearrange("(c p) f -> p c f", p=128)
            fgs = [(i * 512, 512) for i in range(5)] + [(2560, 256)]
        if last:
            orow = Ring([self.sb(st, "orow%d" % i, [128, D], F32) for i in range(2)])
            or_ds = [k.dsem(), k.dsem()]

        def load_chunk(c):
            hc = hcs.bufs[c % 2]
            cols = slice(c * TC, (c + 1) * TC)
            if not first:
                k.dma(k.sp, h_ds[c % 2], hc.ap, hT_v[:, :, cols], reads=[self.hT[0]], writes=[hc])
                oc = ocs.bufs[c % 2]
                k.dma(k.sp, o_ds[c % 2], oc.ap, oT_v[:, :, cols], reads=[self.oT], writes=[oc])

        if not first:
            load_chunk(0)
        for c in range(NCH):
            cols = slice(c * TC, (c + 1) * TC)
            hc = hcs.bufs[c % 2]
            if first:
                xtl = []
                for i in range(4):
                    xt = xts.next()
                    k.dma(k.sp, x_ds[(xts.i - 1) % 8], xt.ap, self.x[c * TC + i * 128:c * TC + (i + 1) * 128, :], writes=[xt])
                    xtl.append(xt)
                for dc in range(8):
                    pb = psr.next()
                    for i in range(4):
                        k.op(k.pe, lambda: nc.tensor.transpose(pb.ap[:, i * 128:(i + 1) * 128], xtl[i].ap[:, dc * 128:(dc + 1) * 128], self.ident_f.ap),
                             reads=[xtl[i], self.ident_f], writes=[pb], signal=(i == 3))
                    e = k.act if dc % 2 == 0 else k.dve
                    if e is k.act:
                        k.op(e, lambda: nc.scalar.copy(out=hc.ap[:, dc, :], in_=pb.ap), reads=[pb], writes=[hc])
                    else:
                        k.op(e, lambda: nc.vector.tensor_copy(out=hc.ap[:, dc, :], in_=pb.ap), reads=[pb], writes=[hc])
            else:
                if c + 1 < NCH:
                    load_chunk(c + 1)
                oc = ocs.bufs[c % 2]
                for dc in range(8):
                    pb = psr.next()
                    for cc in range(8):
                        k.op(k.pe, lambda: nc.tensor.matmul(pb.ap, lhsT=wo.ap[:, cc, dc * 128:(dc + 1) * 128], rhs=oc.ap[:, cc, :], start=(cc == 0), stop=(cc == 7)),
                             reads=[wo, oc], writes=[pb], signal=(cc == 7))
                    k.op(k.dve, lambda: nc.vector.tensor_tensor(out=hc.ap[:, dc, :], in0=pb.ap, in1=hc.ap[:, dc, :], op=ALU.add), reads=[pb, hc], writes=[hc])
                uc = ucs.next()
                self.rmsnorm_fm(hc, 8, TC, lambda dc: self.g_ffn(l, dc), uc, lambda dc: uc.ap[:, dc, :], sqr, ssps, lnb, rstd, float(D),
                                src_ap_fn=lambda dc: hc.ap[:, dc, :])
                def load_w13(gi):
                    f0, fw = fgs[gi]
                    wt = w13.bufs[gi % 2]
                    g = Tok()
                    k.dma(k.sp, w_ds[gi % 2], wt.ap[:, 0, :, :fw], w1v[:, :, f0:f0 + fw], reads=[w1b], writes=[wt], group=g)
                    k.dma(k.sp, w_ds[gi % 2], wt.ap[:, 1, :, :fw], w3v[:, :, f0:f0 + fw], reads=[w3b], writes=[wt], group=g)
                load_w13(0)
                for gi, (f0, fw) in enumerate(fgs):
                    if gi + 1 < len(fgs):
                        load_w13(gi + 1)
                    wt = w13.bufs[gi % 2]
                    for fi in range(fw // 128):
                        fc = f0 // 128 + fi
                        p1 = psr.next()
                        p3 = psr.next()
                        for dc in range(8):
                            k.op(k.pe, lambda: nc.tensor.matmul(p1.ap, lhsT=wt.ap[:, 0, dc, fi * 128:(fi + 1) * 128], rhs=uc.ap[:, dc, :], start=(dc == 0), stop=(dc == 7)),
                                 reads=[wt, uc], writes=[p1], signal=(dc == 7))
                        for dc in range(8):
                            k.op(k.pe, lambda: nc.tensor.matmul(p3.ap, lhsT=wt.ap[:, 1, dc, fi * 128:(fi + 1) * 128], rhs=uc.ap[:, dc, :], start=(dc == 0), stop=(dc == 7)),
                                 reads=[wt, uc], writes=[p3], signal=(dc == 7))
                        sb_ = sil.next()
                        k.op(k.act, lambda: nc.scalar.activation(out=sb_.ap, in_=p1.ap, func=AF.Silu), reads=[p1], writes=[sb_])
                        k.op(k.dve, lambda: nc.vector.tensor_tensor(out=gT.ap[:, fc, :], in0=sb_.ap, in1=p3.ap, op=ALU.mult), reads=[sb_, p3], writes=[gT])
                for dc in range(8):
                    pb = psr.next()
                    for fc in range(22):
                        k.op(k.pe, lambda: nc.tensor.matmul(pb.ap, lhsT=w2.ap[:, fc, dc * 128:(dc + 1) * 128], rhs=gT.ap[:, fc, :], start=(fc == 0), stop=(fc == 21)),
                             reads=[w2, gT], writes=[pb], signal=(fc == 21))
                    k.op(k.dve, lambda: nc.vector.tensor_tensor(out=hc.ap[:, dc, :], in0=pb.ap, in1=hc.ap[:, dc, :], op=ALU.add), reads=[pb, hc], writes=[hc])
            if not last:
                k.dma(k.sp, hst_ds[c % 2], hT_v[:, :, cols], hc.ap, reads=[hc], writes=[self.hT[0]])
                un = uns.next()
                self.rmsnorm_fm(hc, 8, TC, lambda dc: self.g_attn(l + 1, dc), un, lambda dc: un.ap[:, dc, :], sqr, ssps, lnb, rstd, float(D),
                                src_ap_fn=lambda dc: hc.ap[:, dc, :])
                k.dma(k.sp, un_ds[c % 2], uT_v[:, :, cols], un.ap, reads=[un], writes=[self.uT])
            else:
                for dc in range(8):
                    sq = sqr.next()
                    k.op(k.act, lambda: nc.scalar.activation(out=sq.ap, in_=hc.ap[:, dc, :], func=AF.Square), reads=[hc], writes=[sq])
                    k.op(k.pe, lambda: nc.tensor.matmul(ssps.ap, lhsT=self.ones_f.ap, rhs=sq.ap, start=(dc == 0), stop=(dc == 7)),
                         reads=[sq, self.ones_f], writes=[ssps])
                k.op(k.act, lambda: nc.scalar.activation(out=lnb.ap, in_=ssps.ap, func=AF.Ln, scale=1.0 / D, bias=self.epsb.ap), reads=[ssps, self.epsb], writes=[lnb])
                k.op(k.act, lambda: nc.scalar.activation(out=rstd.ap, in_=lnb.ap, func=AF.Exp, scale=-0.5), reads=[lnb], writes=[rstd])
                for dc in range(8):
                    k.op(k.dve, lambda: nc.vector.scalar_tensor_tensor(out=hc.ap[:, dc, :], in0=hc.ap[:, dc, :], scalar=self.g_final(dc), in1=rstd.ap, op0=ALU.mult, op1=ALU.mult),
                         reads=[hc, rstd, self.small], writes=[hc])
                for i in range(4):
                    ob = orow.next()
                    for half in range(2):
                        pb = psr.next()
                        for j in range(4):
                            dc = half * 4 + j
                            k.op(k.pe, lambda: nc.tensor.transpose(pb.ap[:, j * 128:(j + 1) * 128], hc.ap[:, dc, i * 128:(i + 1) * 128], self.ident_f.ap),
                                 reads=[hc, self.ident_f], writes=[pb], signal=(j == 3))
                        if half == 0:
                            k.op(k.act, lambda: nc.scalar.copy(out=ob.ap[:, 0:512], in_=pb.ap), reads=[pb], writes=[ob])
                        else:
                            k.op(k.dve, lambda: nc.vector.tensor_copy(out=ob.ap[:, 512:1024], in_=pb.ap), reads=[pb], writes=[ob])
                    k.dma(k.sp, or_ds[(orow.i - 1) % 2], self.out[c * TC + i * 128:c * TC + (i + 1) * 128, :], ob.ap, reads=[ob])

    def attn_core(self, c, lhs_fn, rhs_fn, rbufs, v_fn, vbuf, E, scale, st_ring, pt_ring, acc, tri=True, mask_fn=None, maskbuf=None, mask_eng=None):
        nc, k = self.nc, self.k
        nk = 4 * c + 4

        def emit_st(j):
            r = j - 4 * c
            off = 128 * max(r, 0)
            n = 512 - off
            stb = st_ring.next()
            ls = lhs_fn(j)
            rs = rhs_fn(off, n)
            nl_ = len(ls)
            for i in range(nl_):
                k.op(k.pe, lambda: nc.tensor.matmul(stb.ap[:, off:512], lhsT=ls[i], rhs=rs[i], start=(i == 0), stop=(i == nl_ - 1)),
                     reads=rbufs, writes=[stb], signal=(i == nl_ - 1))
            return stb, r, off, n

        nxt = emit_st(0)
        for j in range(nk):
            stb, r, off, n = nxt
            if j + 1 < nk:
                nxt = emit_st(j + 1)
            pt = pt_ring.next()
            k.op(k.act, lambda: nc.scalar.activation(out=pt.ap[:, off:512], in_=stb.ap[:, off:512], func=AF.Exp, scale=scale), reads=[stb], writes=[pt])
            if r >= 0 and tri:
                k.op(k.pool, lambda: nc.gpsimd.tensor_tensor(out=pt.ap[:, off:off + 128], in0=pt.ap[:, off:off + 128], in1=self.tri.ap, op=ALU.mult),
                     reads=[pt, self.tri], writes=[pt])
            if mask_fn is not None:
                me = mask_eng(j)
                mm = mask_fn(j, off, n)
                k.op(me, lambda: me.eng.tensor_tensor(out=pt.ap[:, off:512], in0=pt.ap[:, off:512], in1=mm, op=ALU.mult), reads=[pt, maskbuf], writes=[pt])
            for qt in range(max(r, 0), 4):
                k.op(k.pe, lambda: nc.tensor.matmul(acc[qt].ap[:, 0:E + 1], lhsT=pt.ap[:, qt * 128:(qt + 1) * 128], rhs=v_fn(j), start=(j == 0), stop=(j == 4 * c + qt)),
                     reads=[pt, vbuf], writes=[acc[qt]], signal=(qt == 3))

    def rope_fm(self, pa, np_, cs, cols, dst, dst_ap, xbr, rar, rbr, pb, split=None, split3=False):
        nc, k = self.nc, self.k
        import os as _os
        lvl = _os.environ.get("KROPE", "z")
        xb = xbr.next()
        ra = rar.next()
        rb = rbr.next()
        k.op(k.act, lambda: nc.scalar.copy(out=xb.ap[0:np_, :], in_=pa.ap[0:np_, :]), reads=[pa], writes=[xb])
        if lvl >= "b":
            k.op(k.pe, lambda: nc.tensor.matmul(pb.ap[0:np_, :], lhsT=self.rperm.ap[0:np_, 0:np_], rhs=xb.ap[0:np_, :], start=True, stop=True), reads=[xb, self.rperm], writes=[pb])
        if lvl >= "c":
            k.op(k.dve, lambda: nc.vector.tensor_tensor(out=ra.ap[0:np_, :], in0=pa.ap[0:np_, :], in1=cs.ap[0:np_, 0, cols], op=ALU.mult), reads=[pa, cs], writes=[ra])
        if lvl >= "d":
            k.op(k.dve, lambda: nc.vector.tensor_tensor(out=rb.ap[0:np_, :], in0=pb.ap[0:np_, :], in1=cs.ap[0:np_, 1, cols], op=ALU.mult), reads=[pb, cs], writes=[rb])
        if split is not None:
            for hh in range(2):
                oap = dst[hh].ap[hh * 64:(hh + 1) * 64, split, :] if split3 else dst[hh].ap[hh * 64:(hh + 1) * 64, split]
                k.op(k.pool, lambda: nc.gpsimd.tensor_tensor(out=oap, in0=ra.ap[hh * 64:(hh + 1) * 64, :], in1=rb.ap[hh * 64:(hh + 1) * 64, :], op=ALU.add),
                     reads=[ra, rb], writes=[dst[hh]])
        elif lvl >= "e":
            k.op(k.pool, lambda: nc.gpsimd.tensor_tensor(out=dst_ap, in0=ra.ap[0:np_, :], in1=rb.ap[0:np_, :], op=ALU.add), reads=[ra, rb], writes=[dst])
        else:
            k.op(k.act, lambda: nc.scalar.copy(out=dst_ap, in_=pa.ap[0:np_, :]), reads=[pa], writes=[dst])

    def load_full(self, st, name, dbuf, shape_inner, dt, nsplit=8):
        k = self.k
        t = self.sb(st, name, [128] + shape_inner, dt)
        v = dbuf.ap.rearrange("(c p) t -> p c t", p=128)
        g = Tok()
        ds = k.dsem()
        w = S // nsplit
        for i in range(nsplit):
            k.dma(k.sp, ds, t.ap[:, :, i * w:(i + 1) * w], v[:, :, i * w:(i + 1) * w], reads=[dbuf], writes=[t], group=g)
        return t

    def evac_norm(self, acc, E, dst, dst_ap_fn, recr):
        nc, k = self.nc, self.k
        for qt in range(4):
            rec = recr.next()
            k.op(k.dve, lambda: nc.vector.reciprocal(out=rec.ap, in_=acc[qt].ap[:, E:E + 1]), reads=[acc[qt]], writes=[rec])
            k.op(k.dve, lambda: nc.vector.tensor_scalar(out=dst_ap_fn(qt), in0=acc[qt].ap[:, 0:E], scalar1=rec.ap, scalar2=None, op0=ALU.mult), reads=[acc[qt], rec], writes=[dst])

    def transpose_out(self, src, src_ap_fn, pbuf, dst, dst_ap, eng):
        nc, k = self.nc, self.k
        for qt in range(4):
            k.op(k.pe, lambda: nc.tensor.transpose(pbuf.ap[:, qt * 128:(qt + 1) * 128], src_ap_fn(qt), self.ident_f.ap), reads=[src, self.ident_f], writes=[pbuf], signal=(qt == 3))
        if eng is k.act:
            k.op(eng, lambda: nc.scalar.copy(out=dst_ap, in_=pbuf.ap), reads=[pbuf], writes=[dst])
        else:
            k.op(eng, lambda: nc.vector.tensor_copy(out=dst_ap, in_=pbuf.ap), reads=[pbuf], writes=[dst])

    def diff_phase(self, st, l):
        nc, k = self.nc, self.k
        jj = l // 3
        uT = self.load_full(st, "uTf", self.uT, [8, S], BF16)
        cs = self.sb(st, "cs", [128, 2, S], F32)
        k.dma(k.sp, k.dsem(), cs.ap, self.cs_d.ap, reads=[self.cs_d], writes=[cs])
        wqkv = self.wb["wqkv%d" % l]
        wv = wqkv.ap.rearrange("(c p) f -> p c f", p=128)
        whs = Ring([self.sb(st, "wh%d" % i, [128, 8, 384], BF16) for i in range(2)])
        wh_ds = [k.dsem(), k.dsem()]
        qTz = [self.sb(st, "qTz%d" % i, [128, S], BF16) for i in range(2)]
        k.op(k.dve, lambda: nc.vector.memset(qTz[0].ap[64:128, :], 0.0), writes=[qTz[0]])
        k.op(k.dve, lambda: nc.vector.memset(qTz[1].ap[0:64, :], 0.0), writes=[qTz[1]])
        kTs = Ring([self.sb(st, "kT%d" % i, [128, S], BF16) for i in range(2)])
        Vs = Ring([self.sb(st, "V%d" % i, [128, 32, 130], BF16) for i in range(2)])
        for vb in Vs.bufs:
            k.op(k.dve, lambda: nc.vector.memset(vb.ap[:, :, 128:130], 1.0), writes=[vb])
        xbr = Ring([self.sb(st, "xb%d" % i, [128, TC], BF16) for i in range(2)])
        rar = Ring([self.sb(st, "ra%d" % i, [128, TC], F32) for i in range(2)])
        rbr = Ring([self.sb(st, "rb%d" % i, [128, TC], F32) for i in range(2)])
        pts = Ring([self.sb(st, "pt%d" % i, [128, TC], BF16) for i in range(4)])
        recr = Ring([self.sb(st, "rec%d" % i, [128, 1], F32) for i in range(8)])
        om = [self.sb(st, "om%d" % i, [128, 4, 128], F32) for i in range(2)]
        d4 = self.sb(st, "d4", [128, 4, 128], F32)
        sq4 = self.sb(st, "sq4", [128, 4, 128], F32)
        ss4 = self.sb(st, "ss4", [128, 4], F32)
        ln4 = self.sb(st, "ln4", [128, 4], F32)
        rs4 = self.sb(st, "rs4", [128, 4], F32)
        dn = Ring([self.sb(st, "dn%d" % i, [128, 4, 128], F32) for i in range(2)])
        oTh = Ring([self.sb(st, "oTh%d" % i, [128, S], BF16) for i in range(2)])
        oth_ds = [k.dsem(), k.dsem()]
        st_ring = Ring(self.ps[0:2])
        acc = self.ps[2:6]
        misc = Ring(self.ps[6:8])
        sub_ap = self.subln.ap[:, jj * 128:(jj + 1) * 128].unsqueeze(1).broadcast_to([128, 4, 128])
        scale = 64 ** -0.5
        import os as _os
        for h in range(int(_os.environ.get("KHEADS", "8"))):
            wh = whs.next()
            g = Tok()
            for i in range(3):
                k.dma(k.sp, wh_ds[h % 2], wh.ap[:, :, i * 128:(i + 1) * 128], wv[:, :, i * 1024 + h * 128:i * 1024 + (h + 1) * 128], reads=[wqkv], writes=[wh], group=g)
            kT, V = kTs.next(), Vs.next()
            import os as _os
            stage = float(_os.environ.get("KDIFF_STAGE", "3"))
            for tc in range(NCH):
                if stage < 0.5:
                    break
                if _os.environ.get("KTC1") and tc >= int(_os.environ.get("KTC1")):
                    break
                cols = slice(tc * TC, (tc + 1) * TC)
                for which, dstb in ((0, None), (1, kT)):
                    if _os.environ.get("KSKIPQK"):
                        break
                    pa = misc.next()
                    for dc in range(8):
                        k.op(k.pe, lambda: nc.tensor.matmul(pa.ap, lhsT=wh.ap[:, dc, which * 128:(which + 1) * 128], rhs=uT.ap[:, dc, cols], start=(dc == 0), stop=(dc == 7)),
                             reads=[wh, uT], writes=[pa], signal=(dc == 7))
                    pb = misc.next()
                    if stage < 0.7:
                        k.op(k.act, lambda: nc.scalar.copy(out=dstb.ap[:, cols], in_=pa.ap), reads=[pa], writes=[dstb])
                        continue
                    if which == 0:
                        self.rope_fm(pa, 128, cs, cols, qTz, None, xbr, rar, rbr, pb, split=cols)
                    else:
                        self.rope_fm(pa, 128, cs, cols, dstb, dstb.ap[:, cols], xbr, rar, rbr, pb)
                if _os.environ.get("KSKIPV"):
                    continue
                pa = misc.next()
                for i in range(4):
                    for dc in range(8):
                        k.op(k.pe, lambda: nc.tensor.matmul(pa.ap[:, i * 128:(i + 1) * 128], lhsT=uT.ap[:, dc, tc * TC + i * 128:tc * TC + (i + 1) * 128], rhs=wh.ap[:, dc, 256:384], start=(dc == 0), stop=(dc == 7)),
                             reads=[wh, uT], writes=[pa], signal=(dc == 7 and i == 3))
                k.op(k.act, lambda: nc.scalar.copy(out=V.ap[:, tc * 4:tc * 4 + 4, 0:128], in_=pa.ap.rearrange("p (a b) -> p a b", a=4)), reads=[pa], writes=[V])
            oT_h = oTh.next()
            if stage < 3:
                k.op(k.dve, lambda: nc.vector.memset(oT_h.ap, 0.0), writes=[oT_h])
            for c in range(NCH):
                if stage < 2:
                    break
                for m in range(2):
                    self.attn_core(c,
                                   lambda j: [kT.ap[:, j * 128:(j + 1) * 128]],
                                   lambda off, n: [qTz[m].ap[:, c * TC + off:c * TC + off + n]],
                                   [kT, qTz[m]], lambda j: V.ap[:, j, 0:129], V, 128, scale, st_ring, pts, acc)
                    self.evac_norm(acc, 128, om[m], lambda qt: om[m].ap[:, qt, :], recr)
                if stage < 3:
                    continue
                k.op(k.dve, lambda: nc.vector.scalar_tensor_tensor(out=d4.ap, in0=om[1].ap, scalar=self.neglam.ap[:, jj:jj + 1], in1=om[0].ap, op0=ALU.mult, op1=ALU.add),
                     reads=[om[0], om[1], self.neglam], writes=[d4])
                k.op(k.dve, lambda: nc.vector.tensor_tensor(out=sq4.ap, in0=d4.ap, in1=d4.ap, op=ALU.mult), reads=[d4], writes=[sq4])
                k.op(k.dve, lambda: nc.vector.tensor_reduce(out=ss4.ap, in_=sq4.ap, axis=AX.X, op=ALU.add), reads=[sq4], writes=[ss4])
                k.op(k.act, lambda: nc.scalar.activation(out=ln4.ap, in_=ss4.ap, func=AF.Ln, scale=1.0 / 128.0, bias=self.epsb.ap), reads=[ss4, self.epsb], writes=[ln4])
                k.op(k.act, lambda: nc.scalar.activation(out=rs4.ap, in_=ln4.ap, func=AF.Exp, scale=-0.5), reads=[ln4], writes=[rs4])
                dnb = dn.next()
                sub2 = self.subln.ap[:, jj * 128:(jj + 1) * 128]
                for qt in range(4):
                    k.op(k.dve, lambda: nc.vector.scalar_tensor_tensor(out=dnb.ap[:, qt, :], in0=d4.ap[:, qt, :], scalar=rs4.ap[:, qt:qt + 1], in1=sub2, op0=ALU.mult, op1=ALU.mult),
                         reads=[d4, rs4, self.subln], writes=[dnb])
                self.transpose_out(dnb, lambda qt: dnb.ap[:, qt, :], misc.next(), oT_h, oT_h.ap[:, c * TC:(c + 1) * TC], k.act)
            k.dma(k.sp, oth_ds[h % 2], self.oT.ap[h * 128:(h + 1) * 128, :], oT_h.ap, reads=[oT_h], writes=[self.oT])

    def mla_phase(self, st, l):
        nc, k = self.nc, self.k
        wd, wuq, wukv = self.wb["wdown%d" % l], self.wb["wuq%d" % l], self.wb["wukv%d" % l]
        cs = self.sb(st, "cs", [128, 2, S], F32)
        k.dma(k.sp, k.dsem(), cs.ap, self.cs_d.ap, reads=[self.cs_d], writes=[cs])
        wdsb = self.sb(st, "wdsb", [128, 8, 704], BF16)
        k.dma(k.sp, k.dsem(), wdsb.ap, wd.ap.rearrange("(c p) f -> p c f", p=128), reads=[wd], writes=[wdsb])
        cqn = self.sb(st, "cqn", [128, 3, S], BF16)
        ckvn = self.sb(st, "ckvn", [128, 2, S], BF16)
        krT = self.sb(st, "krT", [128, S], BF16)
        k.op(k.dve, lambda: nc.vector.memset(krT.ap[64:128, :], 0.0), writes=[krT])
        ucs = Ring([self.sb(st, "uc%d" % i, [128, 8, TC], BF16) for i in range(2)])
        u_ds = [k.dsem(), k.dsem()]
        sqr = Ring([self.sb(st, "sq%d" % i, [128, TC], F32) for i in range(2)])
        lnb = self.sb(st, "lnb", [128, TC], F32)
        rstd = self.sb(st, "rstd", [128, TC], F32)
        xbr = Ring([self.sb(st, "xb%d" % i, [128, TC], BF16) for i in range(2)])
        rar = Ring([self.sb(st, "ra%d" % i, [128, TC], F32) for i in range(2)])
        rbr = Ring([self.sb(st, "rb%d" % i, [128, TC], F32) for i in range(2)])
        uT_v = self.uT.ap.rearrange("(c p) t -> p c t", p=128)
        ps = self.ps
        for tc in range(NCH):
            cols = slice(tc * TC, (tc + 1) * TC)
            uc = ucs.next()
            k.dma(k.sp, u_ds[tc % 2], uc.ap, uT_v[:, :, cols], reads=[self.uT], writes=[uc])
            for i in range(3):
                for dc in range(8):
                    k.op(k.pe, lambda: nc.tensor.matmul(ps[i].ap, lhsT=wdsb.ap[:, dc, i * 128:(i + 1) * 128], rhs=uc.ap[:, dc, :], start=(dc == 0), stop=(dc == 7)),
                         reads=[wdsb, uc], writes=[ps[i]], signal=(dc == 7))
            self.rmsnorm_fm(None, 3, TC, lambda i: self.small.ap[:, 72 + i:73 + i], cqn, lambda i: cqn.ap[:, i, cols], sqr, ps[7], lnb, rstd, 384.0,
                            src_ap_fn=lambda i: ps[i].ap, src_bufs=[ps[0], ps[1], ps[2]])
            for i in range(2):
                for dc in range(8):
                    k.op(k.pe, lambda: nc.tensor.matmul(ps[3 + i].ap, lhsT=wdsb.ap[:, dc, 384 + i * 128:384 + (i + 1) * 128], rhs=uc.ap[:, dc, :], start=(dc == 0), stop=(dc == 7)),
                         reads=[wdsb, uc], writes=[ps[3 + i]], signal=(dc == 7))
            self.rmsnorm_fm(None, 2, TC, lambda i: self.small.ap[:, 75 + i:76 + i], ckvn, lambda i: ckvn.ap[:, i, cols], sqr, ps[7], lnb, rstd, 256.0,
                            src_ap_fn=lambda i: ps[3 + i].ap, src_bufs=[ps[3], ps[4]])
            for dc in range(8):
                k.op(k.pe, lambda: nc.tensor.matmul(ps[5].ap[0:64, :], lhsT=wdsb.ap[:, dc, 640:704], rhs=uc.ap[:, dc, :], start=(dc == 0), stop=(dc == 7)),
                     reads=[wdsb, uc], writes=[ps[5]], signal=(dc == 7))
            self.rope_fm(ps[5], 64, cs, cols, krT, krT.ap[0:64, cols], xbr, rar, rbr, ps[6])
        wqh = Ring([self.sb(st, "wqh%d" % i, [128, 3, 192], BF16) for i in range(2)])
        wkvh = Ring([self.sb(st, "wkvh%d" % i, [128, 2, 256], BF16) for i in range(2)])
        wq_ds = [k.dsem(), k.dsem()]
        wuq_v = wuq.ap.rearrange("(c p) f -> p c f", p=128)
        wukv_v = wukv.ap.rearrange("(c p) f -> p c f", p=128)
        qnT = self.sb(st, "qnT", [128, S], BF16)
        qrT = self.sb(st, "qrT", [128, S], BF16)
        k.op(k.dve, lambda: nc.vector.memset(qrT.ap[64:128, :], 0.0), writes=[qrT])
        knT = self.sb(st, "knT", [128, S], BF16)
        V = self.sb(st, "V", [128, 32, 130], BF16)
        k.op(k.dve, lambda: nc.vector.memset(V.ap[:, :, 128:130], 1.0), writes=[V])
        pts = Ring([self.sb(st, "pt%d" % i, [128, TC], BF16) for i in range(4)])
        recr = Ring([self.sb(st, "rec%d" % i, [128, 1], F32) for i in range(8)])
        on = Ring([self.sb(st, "on%d" % i, [128, 4, 128], F32) for i in range(2)])
        oTh = Ring([self.sb(st, "oTh%d" % i, [128, S], BF16) for i in range(2)])
        oth_ds = [k.dsem(), k.dsem()]
        st_ring = Ring(ps[0:2])
        acc = ps[2:6]
        misc = Ring(ps[6:8])
        scale = 192 ** -0.5
        for h in range(8):
            wq, wkv = wqh.next(), wkvh.next()
            g = Tok()
            k.dma(k.sp, wq_ds[h % 2], wq.ap, wuq_v[:, :, h * 192:(h + 1) * 192], reads=[wuq], writes=[wq], group=g)
            k.dma(k.sp, wq_ds[h % 2], wkv.ap, wukv_v[:, :, h * 256:(h + 1) * 256], reads=[wukv], writes=[wkv], group=g)
            for tc in range(NCH):
                cols = slice(tc * TC, (tc + 1) * TC)
                pa = misc.next()
                for i in range(3):
                    k.op(k.pe, lambda: nc.tensor.matmul(pa.ap, lhsT=wq.ap[:, i, 0:128], rhs=cqn.ap[:, i, cols], start=(i == 0), stop=(i == 2)), reads=[wq, cqn], writes=[pa], signal=(i == 2))
                k.op(k.act, lambda: nc.scalar.copy(out=qnT.ap[:, cols], in_=pa.ap), reads=[pa], writes=[qnT])
                pa = misc.next()
                for i in range(3):
                    k.op(k.pe, lambda: nc.tensor.matmul(pa.ap[0:64, :], lhsT=wq.ap[:, i, 128:192], rhs=cqn.ap[:, i, cols], start=(i == 0), stop=(i == 2)), reads=[wq, cqn], writes=[pa], signal=(i == 2))
                pb = misc.next()
                self.rope_fm(pa, 64, cs, cols, qrT, qrT.ap[0:64, cols], xbr, rar, rbr, pb)
                pa = misc.next()
                for i in range(2):
                    k.op(k.pe, lambda: nc.tensor.matmul(pa.ap, lhsT=wkv.ap[:, i, 0:128], rhs=ckvn.ap[:, i, cols], start=(i == 0), stop=(i == 1)), reads=[wkv, ckvn], writes=[pa], signal=(i == 1))
                k.op(k.dve, lambda: nc.vector.tensor_copy(out=knT.ap[:, cols], in_=pa.ap), reads=[pa], writes=[knT])
                pa = misc.next()
                for t4 in range(4):
                    for i in range(2):
                        k.op(k.pe, lambda: nc.tensor.matmul(pa.ap[:, t4 * 128:(t4 + 1) * 128], lhsT=ckvn.ap[:, i, tc * TC + t4 * 128:tc * TC + (t4 + 1) * 128], rhs=wkv.ap[:, i, 128:256], start=(i == 0), stop=(i == 1)),
                             reads=[wkv, ckvn], writes=[pa], signal=(i == 1 and t4 == 3))
                k.op(k.act, lambda: nc.scalar.copy(out=V.ap[:, tc * 4:tc * 4 + 4, 0:128], in_=pa.ap.rearrange("p (a b) -> p a b", a=4)), reads=[pa], writes=[V])
            oT_h = oTh.next()
            for c in range(NCH):
                self.attn_core(c,
                               lambda j: [knT.ap[:, j * 128:(j + 1) * 128], krT.ap[:, j * 128:(j + 1) * 128]],
                               lambda off, n: [qnT.ap[:, c * TC + off:c * TC + off + n], qrT.ap[:, c * TC + off:c * TC + off + n]],
                               [knT, krT, qnT, qrT], lambda j: V.ap[:, j, 0:129], V, 128, scale, st_ring, pts, acc)
                onb = on.next()
                self.evac_norm(acc, 128, onb, lambda qt: onb.ap[:, qt, :], recr)
                self.transpose_out(onb, lambda qt: onb.ap[:, qt, :], misc.next(), oT_h, oT_h.ap[:, c * TC:(c + 1) * TC], k.act)
            k.dma(k.sp, oth_ds[h % 2], self.oT.ap[h * 128:(h + 1) * 128, :], oT_h.ap, reads=[oT_h], writes=[self.oT])

    def dsa_phase(self, st, l):
        nc, k = self.nc, self.k
        win = self.wb["win%d" % l]
        wv_ = win.ap.rearrange("(c p) f -> p c f", p=128)
        Wq = self.sb(st, "Wq", [128, 8, 1024], BF16)
        Wkk = self.sb(st, "Wkk", [128, 8, 128], BF16)
        Wikk = self.sb(st, "Wikk", [128, 8, 128], BF16)
        Wv = self.sb(st, "Wv", [128, 8, 64], BF16)
        Wiq = self.sb(st, "Wiq", [128, 8, 512], BF16)
        Wiw = self.sb(st, "Wiw", [128, 8, 8], BF16)
        k.dma(k.sp, k.dsem(), Wq.ap, wv_[:, :, 0:1024], reads=[win], writes=[Wq])
        g = Tok(); dsx = k.dsem()
        k.dma(k.sp, dsx, Wkk.ap[:, :, 0:64], wv_[:, :, 1024:1088], reads=[win], writes=[Wkk], group=g)
        k.dma(k.sp, dsx, Wkk.ap[:, :, 64:128], wv_[:, :, 1024:1088], reads=[win], writes=[Wkk], group=g)
        k.dma(k.sp, dsx, Wikk.ap[:, :, 0:64], wv_[:, :, 1664:1728], reads=[win], writes=[Wikk], group=g)
        k.dma(k.sp, dsx, Wikk.ap[:, :, 64:128], wv_[:, :, 1664:1728], reads=[win], writes=[Wikk], group=g)
        k.dma(k.sp, dsx, Wv.ap, wv_[:, :, 1088:1152], reads=[win], writes=[Wv], group=g)
        k.dma(k.sp, dsx, Wiq.ap, wv_[:, :, 1152:1664], reads=[win], writes=[Wiq], group=g)
        k.dma(k.sp, dsx, Wiw.ap, wv_[:, :, 1728:1736], reads=[win], writes=[Wiw], group=g)
        KK = self.sb(st, "KK", [128, S], BF16)
        IKK = self.sb(st, "IKK", [128, S], BF16)
        Vaug = self.sb(st, "Vaug", [128, 32, 66], BF16)
        k.op(k.dve, lambda: nc.vector.memset(Vaug.ap[:, :, 64:66], 1.0), writes=[Vaug])
        ucs = Ring([self.sb(st, "uc%d" % i, [128, 8, TC], BF16) for i in range(1)])
        u_ds = [k.dsem()]
        csc = Ring([self.sb(st, "csc%d" % i, [128, 2, TC], F32) for i in range(2)])
        cs_ds = [k.dsem(), k.dsem()]
        xbr = Ring([self.sb(st, "xb%d" % i, [128, TC], BF16) for i in range(2)])
        rar = Ring([self.sb(st, "ra%d" % i, [128, TC], F32) for i in range(2)])
        rbr = Ring([self.sb(st, "rb%d" % i, [128, TC], F32) for i in range(2)])
        uT_v = self.uT.ap.rearrange("(c p) t -> p c t", p=128)
        ps = self.ps
        st_ring = Ring(ps[0:2])
        acc = ps[2:6]
        misc = Ring(ps[6:8])

        class CsView:
            pass

        def load_chunk(tc):
            cols = slice(tc * TC, (tc + 1) * TC)
            uc = ucs.next()
            k.dma(k.sp, u_ds[0], uc.ap, uT_v[:, :, cols], reads=[self.uT], writes=[uc])
            cc = csc.next()
            k.dma(k.sp, cs_ds[(csc.i - 1) % 2], cc.ap, self.cs_d.ap[:, :, cols], reads=[self.cs_d], writes=[cc])
            return uc, cc

        lcols = slice(0, TC)
        for tc in range(NCH):
            cols = slice(tc * TC, (tc + 1) * TC)
            uc, cc = load_chunk(tc)
            for W_, dstb in ((Wkk, KK), (Wikk, IKK)):
                pa = misc.next()
                for dc in range(8):
                    k.op(k.pe, lambda: nc.tensor.matmul(pa.ap, lhsT=W_.ap[:, dc, :], rhs=uc.ap[:, dc, :], start=(dc == 0), stop=(dc == 7)), reads=[W_, uc], writes=[pa], signal=(dc == 7))
                pb = misc.next()
                self.rope_fm(pa, 128, cc, lcols, dstb, dstb.ap[:, cols], xbr, rar, rbr, pb)
            pa = misc.next()
            for t4 in range(4):
                for dc in range(8):
                    k.op(k.pe, lambda: nc.tensor.matmul(pa.ap[:, t4 * 64:(t4 + 1) * 64], lhsT=uc.ap[:, dc, t4 * 128:(t4 + 1) * 128], rhs=Wv.ap[:, dc, :], start=(dc == 0), stop=(dc == 7)),
                         reads=[Wv, uc], writes=[pa], signal=(dc == 7 and t4 == 3))
            k.op(k.act, lambda: nc.scalar.copy(out=Vaug.ap[:, tc * 4:tc * 4 + 4, 0:64], in_=pa.ap[:, 0:256].rearrange("p (a b) -> p a b", a=4)), reads=[pa], writes=[Vaug])
        qTz2 = [self.sb(st, "qTcz%d" % i, [128, 8, TC], BF16) for i in range(2)]
        k.op(k.dve, lambda: nc.vector.memset(qTz2[0].ap[64:128, :, :], 0.0), writes=[qTz2[0]])
        k.op(k.dve, lambda: nc.vector.memset(qTz2[1].ap[0:64, :, :], 0.0), writes=[qTz2[1]])
        iqTc = self.sb(st, "iqTc", [128, 4, TC], BF16)
        iwc = self.sb(st, "iwc", [128, 4, 8], F32)
        accr = Ring([self.sb(st, "sacc%d" % i, [128, S], F32) for i in range(2)])
        mb = self.sb(st, "mb", [128, S], F32)
        maskT = self.sb(st, "maskT", [128, 32, TC], BF16)
        rlr = Ring([self.sb(st, "rl%d" % i, [128, TC], F32) for i in range(3)])
        lo = self.sb(st, "lo", [128, 1], F32)
        mid = self.sb(st, "mid", [128, 1], F32)
        cnt = self.sb(st, "cnt", [128, 1], F32)
        tt = self.sb(st, "tt", [128, 1], F32)
        pts = Ring([self.sb(st, "pt%d" % i, [128, TC], BF16) for i in range(4)])
        recr = Ring([self.sb(st, "rec%d" % i, [128, 1], F32) for i in range(8)])
        onp = Ring([self.sb(st, "onp%d" % i, [128, 4, 128], F32) for i in range(2)])
        oTc = self.sb(st, "oTc", [128, 8, TC], BF16)
        oc_ds = k.dsem()
        oT_v = self.oT.ap.rearrange("(c p) t -> p c t", p=128)
        scale = 64 ** -0.5
        iw_scale = (64 ** -0.5) * (8 ** -0.5)
        NIT = 18
        for c in range(NCH):
            cols = slice(c * TC, (c + 1) * TC)
            uc, cc = load_chunk(c)
            for pair in range(8):
                pa = misc.next()
                for dc in range(8):
                    k.op(k.pe, lambda: nc.tensor.matmul(pa.ap, lhsT=Wq.ap[:, dc, pair * 128:(pair + 1) * 128], rhs=uc.ap[:, dc, :], start=(dc == 0), stop=(dc == 7)), reads=[Wq, uc], writes=[pa], signal=(dc == 7))
                pb = misc.next()
                self.rope_fm(pa, 128, cc, lcols, qTz2, None, xbr, rar, rbr, pb, split=pair, split3=True)
            for pair in range(4):
                pa = misc.next()
                for dc in range(8):
                    k.op(k.pe, lambda: nc.tensor.matmul(pa.ap, lhsT=Wiq.ap[:, dc, pair * 128:(pair + 1) * 128], rhs=uc.ap[:, dc, :], start=(dc == 0), stop=(dc == 7)), reads=[Wiq, uc], writes=[pa], signal=(dc == 7))
                pb = misc.next()
                self.rope_fm(pa, 128, cc, lcols, iqTc, iqTc.ap[:, pair, :], xbr, rar, rbr, pb)
            pa = misc.next()
            for qt in range(4):
                for dc in range(8):
                    k.op(k.pe, lambda: nc.tensor.matmul(pa.ap[:, qt * 8:(qt + 1) * 8], lhsT=uc.ap[:, dc, qt * 128:(qt + 1) * 128], rhs=Wiw.ap[:, dc, :], start=(dc == 0), stop=(dc == 7)),
                         reads=[Wiw, uc], writes=[pa], signal=(dc == 7 and qt == 3))
            k.op(k.dve, lambda: nc.vector.tensor_scalar(out=iwc.ap, in0=pa.ap[:, 0:32].rearrange("p (a b) -> p a b", a=4), scalar1=iw_scale, scalar2=None, op0=ALU.mult), reads=[pa], writes=[iwc])
            for qt in range(4):
                L = c * TC + 128 * (qt + 1)
                sa = accr.next()
                for kc in range(c + 1):
                    n = min(TC, L - kc * TC)
                    for h in range(8):
                        b0 = 64 * (h % 2)
                        stb = st_ring.next()
                        k.op(k.pe, lambda: nc.tensor.matmul(stb.ap[:, 0:n], lhsT=iqTc.ap[b0:b0 + 64, h // 2, qt * 128:(qt + 1) * 128], rhs=IKK.ap[b0:b0 + 64, kc * TC:kc * TC + n], start=True, stop=True),
                             reads=[iqTc, IKK], writes=[stb])
                        rl = rlr.next()
                        k.op(k.act, lambda: nc.scalar.activation(out=rl.ap[:, 0:n], in_=stb.ap[:, 0:n], func=AF.Relu), reads=[stb], writes=[rl])
                        if h == 0:
                            k.op(k.dve, lambda: nc.vector.tensor_scalar(out=sa.ap[:, kc * TC:kc * TC + n], in0=rl.ap[:, 0:n], scalar1=iwc.ap[:, qt, 0:1], scalar2=None, op0=ALU.mult), reads=[rl, iwc], writes=[sa])
                        else:
                            k.op(k.dve, lambda: nc.vector.scalar_tensor_tensor(out=sa.ap[:, kc * TC:kc * TC + n], in0=rl.ap[:, 0:n], scalar=iwc.ap[:, qt, h:h + 1], in1=sa.ap[:, kc * TC:kc * TC + n], op0=ALU.mult, op1=ALU.add),
                                 reads=[rl, iwc, sa], writes=[sa])
                k.op(k.pool, lambda: nc.gpsimd.affine_select(out=sa.ap[:, L - 128:L], in_=sa.ap[:, L - 128:L], pattern=[[-1, 128]], compare_op=ALU.is_ge, fill=NEG, base=0, channel_multiplier=1),
                     reads=[sa], writes=[sa])
                k.op(k.dve, lambda: nc.vector.memset(lo.ap, -32.0), writes=[lo])
                k.op(k.dve, lambda: nc.vector.memset(mid.ap, 0.0), writes=[mid])
                w = 64.0
                for it in range(NIT):
                    k.op(k.dve, lambda: nc.vector.tensor_scalar(out=mb.ap[:, 0:L], in0=sa.ap[:, 0:L], scalar1=mid.ap, scalar2=None, op0=ALU.is_ge, op1=ALU.add, accum_out=cnt.ap), reads=[sa, mid], writes=[mb, cnt])
                    k.op(k.dve, lambda: nc.vector.tensor_scalar(out=tt.ap, in0=cnt.ap, scalar1=256.0, scalar2=w / 2, op0=ALU.is_ge, op1=ALU.mult), reads=[cnt], writes=[tt])
                    k.op(k.dve, lambda: nc.vector.tensor_tensor(out=lo.ap, in0=lo.ap, in1=tt.ap, op=ALU.add), reads=[lo, tt], writes=[lo])
                    k.op(k.dve, lambda: nc.vector.tensor_scalar(out=mid.ap, in0=lo.ap, scalar1=w / 4, scalar2=None, op0=ALU.add), reads=[lo], writes=[mid])
                    w = w / 2
                k.op(k.dve, lambda: nc.vector.tensor_scalar(out=mb.ap[:, 0:L], in0=sa.ap[:, 0:L], scalar1=lo.ap, scalar2=None, op0=ALU.is_ge), reads=[sa, lo], writes=[mb])
                nkt = L // 128
                for k0 in range(0, nkt, 4):
                    nb = min(4, nkt - k0)
                    pbk = misc.next()
                    for i in range(nb):
                        k.op(k.pe, lambda: nc.tensor.transpose(pbk.ap[:, i * 128:(i + 1) * 128], mb.ap[:, (k0 + i) * 128:(k0 + i + 1) * 128], self.ident_f.ap), reads=[mb, self.ident_f], writes=[pbk], signal=(i == nb - 1))
                    k.op(k.act, lambda: nc.scalar.copy(out=maskT.ap[:, k0:k0 + nb, qt * 128:(qt + 1) * 128], in_=pbk.ap[:, 0:nb * 128].rearrange("p (a b) -> p a b", a=nb)), reads=[pbk], writes=[maskT])
            for h in range(16):
                b0 = 64 * (h % 2)
                self.attn_core(c,
                               lambda j: [KK.ap[:, j * 128:(j + 1) * 128]],
                               lambda off, n: [qTz2[h % 2].ap[:, h // 2, off:off + n]],
                               [KK, qTz2[h % 2]], lambda j: Vaug.ap[:, j, 0:65], Vaug, 64, scale, st_ring, pts, acc, tri=False,
                               mask_fn=lambda j, off, n: maskT.ap[:, j, off:off + n], maskbuf=maskT, mask_eng=lambda j: (k.pool if j % 3 != 2 else k.dve))
                if h % 2 == 0:
                    onb = onp.next()
                self.evac_norm(acc, 64, onb, lambda qt: onb.ap[:, qt, b0:b0 + 64], recr)
                if h % 2 == 1:
                    self.transpose_out(onb, lambda qt: onb.ap[:, qt, :], misc.next(), oTc, oTc.ap[:, h // 2, :], k.act)
            k.dma(k.sp, oc_ds, oT_v[:, :, cols], oTc.ap, reads=[oTc], writes=[self.oT])


_PROG_CACHE = {}


def _get_prog(n_layers=DEPTH):
    if n_layers not in _PROG_CACHE:
        _PROG_CACHE[n_layers] = Prog(n_layers)
    return _PROG_CACHE[n_layers]


def _host_inputs(inputs):
    f = lambda a: np.ascontiguousarray(np.asarray(a, dtype=np.float32))
    an = f(inputs["attn_norm"]).reshape(4, 8, 128).transpose(2, 0, 1).reshape(128, 32)
    fn = f(inputs["ffn_norm"]).reshape(4, 8, 128).transpose(2, 0, 1).reshape(128, 32)
    fin = f(inputs["final_norm"]).reshape(8, 128).T
    qn = f(inputs["mla_q_norm"]).reshape(3, 128).T
    kn = f(inputs["mla_kv_norm"]).reshape(2, 128).T
    small = f(np.concatenate([an, fn, fin, qn, kn], axis=1))
    lam = f(np.broadcast_to(f(inputs["diff_lambda"]).reshape(1, 512), (128, 512)))
    subln = f(np.broadcast_to(f(inputs["diff_subln"]).reshape(1, 256), (128, 256)))
    shared = {"small": small, "lam": lam, "subln": subln}
    for l in range(DEPTH):
        m, j = l % 3, l // 3
        if m == 0:
            shared["wqkv%d" % l] = f(inputs["diff_wqkv"][j])
            shared["wo%d" % l] = f(inputs["diff_wo"][j])
        elif m == 1:
            shared["win%d" % l] = f(inputs["dsa_win"][j])
            shared["wo%d" % l] = f(inputs["dsa_wo"][j])
        else:
            shared["wdown%d" % l] = f(inputs["mla_wdown"][j])
            shared["wuq%d" % l] = f(inputs["mla_wuq"][j])
            shared["wukv%d" % l] = f(inputs["mla_wukv"][j])
            shared["wo%d" % l] = f(inputs["mla_wo"][j])
        shared["w1_%d" % l] = f(inputs["ffn_w1"][l])
        shared["w3_%d" % l] = f(inputs["ffn_w3"][l])
        shared["w2_%d" % l] = f(inputs["ffn_w2"][l])
    return shared


def kernel(**inputs):
    x = np.asarray(inputs["x"], dtype=np.float32)
    B = x.shape[0]
    shared = _host_inputs(inputs)
    prog = _get_prog(DEPTH)
    in_maps = []
    for b in range(B):
        m = dict(shared)
        m["x"] = np.ascontiguousarray(x[b])
        in_maps.append(m)
    res = run_bass_kernel_spmd(prog.nc, in_maps, core_ids=list(range(B)))
    return np.stack([np.asarray(r["out"], dtype=np.float32) for r in res.results], axis=0)
```
